# Optimizing a Trainium2 kernel written in Bass

```python
import jax, jax.numpy as jnp
from jax import lax
import numpy as np

D_MODEL = 1024
BATCH = 2
SEQ = 8192
DEPTH = 2

HEAD_DIM = 64
A_Q_HEADS = 6
A_KV_HEADS = 2
WINDOW = 128
B_HEADS = 6
MOBA_BLOCK = 256
MOBA_TOPK = 3
MOBA_Q_CHUNK = 64
C_HEADS = 12
FOX_Q_BLOCK = 128
MEM_HEADS = 4
MEM_LEN = 256
EVEN_MIX = (A_Q_HEADS + B_HEADS + MEM_HEADS) * HEAD_DIM
ODD_MIX = (C_HEADS + MEM_HEADS) * HEAD_DIM
EVEN_SPLIT = [A_Q_HEADS * HEAD_DIM, A_KV_HEADS * HEAD_DIM, A_KV_HEADS * HEAD_DIM,
              B_HEADS * HEAD_DIM, B_HEADS * HEAD_DIM, B_HEADS * HEAD_DIM,
              MEM_HEADS * HEAD_DIM, EVEN_MIX]
ODD_SPLIT = [C_HEADS * HEAD_DIM, C_HEADS * HEAD_DIM, C_HEADS * HEAD_DIM, C_HEADS,
             MEM_HEADS * HEAD_DIM, ODD_MIX]
EVEN_IN = sum(EVEN_SPLIT)
ODD_IN = sum(ODD_SPLIT)
N_ALIBI = A_Q_HEADS + B_HEADS
EPS = 1e-6
NEG = -1e30
SCALE = HEAD_DIM ** -0.5

kernel_name = 'hybrid_swa_moba_fox_memory_trunk'


def rms_norm(x, g):
    xf = x.astype(jnp.float32)
    y = xf * lax.rsqrt(jnp.mean(xf * xf, axis=-1, keepdims=True) + EPS)
    return (y * g.astype(jnp.float32)).astype(x.dtype)


def split_cols(h, sizes):
    return jnp.split(h, np.cumsum(sizes)[:-1].tolist(), axis=-1)


def split_heads(t, n):
    return t.reshape(t.shape[0], t.shape[1], n, HEAD_DIM)


def alibi_slopes():
    h = jnp.arange(1, N_ALIBI + 1, dtype=jnp.float32)
    return 2.0 ** (-8.0 * h / N_ALIBI)


def sliding_window_sink_attn(q, k, v, sinks, slopes):
    B, S, HQ, _ = q.shape
    HKV = k.shape[2]
    G = HQ // HKV
    W = WINDOW
    nb = S // W
    qb = q.reshape(B, nb, W, HKV, G, HEAD_DIM)

    def band(t):
        tb = t.reshape(B, nb, W, HKV, HEAD_DIM)
        prev = jnp.pad(tb, ((0, 0), (1, 0), (0, 0), (0, 0), (0, 0)))[:, :-1]
        return jnp.concatenate([prev, tb], axis=2)

    kb, vb = band(k), band(v)
    logits = jnp.einsum('bnqkgd,bnskd->bnkgqs', qb, kb,
                        preferred_element_type=jnp.float32) * SCALE
    qpos = jnp.arange(nb)[:, None] * W + jnp.arange(W)[None, :]
    kpos = jnp.arange(nb)[:, None] * W - W + jnp.arange(2 * W)[None, :]
    dist = qpos[:, :, None] - kpos[:, None, :]
    allowed = (dist >= 0) & (dist < W) & (kpos[:, None, :] >= 0)
    sl = slopes.reshape(HKV, G)[None, None, :, :, None, None]
    logits = logits - sl * dist[None, :, None, None].astype(jnp.float32)
    logits = jnp.where(allowed[None, :, None, None], logits, NEG)
    sink = jnp.broadcast_to(
        sinks.astype(jnp.float32).reshape(HKV, G)[None, None, :, :, None, None],
        logits.shape[:-1] + (1,))
    p = jax.nn.softmax(jnp.concatenate([logits, sink], axis=-1), axis=-1)[..., :-1]
    o = jnp.einsum('bnkgqs,bnskd->bnqkgd', p.astype(v.dtype), vb)
    return o.reshape(B, S, HQ * HEAD_DIM)


def moba_attn(q, k, v, slopes):
    B, S, H, _ = q.shape
    L = MOBA_BLOCK
    nblk = -(-S // L)
    Sp = nblk * L
    pad = ((0, 0), (0, Sp - S), (0, 0), (0, 0))
    q, k, v = [jnp.pad(t, pad).transpose(0, 2, 1, 3) for t in (q, k, v)]
    kblk = k.reshape(B, H, nblk, L, HEAD_DIM)
    vblk = v.reshape(B, H, nblk, L, HEAD_DIM)
    kmean = jnp.mean(kblk.astype(jnp.float32), axis=3)
    gate = jnp.einsum('bhtd,bhnd->bhtn', q.astype(jnp.float32), kmean)
    qblk_id = jnp.arange(Sp) // L
    past = jnp.arange(nblk)[None, :] < qblk_id[:, None]
    gate = jnp.where(past, gate, NEG)
    K = min(MOBA_TOPK, nblk)
    _, sel = lax.top_k(gate, K)
    sel_valid = jnp.arange(K)[None, :] < qblk_id[:, None]

    C = MOBA_Q_CHUNK
    nch = Sp // C
    q_c = q.reshape(B, H, nch, C, HEAD_DIM).transpose(2, 0, 1, 3, 4)
    sel_c = sel.reshape(B, H, nch, C, K).transpose(2, 0, 1, 3, 4)
    valid_c = sel_valid.reshape(nch, C, K)
    qpos_c = jnp.arange(Sp).reshape(nch, C)
    bi = jnp.arange(B)[:, None, None, None]
    hi = jnp.arange(H)[None, :, None, None]
    sl = slopes.astype(jnp.float32)
    offs = jnp.arange(L)

    def chunk_fn(args):
        qc, selc, validc, qposc = args
        own = qposc[0] // L
        ksel = kblk[bi, hi, selc]
        vsel = vblk[bi, hi, selc]
        kown = lax.dynamic_index_in_dim(kblk, own, axis=2, keepdims=False)
        vown = lax.dynamic_index_in_dim(vblk, own, axis=2, keepdims=False)
        s_sel = jnp.einsum('bhcd,bhckld->bhckl', qc, ksel,
                           preferred_element_type=jnp.float32) * SCALE
        dist_sel = (qposc[None, None, :, None, None]
                    - (selc[..., None] * L + offs)).astype(jnp.float32)
        s_sel = s_sel - sl[None, :, None, None, None] * dist_sel
        s_sel = jnp.where(validc[None, None, :, :, None], s_sel, NEG)
        s_own = jnp.einsum('bhcd,bhld->bhcl', qc, kown,
                           preferred_element_type=jnp.float32) * SCALE
        dist_own = qposc[:, None] - (own * L + offs)[None, :]
        s_own = s_own - sl[None, :, None, None] * dist_own.astype(jnp.float32)[None, None]
        s_own = jnp.where((dist_own >= 0)[None, None], s_own, NEG)
        logits = jnp.concatenate([s_sel.reshape(B, H, C, K * L), s_own], axis=-1)
        p = jax.nn.softmax(logits, axis=-1).astype(v.dtype)
        p_sel = p[..., :K * L].reshape(B, H, C, K, L)
        p_own = p[..., K * L:]
        return (jnp.einsum('bhckl,bhckld->bhcd', p_sel, vsel)
                + jnp.einsum('bhcl,bhld->bhcd', p_own, vown))

    o = lax.map(chunk_fn, (q_c, sel_c, valid_c, qpos_c))
    o = o.transpose(1, 0, 3, 2, 4).reshape(B, Sp, H * HEAD_DIM)
    return o[:, :S]


def forgetting_attn(q, k, v, f_logit):
    B, S, H, _ = q.shape
    logf = jax.nn.log_sigmoid(f_logit.astype(jnp.float32))
    c = jnp.cumsum(logf, axis=1)
    cT = c.transpose(0, 2, 1)
    Q = FOX_Q_BLOCK
    nb = S // Q
    qb = q.reshape(B, nb, Q, H, HEAD_DIM).transpose(1, 0, 2, 3, 4)
    cb = c.reshape(B, nb, Q, H).transpose(1, 0, 3, 2)
    kpos = jnp.arange(S)

    def block_fn(args):
        qblk, cblk, n = args
        s = jnp.einsum('bqhd,bshd->bhqs', qblk, k,
                       preferred_element_type=jnp.float32) * SCALE
        s = s + cblk[..., None] - cT[:, :, None, :]
        qpos = n * Q + jnp.arange(Q)
        s = jnp.where((kpos[None, :] <= qpos[:, None])[None, None], s, NEG)
        p = jax.nn.softmax(s, axis=-1).astype(v.dtype)
        return jnp.einsum('bhqs,bshd->bqhd', p, v)

    o = lax.map(block_fn, (qb, cb, jnp.arange(nb)))
    return o.transpose(1, 0, 2, 3, 4).reshape(B, S, H * HEAD_DIM)


def head_rms(t, g):
    tf = t.astype(jnp.float32)
    y = tf * lax.rsqrt(jnp.mean(tf * tf, axis=-1, keepdims=True) + EPS)
    return (y * g.astype(jnp.float32)).astype(t.dtype)


def memory_kv(mem, mem_norm_g, w_mem_kv, k_gain):
    hm = rms_norm(mem, mem_norm_g)
    mk, mv = jnp.split(hm @ w_mem_kv, 2, axis=-1)
    return head_rms(split_heads(mk, MEM_HEADS), k_gain), split_heads(mv, MEM_HEADS)


def memory_cross_attn(q, mk, mv):
    B, S = q.shape[0], q.shape[1]
    s = jnp.einsum('bshd,bmhd->bhsm', q, mk, preferred_element_type=jnp.float32) * SCALE
    p = jax.nn.softmax(s, axis=-1).astype(mv.dtype)
    return jnp.einsum('bhsm,bmhd->bshd', p, mv).reshape(B, S, MEM_HEADS * HEAD_DIM)


def even_layer(x, mem, norm_g, w_in, qk_g, sinks, mem_norm_g, w_mem_kv, w_out):
    h = rms_norm(x, norm_g)
    aq, ak, av, bq, bk, bv, mq, z = split_cols(h @ w_in, EVEN_SPLIT)
    aq = head_rms(split_heads(aq, A_Q_HEADS), qk_g[0])
    ak = head_rms(split_heads(ak, A_KV_HEADS), qk_g[1])
    av = split_heads(av, A_KV_HEADS)
    bq = head_rms(split_heads(bq, B_HEADS), qk_g[2])
    bk = head_rms(split_heads(bk, B_HEADS), qk_g[3])
    bv = split_heads(bv, B_HEADS)
    mq = head_rms(split_heads(mq, MEM_HEADS), qk_g[4])
    mk, mv = memory_kv(mem, mem_norm_g, w_mem_kv, qk_g[5])
    slopes = alibi_slopes()
    ya = sliding_window_sink_attn(aq, ak, av, sinks, slopes[:A_Q_HEADS])
    yb = moba_attn(bq, bk, bv, slopes[A_Q_HEADS:])
    ym = memory_cross_attn(mq, mk, mv)
    y = jnp.concatenate([ya, yb, ym], axis=-1) * jax.nn.silu(z)
    return x + y @ w_out


def odd_layer(x, mem, norm_g, w_in, qk_g, b_f, mem_norm_g, w_mem_kv, w_out):
    h = rms_norm(x, norm_g)
    cq, ck, cv, cf, mq, z = split_cols(h @ w_in, ODD_SPLIT)
    cq = head_rms(split_heads(cq, C_HEADS), qk_g[0])
    ck = head_rms(split_heads(ck, C_HEADS), qk_g[1])
    cv = split_heads(cv, C_HEADS)
    mq = head_rms(split_heads(mq, MEM_HEADS), qk_g[2])
    mk, mv = memory_kv(mem, mem_norm_g, w_mem_kv, qk_g[3])
    yc = forgetting_attn(cq, ck, cv, cf + b_f)
    ym = memory_cross_attn(mq, mk, mv)
    y = jnp.concatenate([yc, ym], axis=-1) * jax.nn.silu(z)
    return x + y @ w_out


def setup_inputs(seed: int = 0) -> dict:
    key = jax.random.key(seed)
    ks = jax.random.split(key, 18)
    NE = (DEPTH + 1) // 2
    NO = DEPTH // 2
    D = D_MODEL
    f32 = jnp.float32

    def w(k, shape, fan_in):
        return jax.random.normal(k, shape, f32) * fan_in ** -0.5

    def gain(k, shape):
        return 1.0 + 0.02 * jax.random.normal(k, shape, f32)

    return {
        'x': jax.random.normal(ks[0], (BATCH, SEQ, D), f32),
        'mem': jax.random.normal(ks[1], (BATCH, MEM_LEN, D), f32),
        'e_norm': gain(ks[2], (NE, D)),
        'e_w_in': w(ks[3], (NE, D, EVEN_IN), D),
        'e_qk_norm': gain(ks[4], (NE, 6, HEAD_DIM)),
        'e_sinks': 0.5 * jax.random.normal(ks[5], (NE, A_Q_HEADS), f32),
        'e_mem_norm': gain(ks[6], (NE, D)),
        'e_w_mem_kv': w(ks[7], (NE, D, 2 * MEM_HEADS * HEAD_DIM), D),
        'e_w_out': w(ks[8], (NE, EVEN_MIX, D), EVEN_MIX),
        'o_norm': gain(ks[9], (NO, D)),
        'o_w_in': w(ks[10], (NO, D, ODD_IN), D),
        'o_qk_norm': gain(ks[11], (NO, 4, HEAD_DIM)),
        'o_b_f': jax.random.uniform(ks[12], (NO, C_HEADS), f32, 1.0, 3.0),
        'o_mem_norm': gain(ks[13], (NO, D)),
        'o_w_mem_kv': w(ks[14], (NO, D, 2 * MEM_HEADS * HEAD_DIM), D),
        'o_w_out': w(ks[15], (NO, ODD_MIX, D), ODD_MIX),
    }


def reference(x, mem, e_norm, e_w_in, e_qk_norm, e_sinks, e_mem_norm, e_w_mem_kv, e_w_out,
              o_norm, o_w_in, o_qk_norm, o_b_f, o_mem_norm, o_w_mem_kv, o_w_out):
    for layer in range(DEPTH):
        i = layer // 2
        if layer % 2 == 0:
            x = even_layer(x, mem, e_norm[i], e_w_in[i], e_qk_norm[i], e_sinks[i],
                           e_mem_norm[i], e_w_mem_kv[i], e_w_out[i])
        else:
            x = odd_layer(x, mem, o_norm[i], o_w_in[i], o_qk_norm[i], o_b_f[i],
                          o_mem_norm[i], o_w_mem_kv[i], o_w_out[i])
    return x
```

```python
from contextlib import ExitStack
import numpy as np
import ml_dtypes
import concourse.bass as bass
import concourse.mybir as mybir
from concourse.bass_utils import run_bass_kernel_spmd

F32 = mybir.dt.float32
BF16 = mybir.dt.bfloat16
AF = mybir.ActivationFunctionType
ALU = mybir.AluOpType

S = 8192
D = 1024
HD = 64
NG = 16
EPS = 1e-6
SCALE = HD ** -0.5
NEGB = -30000.0
N_ALIBI = 12


class Op:
    __slots__ = ("eng", "fn", "reads", "writes", "kind", "grp", "ticket", "deps", "need_inc", "idx")


class Prog:
    def __init__(self, nc):
        self.nc = nc
        self.ops = []
        self.last_w = {}
        self.readers = {}
        self.bar = None

    def add(self, eng, fn, reads=(), writes=(), kind="c", grp=None):
        op = Op()
        op.eng, op.fn, op.kind, op.grp = eng, fn, kind, grp
        op.reads, op.writes = tuple(reads), tuple(writes)
        op.ticket, op.need_inc = None, False
        op.idx = len(self.ops)
        deps = set()
        for r in op.reads:
            w = self.last_w.get(r)
            if w is not None:
                deps.add(w)
        for w_ in op.writes:
            w = self.last_w.get(w_)
            if w is not None:
                deps.add(w)
            deps.update(self.readers.get(w_, ()))
        if self.bar is not None:
            deps.add(self.bar)
        deps.discard(op.idx)
        op.deps = sorted(deps)
        for r in op.reads:
            self.readers.setdefault(r, []).append(op.idx)
        for w_ in op.writes:
            self.last_w[w_] = op.idx
            self.readers[w_] = []
        self.ops.append(op)
        return op

    def pe(self, fn, reads=(), writes=()):
        return self.add("pe", fn, reads, writes)

    def act(self, fn, reads=(), writes=()):
        return self.add("act", fn, reads, writes)

    def dve(self, fn, reads=(), writes=()):
        return self.add("dve", fn, reads, writes)

    def pool(self, fn, reads=(), writes=()):
        return self.add("pool", fn, reads, writes)

    def dma(self, fn, reads=(), writes=(), q="sp", grp="d0"):
        return self.add(q, fn, reads, writes, kind="d", grp=grp)

    def barrier(self):
        op = self.add("sp", lambda e: e.nop(), (), ())
        last = {}
        for o in self.ops[:-1]:
            if o.kind == "c":
                last[("e", o.eng)] = o.idx
            else:
                last[(o.kind, o.grp, o.idx)] = o.idx
        op.deps = sorted(set(last.values()))
        self.bar = op.idx
        self.last_w = {}
        self.readers = {}

    def emit(self, stack):
        nc = self.nc
        ops = self.ops

        def skip(dop, op):
            return dop.kind == "c" and dop.eng == "pe" and op.eng == "pe" and op.kind == "c"

        for op in ops:
            if op.kind != "c":
                op.need_inc = True
            for d in op.deps:
                if not skip(ops[d], op):
                    ops[d].need_inc = True
        cnt = {}
        sems = {}
        NSLOT = {"sp": 24, "pool": 6, "act": 4, "cc": 8}
        rr = {}
        for op in ops:
            if not op.need_inc:
                continue
            if op.kind == "c":
                key, inc = ("e", op.eng), 1
            elif op.kind == "d":
                i = rr.get(op.eng, 0)
                rr[op.eng] = i + 1
                key, inc = ("d", op.eng, i % NSLOT[op.eng]), 16
            else:
                i = rr.get("cc", 0)
                rr["cc"] = i + 1
                key, inc = ("cc", i % NSLOT["cc"]), 1
            cnt[key] = cnt.get(key, 0) + inc
            op.ticket = (key, cnt[key])
            if key not in sems:
                sems[key] = stack.enter_context(nc.semaphore("s_" + "_".join(str(k) for k in key)))
        final = dict(cnt)
        engobj = {"pe": nc.tensor, "act": nc.scalar, "dve": nc.vector, "pool": nc.gpsimd, "sp": nc.sync}
        block = stack.enter_context(nc.Block())

        def stream(ename):
            def body(_e):
                e = engobj[ename]
                seen = {}
                for op in ops:
                    if op.eng != ename:
                        continue
                    need = {}
                    for d in op.deps:
                        dop = ops[d]
                        if dop.ticket is None or skip(dop, op):
                            continue
                        k, v = dop.ticket
                        if v > need.get(k, 0):
                            need[k] = v
                    for k, v in need.items():
                        if seen.get(k, 0) >= v:
                            continue
                        e.wait_ge(sems[k], v)
                        seen[k] = v
                    if op.kind != "c" and op.ticket is not None:
                        k, v = op.ticket
                        prev = v - (16 if op.kind == "d" else 1)
                        if prev > 0 and seen.get(k, 0) < prev:
                            e.wait_ge(sems[k], prev)
                            seen[k] = prev
                    ins = op.fn(e)
                    if op.ticket is not None:
                        k, v = op.ticket
                        ins.then_inc(sems[k], 16 if op.kind == "d" else 1)
                if ename == "sp":
                    for k, v in final.items():
                        if k[0] in ("d", "cc"):
                            e.wait_ge(sems[k], v)
            return body

        for en, deco in (("pe", block.tensor), ("act", block.scalar), ("dve", block.vector),
                         ("pool", block.gpsimd), ("sp", block.sync)):
            deco(stream(en))


def layer_cfg(L):
    if L == 0:
        return dict(NT=5, NV=192, NSLOT=6, NBIG=2, NZ=1536, KR=96, mq_tile=1, nvh=3, nf=0)
    return dict(NT=4, NV=195, NSLOT=4, NBIG=3, NZ=1024, KR=65, mq_tile=1, nvh=3, nf=3)


def alibi_slopes():
    h = np.arange(1, N_ALIBI + 1, dtype=np.float32)
    return (2.0 ** (-8.0 * h / N_ALIBI)).astype(np.float32)


def build(n_layers=2, debug=False, stop=None):
    nc = bass.Bass("TRN2", target_bir_lowering=False)
    P = Prog(nc)
    AX = mybir.AxisListType.X

    def din(name, shape, dt=F32):
        return nc.dram_tensor(name, list(shape), dt, kind="ExternalInput").ap()

    xT = din("xT", [D, S])
    memT = din("memT", [D, 256])
    out = nc.dram_tensor("out", [D, 2048], F32, kind="ExternalOutput").ap()
    cst_bf = din("cst_bf", [128, 4, 128], BF16)
    cst_f = din("cst_f", [128, 2, 128])
    kaug = din("kaug", [32, S], BF16)
    pastb = din("pastb", [128, 32, 32])
    ownz = din("ownz", [128, 32, 32])
    LW = []
    for L in range(2):
        c = layer_cfg(L)
        LW.append(dict(
            wt=din(f"wt{L}", [D, c["NT"] * 128]), wv=din(f"wv{L}", [D, c["NV"]]),
            wmk=din(f"wmk{L}", [D, 128]), wmv=din(f"wmv{L}", [D, 64]),
            wz=din(f"wz{L}", [D, c["NZ"]]), wo=din(f"wo{L}", [c["NZ"], D]),
            gx=din(f"gx{L}", [128, 8]), gm=din(f"gm{L}", [128, 8]),
            gcol=din(f"gcol{L}", [128, c["NT"] + 1]),
        ))
    maskA = din("maskA", [128, 3, 2, 128])
    sinkb = din("sinkb", [128, 3])
    rq0 = din("rq0", [128, 2, 64])
    kb0 = din("kb0", [128, 2, 64])
    bfb = din("bfb", [128, 3])
    QTs = nc.dram_tensor("QTs", [3, 97, S], BF16).ap()
    KTs = nc.dram_tensor("KTs", [3, 64, S], BF16).ap()
    VAs = nc.dram_tensor("VAs", [3, 128, 64, 128], BF16).ap()
    YT = [[nc.dram_tensor(f"YT{L}_{i}", [64, S], BF16).ap() for i in range(layer_cfg(L)["NSLOT"])] for L in range(2)]
    YG = [[nc.dram_tensor(f"YG{L}_{i}", [256, S], BF16).ap() for i in range(layer_cfg(L)["NSLOT"])] for L in range(2)]
    X1in = [[nc.dram_tensor(f"X1in{g}_{h}", [512, 512], F32).ap() for h in range(2)] for g in range(4)]
    X1out = [[nc.dram_tensor(f"X1out{g}_{h}", [2048, 512], F32).ap() for h in range(2)] for g in range(4)]
    if debug:
        dbg_y = [nc.dram_tensor("dbg_y0", [384, S], BF16, kind="ExternalOutput").ap(),
                 nc.dram_tensor("dbg_y1", [256, S], BF16, kind="ExternalOutput").ap()]
        dbg_x1 = nc.dram_tensor("dbg_x1", [D, 2048], F32, kind="ExternalOutput").ap()

    with ExitStack() as st:
        def sb(name, shape, dt=F32):
            return st.enter_context(nc.sbuf_tensor(name, list(shape), dt))

        dyn_cache = {}
        pb = [st.enter_context(nc.psum_tensor(f"pb{i}", [128, 512], F32)) for i in range(7)]
        pbT = st.enter_context(nc.psum_tensor("pbT", [128, 1024], BF16))
        rot = {}

        def bank(role, banks):
            i = rot.get(role, 0)
            rot[role] = i + 1
            b = banks[i % len(banks)]
            return pb[b], ("pb", b)

        def MM(out_, lhsT, rhs, start, stop, r, w):
            P.pe(lambda e: e.matmul(out_, lhsT=lhsT, rhs=rhs, start=start, stop=stop), r, w)

        def TR(out_, in_, idn, r, w):
            P.pe(lambda e: e.transpose(out=out_, in_=in_, identity=idn), r, w)

        def ACTV(out_, in_, func, r, w, bias=None, scale=None):
            kw = {}
            if bias is not None:
                kw["bias"] = bias
            if scale is not None:
                kw["scale"] = scale
            P.act(lambda e: e.activation(out=out_, in_=in_, func=func, **kw), r, w)

        def TT(eng, out_, in0, in1, op, r, w):
            P.add(eng, lambda e: e.tensor_tensor(out=out_, in0=in0, in1=in1, op=op), r, w)

        def STT(out_, in0, scalar, in1, op0, op1, r, w):
            P.dve(lambda e: e.scalar_tensor_tensor(out=out_, in0=in0, scalar=scalar, in1=in1, op0=op0, op1=op1), r, w)

        def TS(out_, in0, s1, op0, r, w):
            P.dve(lambda e: e.tensor_scalar(out=out_, in0=in0, scalar1=s1, scalar2=None, op0=op0), r, w)

        def CP(eng, out_, in_, r, w):
            P.add(eng, lambda e: e.tensor_copy(out=out_, in_=in_), r, w)

        def RECIP(out_, in_, r, w):
            P.dve(lambda e: e.reciprocal(out=out_, in_=in_), r, w)

        def MAX8(out_, in_, r, w):
            P.dve(lambda e: e.max(out=out_, in_=in_), r, w)

        def RED(out_, in_, r, w):
            P.dve(lambda e: e.tensor_reduce(out=out_, in_=in_, axis=AX, op=ALU.add), r, w)

        def MEMSET(ap, val, w):
            P.pool(lambda e: e.memset(ap, val), (), w)

        def AG(src, dst, r, w):
            P.add("pool", lambda e: e.collective_compute("AllGather", ALU.bypass, replica_groups=[[0, 1, 2, 3], [4, 5, 6, 7]],
                                                        ins=[src.opt()], outs=[dst.opt()]), r, w, kind="cc", grp="g")

        def DMA(out_, in_, r, w, q="sp", grp="ld", force=False):
            op = P.dma(lambda e: e.dma_start(out=out_, in_=in_), r, w, q=q, grp=grp)
            if force:
                op.need_inc = True
            return op

        cbf = sb("cbf", [128, 4, 128], BF16)
        cf = sb("cf", [128, 2, 128])
        ident, ones, onesbd, tri = cbf[:, 0, :], cbf[:, 1, :], cbf[:, 2, :], cbf[:, 3, :]
        Umat, E127 = cf[:, 0, :], cf[:, 1, :]
        ARENA = sb("arena", [128, 24576], BF16)
        Kg = ARENA[:, 0:8192]
        Vg = ARENA[:, 8192:16384].rearrange("p (g c) -> p g c", c=128)
        WTb = sb("WTb", [128, 8, 640], BF16)
        Wvb = sb("Wvb", [128, 8, 195], BF16)
        WMKb = sb("WMKb", [128, 8, 128], BF16)
        WMVb = sb("WMVb", [128, 8, 64], BF16)
        gx = sb("gx", [128, 8]); gmm = sb("gmm", [128, 8]); gcol = sb("gcol", [128, 6])
        xs = [sb(f"xs{i}", [128, 8, 512]) for i in range(2)]
        hT = [sb(f"hT{i}", [128, 8, 512], BF16) for i in range(2)]
        sq = sb("sq", [128, 8, 512], BF16)
        lnv = [sb(f"lnv{i}", [128, 512]) for i in range(2)]
        rs = [sb(f"rs{i}", [128, 512]) for i in range(2)]
        sq1 = [sb(f"sq1{i}", [128, 512], BF16) for i in range(2)]
        stg = [sb(f"stg{i}", [128, 512], BF16) for i in range(5)]
        Vstg = [sb(f"Vstg{i}", [128, 3, 4, 128], BF16) for i in range(2)]
        mkT = sb("mkT", [128, 256], BF16)
        mva = sb("mva", [128, 2, 128], BF16)
        Pm = sb("Pm", [128, 2, 512], BF16)
        Pt = [sb(f"Pt{i}", [128, 512], BF16) for i in range(4)]
        Qg = [sb(f"Qg{i}", [128, 512], BF16) for i in range(3)]
        rec = [sb(f"rec{i}", [128, 512]) for i in range(2)]
        ystg = [sb(f"ystg{i}", [128, 512], BF16) for i in range(4)]
        kbt = sb("kbt", [128, 3, 64])
        pastb_s = sb("pastb_s", [128, 32, 32])
        ownz_s = sb("ownz_s", [128, 32, 32])
        rq0_s = sb("rq0_s", [128, 2, 64])
        maskA_s = sb("maskA_s", [128, 3, 2, 128]); sink_s = sb("sink_s", [128, 3]); esink = sb("esink", [128, 3])
        KAr = sb("KAr", [128, 5, 128], BF16)
        VAr = sb("VAr", [128, 5, 128], BF16)
        kmT = sb("kmT", [128, 32], BF16)
        ksum = sb("ksum", [128, 32])
        gmt = sb("gmt", [128, 4, 32]); top8 = sb("top8", [128, 4, 8]); thr = sb("thr", [128, 4, 1])
        selx = sb("selx", [128, 4, 32])
        selm = sb("selm", [128, 4, 32], BF16)
        selT = sb("selT", [32, 512], BF16)
        swS = sb("swS", [128, 2, 128]); swP = sb("swP", [128, 2, 128], BF16)
        bfb_s = sb("bfb_s", [128, 3])
        lall = sb("lall", [128, 64, 3]); nloc = sb("nloc", [128, 64, 3]); scA = sb("scA", [128, 64, 3]); scB = sb("scB", [128, 64, 3])
        nbf = sb("nbf", [128, 64, 3], BF16)
        fb = sb("fb", [128, 3]); fe = sb("fe", [128, 3])
        rstg = sb("rstg", [3, 512], BF16)
        yg = sb("yg", [128, 12, 512], BF16)
        ez = [sb(f"ez{i}", [128, 512], BF16) for i in range(3)]
        WZ = ARENA[:, 0:12288]
        WO = ARENA[:, 12288:24576]

        DMA(cbf[:], cst_bf, [], ["cbf"])
        DMA(cf[:], cst_f, [], ["cf"])
        DMA(pastb_s[:], pastb, [], ["pastb_s"])
        DMA(ownz_s[:], ownz, [], ["ownz_s"])
        DMA(rq0_s[:], rq0, [], ["rq0_s"])
        DMA(maskA_s[:], maskA, [], ["maskA_s"])
        DMA(sink_s[:], sinkb, [], ["sink_s"])
        DMA(bfb_s[:], bfb, [], ["bfb_s"])
        ACTV(esink[:], sink_s[:], AF.Exp, ["sink_s"], ["esink"])

        def rmsnorm(xsrc, xkey, hdst, hkey, gains, n):
            ACTV(sq[:, :, 0:n], xsrc, AF.Square, [xkey], ["sq"])
            ps, pk = bank("ss", [2])
            for t in range(8):
                MM(ps[:, 0:n], ones, sq[:, t, 0:n], t == 0, t == 7, ["sq", "cbf"], [pk])
            ACTV(lnv[0][:, 0:n], ps[:, 0:n], AF.Ln, [pk], ["lnv0"], bias=EPS, scale=1.0 / D)
            ACTV(rs[0][:, 0:n], lnv[0][:, 0:n], AF.Exp, ["lnv0"], ["rs0"], scale=-0.5)
            for t in range(8):
                STT(hdst[:, t, :], xsrc[:, t, :], gains[:, t:t + 1], rs[0][:, 0:n], ALU.mult, ALU.mult,
                    [xkey, "rs0", "gains"], [hkey])

        def head_rms(ps, pk, n, gc, dst, dkey, i):
            ACTV(sq1[i][:, 0:n], ps[:, 0:n], AF.Square, [pk], [f"sq1{i}"])
            p2, p2k = bank("ss", [2])
            MM(p2[:, 0:n], onesbd, sq1[i][:, 0:n], True, True, [f"sq1{i}", "cbf"], [p2k])
            ACTV(lnv[i][:, 0:n], p2[:, 0:n], AF.Ln, [p2k], [f"lnv{i}"], bias=EPS, scale=1.0 / HD)
            ACTV(rs[i][:, 0:n], lnv[i][:, 0:n], AF.Exp, [f"lnv{i}"], [f"rs{i}"], scale=-0.5)
            STT(dst, ps[:, 0:n], gc, rs[i][:, 0:n], ALU.mult, ALU.mult, [pk, f"rs{i}", "gains"], [dkey])

        def finalize(ops_, opk, dst, dkey, ri, extra=None):
            r = rec[ri]
            rk = f"rec{ri}"
            if extra is None:
                RECIP(r[64:128, :], ops_[64:128, :], [opk], [rk])
            else:
                TS(r[64:128, :], ops_[64:128, :], extra, ALU.add, [opk, "esink"], [rk])
                RECIP(r[64:128, :], r[64:128, :], [rk], [rk])
            TT("dve", dst, ops_[0:64, :], r[64:128, :], ALU.mult, [opk, rk], [dkey])

        def layer(L):
            c = layer_cfg(L)
            W = LW[L]
            NT, NV, KR, NBIG, NSLOT = c["NT"], c["NV"], c["KR"], c["NBIG"], c["NSLOT"]
            xT_v = xT.rearrange("(t p) n -> p t n", p=128)
            out_v = out.rearrange("(t p) n -> p t n", p=128)
            last = (L == n_layers - 1)

            def load_x_full(xb, xk, tg):
                if L == 0:
                    DMA(xb[:], xT_v[:, :, tg * 512:(tg + 1) * 512], [], [xk])
                else:
                    r, g = tg // 4, tg % 4
                    for h in range(2):
                        DMA(xb[:, 4 * h:4 * h + 4, :], X1out[g][h].rearrange("(r t p) n -> r p t n", r=4, t=4)[r],
                            [("X1out", g, h)], [xk])

            def DMAdyn(out_, src_v, base, r, w):
                def f(e):
                    if "j2048" not in dyn_cache:
                        dyn_cache["j2048"] = e.snap((e.partition_id() % 4) * 2048)
                    return e.dma_start(out=out_, in_=src_v[:, :, bass.ds(dyn_cache["j2048"] + base, 512)])
                P.dma(f, r, w, q="sp", grp="ld")
            P.barrier()
            wv3 = lambda a: a.rearrange("(t p) c -> p t c", p=128)
            DMA(WTb[:, :, 0:NT * 128], wv3(W["wt"]), [], ["WTb"], q="pool", grp="w")
            DMA(Wvb[:, :, 0:NV], wv3(W["wv"]), [], ["Wvb"], q="pool", grp="w")
            DMA(WMKb[:], wv3(W["wmk"]), [], ["WMKb"], q="pool", grp="w")
            DMA(WMVb[:], wv3(W["wmv"]), [], ["WMVb"], q="pool", grp="w")
            DMA(gx[:], W["gx"], [], ["gains"])
            DMA(gmm[:], W["gm"], [], ["gains"])
            DMA(gcol[:, 0:NT + 1], W["gcol"], [], ["gains"])
            mx = xs[1][:, :, 0:256]
            DMA(mx, memT.rearrange("(t p) n -> p t n", p=128), [], ["xs1"])
            mh = hT[1][:, :, 0:256]
            rmsnorm(mx, "xs1", mh, "hT1", gmm, 256)
            ps, pk = bank("pa", [0, 1])
            for t in range(8):
                MM(ps[:, 0:256], WMKb[:, t, :], mh[:, t, :], t == 0, t == 7, ["WMKb", "hT1"], [pk])
            head_rms(ps, pk, 256, gcol[:, NT:NT + 1], mkT[:], "mkT", 0)
            MEMSET(mva[:, :, 64:128], 1.0, ["mva1"])
            for mt in range(2):
                ps, pk = bank("pv", [3])
                for t in range(8):
                    MM(ps[:, 0:64], mh[:, t, mt * 128:(mt + 1) * 128], WMVb[:, t, :], t == 0, t == 7, ["WMVb", "hT1"], [pk])
                ACTV(mva[:, mt, 0:64], ps[:, 0:64], AF.Copy, [pk], ["mva0"])
            if L == 0:
                DMA(kbt[:, 0:2, :], kb0, [], ["kbt"])
                MEMSET(ksum[:], 0.0, ["ksum"])
                MEMSET(kmT[:], 0.0, ["kmT"])
                MEMSET(VAr[:, :, 64:128], 1.0, ["VAr1"])
            for i in range(2):
                MEMSET(Vstg[i][:, :, :, 64:128], 1.0, [f"Vstg{i}"])

            for tg in range(NG):
                c0 = tg * 512
                xb, hb = xs[tg % 2], hT[tg % 2]
                xk, hk = f"xs{tg % 2}", f"hT{tg % 2}"
                load_x_full(xb, xk, tg)
                rmsnorm(xb[:], xk, hb, hk, gx, 512)
                for ti in range(NT):
                    ps, pk = bank("pa", [0, 1])
                    for t in range(8):
                        MM(ps[:], WTb[:, t, ti * 128:(ti + 1) * 128], hb[:, t, :], t == 0, t == 7, ["WTb", hk], [pk])
                    head_rms(ps, pk, 512, gcol[:, ti:ti + 1], stg[ti][:], f"stg{ti}", ti % 2)
                vs = Vstg[tg % 2]
                vsk = f"Vstg{tg % 2}"
                for tt in range(4):
                    G = tg * 4 + tt
                    ps, pk = bank("pv", [3])
                    for t in range(8):
                        MM(ps[:, 0:NV], hb[:, t, tt * 128:(tt + 1) * 128], Wvb[:, t, 0:NV], t == 0, t == 7, ["Wvb", hk], [pk])
                    ACTV(vs[:, :, tt, 0:64], ps[:, 0:192].rearrange("p (h c) -> p h c", c=64), AF.Copy, [pk], [vsk])
                    if L == 0:
                        CP("pool", VAr[:, tt + 1, 0:64], vs[:, 0, tt, 0:64], [vsk], ["VAr0"])
                    else:
                        TT("dve", fb[:], ps[:, 192:195], bfb_s[:], ALU.add, [pk, "bfb_s"], ["fb"])
                        ACTV(fe[:], fb[:], AF.Exp, ["fb"], ["fe"], scale=-1.0)
                        ACTV(lall[:, G, :], fe[:], AF.Ln, ["fe"], ["lall"], bias=1.0)
                if L == 0:
                    DMA(VAs[0:2, :, tg * 4:tg * 4 + 4, :].rearrange("h p g c -> p h g c"), vs[:, 1:3, :, :],
                        [vsk], [("VAs", 0), ("VAs", 1)], grp="st")
                    for u in range(2):
                        DMA(QTs[u][0:64, c0:c0 + 512], stg[3][u * 64:(u + 1) * 64, :], ["stg3"], [("QTs", u)], grp="st")
                        DMA(KTs[u][:, c0:c0 + 512], stg[4][u * 64:(u + 1) * 64, :], ["stg4"], [("KTs", u)], grp="st")
                else:
                    DMA(VAs[0:3, :, tg * 4:tg * 4 + 4, :].rearrange("h p g c -> p h g c"), vs[:, 0:3, :, :],
                        [vsk], [("VAs", 0), ("VAs", 1), ("VAs", 2)], grp="st")
                    for u in range(3):
                        qsrc = stg[0][u * 64:(u + 1) * 64, :] if u < 2 else stg[1][0:64, :]
                        ksrc = stg[2][u * 64:(u + 1) * 64, :] if u < 2 else stg[3][0:64, :]
                        DMA(QTs[u][0:64, c0:c0 + 512], qsrc, ["stg0" if u < 2 else "stg1"], [("QTs", u)], grp="st")
                        DMA(KTs[u][:, c0:c0 + 512], ksrc, ["stg2" if u < 2 else "stg3"], [("KTs", u)], grp="st")
                mslot = NSLOT - 1
                for mt in range(2):
                    ps, pk = bank("ms", [4, 5])
                    MM(ps[:], mkT[64:128, mt * 128:(mt + 1) * 128], stg[1][64:128, :], True, True, ["mkT", "stg1"], [pk])
                    ACTV(Pm[:, mt, :], ps[:], AF.Exp, [pk], ["Pm"])
                po, pok = bank("o", [6])
                for mt in range(2):
                    MM(po[:], mva[:, mt, :], Pm[:, mt, :], mt == 0, mt == 1, ["Pm", "mva0", "mva1"], [pok])
                ys = ystg[tg % 2]
                ysk = f"ystg{tg % 2}"
                finalize(po, pok, ys[0:64, :], ysk, 0)
                DMA(YT[L][mslot][:, c0:c0 + 512], ys[0:64, :], [ysk], [("YT", L, mslot)], grp="st")

                if L == 0:
                    RED(ksum[:, 2 * tg:2 * tg + 2], stg[4][:].rearrange("p (b t) -> p b t", t=256), ["stg4"], ["ksum"])
                    CP("dve", kmT[:, 2 * tg:2 * tg + 2], ksum[:, 2 * tg:2 * tg + 2], ["ksum"], ["kmT"])
                    for u in range(2):
                        gp, gpk = bank("g", [3])
                        for qt in range(4):
                            MM(gp[:, qt * 32:(qt + 1) * 32], stg[3][u * 64:(u + 1) * 64, qt * 128:(qt + 1) * 128],
                               kmT[u * 64:(u + 1) * 64, :], True, True, ["stg3", "kmT"], [gpk])
                        TT("dve", gmt[:].rearrange("p (b s) n -> p b s n", s=2),
                           gp[:, 0:128].rearrange("p (b s n) -> p b s n", b=2, s=2),
                           pastb_s[:, 2 * tg:2 * tg + 2, :].unsqueeze(2).to_broadcast([128, 2, 2, 32]), ALU.add,
                           [gpk, "pastb_s"], ["gmt"])
                        for qt in range(4):
                            MAX8(top8[:, qt, :], gmt[:, qt, :], ["gmt"], ["top8"])
                        TS(thr[:], top8[:, :, 2:3], -1e29, ALU.max, ["top8"], ["thr"])
                        TT("dve", selx[:], gmt[:], thr[:].to_broadcast([128, 4, 32]), ALU.is_lt, ["gmt", "thr"], ["selx"])
                        TT("dve", selm[:, :, 1:32].rearrange("p (b s) n -> p b s n", s=2),
                           selx[:, :, 0:31].rearrange("p (b s) n -> p b s n", s=2),
                           ownz_s[:, 2 * tg:2 * tg + 2, 0:31].unsqueeze(2).to_broadcast([128, 2, 2, 31]), ALU.mult,
                           ["selx", "ownz_s"], ["selm"])
                        CP("dve", selm[:, :, 0:1], rq0_s[:, u, 4 * tg:4 * tg + 4].unsqueeze(2), ["rq0_s"], ["selm"])
                        for qt in range(4):
                            TR(pbT[0:32, qt * 128:(qt + 1) * 128], selm[:, qt, :], ident, ["selm", "cbf"], ["pbT"])
                        ACTV(selT[:], pbT[0:32, 0:512], AF.Copy, ["pbT"], ["selT"])
                        DMA(QTs[u][64:96, c0:c0 + 512], selT[:], ["selT"], [("QTs", u)], grp="st")
                    CP("pool", KAr[:, 1:5, :], stg[2][:].rearrange("p (t c) -> p t c", c=128), ["stg2"], ["KArc"])
                    for hh in range(3):
                        half = hh % 2 if hh < 2 else 0
                        qsrc = stg[0] if hh < 2 else stg[1]
                        qkey = "stg0" if hh < 2 else "stg1"
                        b0 = half * 64
                        yb = ystg[2 + (hh % 2)]
                        ybk = f"ystg{2 + (hh % 2)}"
                        po, pok = bank("o", [6])
                        for qt in range(4):
                            first = (tg == 0 and qt == 0)
                            kts = [1] if first else [0, 1]
                            k0 = kts[0]
                            sp_, spk = bank("ms", [4, 5])
                            for kk in kts:
                                MM(sp_[:, kk * 128:(kk + 1) * 128], KAr[b0:b0 + 64, qt + kk, :], qsrc[b0:b0 + 64, qt * 128:(qt + 1) * 128],
                                   True, True, ["KArc", "KArp", qkey], [spk])
                            TT("dve", swS[:, k0:2, :], sp_[:, k0 * 128:256].rearrange("p (k q) -> p k q", q=128),
                               maskA_s[:, hh, k0:2, :], ALU.add, [spk, "maskA_s"], ["swS"])
                            ACTV(swP[:, k0:2, :], swS[:, k0:2, :], AF.Exp, ["swS"], ["swP"])
                            for kk in kts:
                                MM(po[:, qt * 128:(qt + 1) * 128], VAr[:, qt + kk, :], swP[:, kk, :], kk == k0, kk == 1,
                                   ["swP", "VAr0", "VAr1", "VArp"], [pok])
                        finalize(po, pok, yb[0:64, :], ybk, 1, extra=esink[64:128, hh:hh + 1])
                        DMA(YT[0][hh][:, c0:c0 + 512], yb[0:64, :], [ybk], [("YT", 0, hh)], grp="st")
                    CP("pool", KAr[:, 0, :], KAr[:, 4, :], ["KArc"], ["KArp"])
                    CP("pool", VAr[:, 0, 0:64], VAr[:, 4, 0:64], ["VAr0"], ["VArp"])

            if stop == "A0" and L == 0:
                return
            if L == 1:
                fl = lambda a: a.rearrange("p g h -> p (g h)")
                ps, pk = bank("pa", [0, 1])
                MM(ps[:, 0:192], Umat, fl(lall[:]), True, True, ["cf", "lall"], [pk])
                ACTV(fl(nloc[:]), ps[:, 0:192], AF.Copy, [pk], ["nloc"])
                ps2, pk2 = bank("pa", [0, 1])
                MM(ps2[:, 0:192], E127, fl(nloc[:]), True, True, ["cf", "nloc"], [pk2])
                ACTV(fl(scA[:]), ps2[:, 0:192], AF.Copy, [pk2], ["scA"])
                a_, b_, ak_, bk_ = scA, scB, "scA", "scB"
                for d in (1, 2, 4, 8, 16, 32):
                    TT("dve", b_[:, d:64, :], a_[:, d:64, :], a_[:, 0:64 - d, :], ALU.add, [ak_], [bk_])
                    CP("dve", b_[:, 0:d, :], a_[:, 0:d, :], [ak_], [bk_])
                    a_, b_, ak_, bk_ = b_, a_, bk_, ak_
                TT("dve", kbt[:, :, 1:64].rearrange("p h g -> p g h"), nloc[:, 1:64, :], a_[:, 0:63, :], ALU.add, [ak_, "nloc"], ["kbt"])
                CP("dve", kbt[:, :, 0:1].rearrange("p h g -> p g h"), nloc[:, 0:1, :], ["nloc"], ["kbt"])
                CP("dve", nbf[:], kbt[:].rearrange("p h g -> p g h"), ["kbt"], ["nbf"])
                for ch in range(16):
                    ps, pk = bank("pa", [0, 1])
                    for tt in range(4):
                        G = ch * 4 + tt
                        MM(ps[0:3, tt * 128:(tt + 1) * 128], nbf[:, G, :], ident, True, True, ["nbf", "cbf"], [pk])
                    ACTV(rstg[:], ps[0:3, :], AF.Copy, [pk], ["rstg"], scale=-1.0)
                    DMA(QTs[0:3, 64, ch * 512:(ch + 1) * 512], rstg[:], ["rstg"], [("QTs", 0), ("QTs", 1), ("QTs", 2)], grp="st")

            if stop == "A":
                return
            P.barrier()
            DMA(Kg[64:96, :], kaug, [], ["Kaug"])
            for s_ in range(NBIG):
                DMA(Kg[0:64, :], KTs[s_], [("KTs", s_)], ["Kg"])
                DMA(Vg, VAs[s_], [("VAs", s_)], ["Vg"])
                yslot = 3 + s_ if L == 0 else s_
                for qg in range(NG):
                    c0 = qg * 512
                    qb = Qg[qg % 3]
                    qk_ = f"Qg{qg % 3}"
                    DMA(qb[0:KR, :], QTs[s_][0:KR, c0:c0 + 512], [("QTs", s_)], [qk_], grp="lq")
                    po, pok = bank("O", [3, 4])
                    nkt = 4 * qg + 4
                    pend = []
                    for G in range(nkt + 2):
                        if G < nkt:
                            kt = G - 4 * qg
                            n0 = 128 * kt if kt > 0 else 0
                            sp_, spk = bank("S", [0, 1, 2])
                            MM(sp_[:, n0:512], Kg[0:KR, G * 128:(G + 1) * 128], qb[0:KR, n0:512], True, kt < 0,
                               ["Kg", "Kaug", qk_], [spk])
                            if kt >= 0:
                                MM(sp_[:, n0:n0 + 128], ident, tri, False, True, ["cbf"], [spk])
                            pend.append((G, n0, sp_, spk))
                        if G >= 2:
                            G2, n2, sp2, spk2 = pend.pop(0)
                            pt = Pt[G2 % 4]
                            ptk = f"Pt{G2 % 4}"
                            ACTV(pt[:, n2:512], sp2[:, n2:512], AF.Exp, [spk2, "kbt"], [ptk], bias=kbt[:, s_, G2:G2 + 1])
                            MM(po[:, n2:512], Vg[:, G2, :], pt[:, n2:512], G2 == 0, G2 == nkt - 1, [ptk, "Vg"], [pok])
                    ys = ystg[qg % 2]
                    ysk = f"ystg{qg % 2}"
                    finalize(po, pok, ys[0:64, :], ysk, qg % 2)
                    DMA(YT[L][yslot][:, c0:c0 + 512], ys[0:64, :], [ysk], [("YT", L, yslot)], grp="st")

            if stop == "B":
                return
            P.barrier()
            for sl in range(NSLOT):
                if debug:
                    DMA(dbg_y[L][sl * 64:(sl + 1) * 64, :], YT[L][sl], [("YT", L, sl)], [("dbgy", L, sl)], grp="so", force=True)
                AG(YT[L][sl], YG[L][sl], [("YT", L, sl)], [("YG", L, sl)])
            if stop == "G":
                return
            NZ = c["NZ"]
            NCT = NZ // 128
            WZv = WZ[:, 0:8 * NZ].rearrange("p (t c) -> p t c", t=8)
            WOv = WO[:, 0:NCT * 1024].rearrange("p (t c) -> p t c", t=NCT)
            DMA(WZv, wv3(W["wz"]), [], ["WZ"], q="pool", grp="w")
            DMA(WOv, wv3(W["wo"]), [], ["WO"], q="pool", grp="w")
            for tgq in range(4):
                c0 = tgq * 512
                xb, hb = xs[tgq % 2], hT[tgq % 2]
                xk, hk = f"xs{tgq % 2}", f"hT{tgq % 2}"
                if L == 0:
                    DMAdyn(xb[:], xT_v, c0, [], [xk])
                else:
                    for h in range(2):
                        DMA(xb[:, 4 * h:4 * h + 4, :], X1in[tgq][h].rearrange("(t p) n -> p t n", p=128), [("X1in", tgq, h)], [xk])
                for sl in range(NSLOT):
                    DMAdyn(yg[:, 2 * sl:2 * sl + 2, :], YG[L][sl].rearrange("(t p) n -> p t n", p=128), c0, [("YG", L, sl)],
                           [("ygl", sl), ("ygc", 2 * sl), ("ygc", 2 * sl + 1)])
                rmsnorm(xb[:], xk, hb, hk, gx, 512)
                for ct in range(NCT):
                    ps, pk = bank("pa", [0, 1])
                    for t in range(8):
                        MM(ps[:], WZv[:, t, ct * 128:(ct + 1) * 128], hb[:, t, :], t == 0, t == 7, ["WZ", hk], [pk])
                    ezb = ez[ct % 3]
                    ezk = f"ez{ct % 3}"
                    ACTV(ezb[:], ps[:], AF.Silu, [pk], [ezk])
                    TT("pool" if ct % 2 else "dve", yg[:, ct, :], yg[:, ct, :], ezb[:], ALU.mult, [("ygl", ct // 2), ezk], [("ygc", ct)])
                for oc in range(8):
                    ps, pk = bank("po", [3, 4])
                    for ct in range(NCT):
                        MM(ps[:], WOv[:, ct, oc * 128:(oc + 1) * 128], yg[:, ct, :], ct == 0, ct == NCT - 1, ["WO", ("ygc", ct)], [pk])
                    TT("dve", xb[:, oc, :], ps[:], xb[:, oc, :], ALU.add, [pk, xk], [xk])
                if last:
                    DMA(out_v[:, :, c0:c0 + 512], xb[:], [xk], [("out", tgq)], grp="so")
                else:
                    for h in range(2):
                        DMA(X1in[tgq][h].rearrange("(t p) n -> p t n", p=128), xb[:, 4 * h:4 * h + 4, :], [xk], [("X1in", tgq, h)], grp="so")
                        AG(X1in[tgq][h], X1out[tgq][h], [("X1in", tgq, h)], [("X1out", tgq, h)])
                if debug and L == 0:
                    DMA(dbg_x1.rearrange("(t p) n -> p t n", p=128)[:, :, c0:c0 + 512], xb[:], [xk], [("dbgx", tgq)], grp="so")

        for L in range(n_layers):
            layer(L)
        P.emit(st)
    return nc


def _bf(a):
    return np.ascontiguousarray(a).astype(ml_dtypes.bfloat16)


def _consts():
    idn = np.eye(128, dtype=np.float32)
    ones = np.ones((128, 128), np.float32)
    bd = np.zeros((128, 128), np.float32)
    bd[:64, :64] = 1
    bd[64:, 64:] = 1
    k = np.arange(128)[:, None]
    q = np.arange(128)[None, :]
    tri = np.where(k > q, NEGB, 0.0).astype(np.float32)
    cst_bf = _bf(np.stack([idn, ones, bd, tri], 1))
    U = (k <= q).astype(np.float32)
    E = np.zeros((128, 128), np.float32)
    E[127, :] = 1
    cst_f = np.ascontiguousarray(np.stack([U, E], 1))
    kaug = np.zeros((32, S), np.float32)
    kaug[0] = 1
    for n in range(31):
        kaug[1 + n, n * 256:(n + 1) * 256] = NEGB
    pb = np.where(np.arange(32)[None, :] >= np.arange(32)[:, None], -1e30, 0.0).astype(np.float32)
    pastb = np.ascontiguousarray(np.broadcast_to(pb[None], (128, 32, 32)))
    oz = (np.arange(32)[None, :] != np.arange(32)[:, None]).astype(np.float32)
    ownz = np.ascontiguousarray(np.broadcast_to(oz[None], (128, 32, 32)))
    return dict(cst_bf=cst_bf, cst_f=cst_f, kaug=_bf(kaug), pastb=pastb, ownz=ownz)


def _col(v):
    return np.ascontiguousarray(v.reshape(8, 128).T)


def _core_inputs(inp, b, j):
    sl = alibi_slopes()
    m = {}
    m["xT"] = np.ascontiguousarray(inp["x"][b].T)
    m["memT"] = np.ascontiguousarray(inp["mem"][b].T)
    w = inp["e_w_in"][0]
    qk = inp["e_qk_norm"][0]
    ga = j % 2
    ah = [3 * ga + i for i in range(3)]
    bh = [(2 * j) % 6, (2 * j + 1) % 6]
    u = lambda c0: w[:, c0:c0 + 64]
    aq = [u(64 * h) for h in ah]
    ak = u(384 + 64 * ga)
    bq = [u(640 + 64 * h) for h in bh]
    bk = [u(1024 + 64 * h) for h in bh]
    mq = u(1792 + 64 * j)
    m["wt0"] = np.ascontiguousarray(np.concatenate([aq[0], aq[1], aq[2], mq, ak, ak, bq[0], bq[1], bk[0], bk[1]], 1))
    m["wv0"] = np.ascontiguousarray(np.concatenate([u(512 + 64 * ga), u(1408 + 64 * bh[0]), u(1408 + 64 * bh[1])], 1))
    wm = inp["e_w_mem_kv"][0]
    m["wmk0"] = np.ascontiguousarray(np.concatenate([wm[:, 64 * j:64 * j + 64]] * 2, 1))
    m["wmv0"] = np.ascontiguousarray(wm[:, 256 + 64 * j:256 + 64 * j + 64])
    g2 = lambda a, bb: np.concatenate([a, bb])[:, None]
    m["gcol0"] = np.ascontiguousarray(np.concatenate([
        g2(qk[0] * SCALE, qk[0] * SCALE), g2(qk[0] * SCALE, qk[4] * SCALE), g2(qk[1], qk[1]),
        g2(qk[2] * SCALE, qk[2] * SCALE), g2(qk[3], qk[3]), g2(qk[5], qk[5])], 1).astype(np.float32))
    m["gx0"] = _col(inp["e_norm"][0])
    m["gm0"] = _col(inp["e_mem_norm"][0])
    ycols, valid = [], []
    seen = set()
    for si in range(6):
        for r in range(4):
            gr = r % 2
            hs = [("A", 3 * gr + i) for i in range(3)] + [("B", (2 * r) % 6), ("B", (2 * r + 1) % 6), ("M", r)]
            kind, h = hs[si]
            yc = {"A": 64 * h, "B": 384 + 64 * h, "M": 768 + 64 * h}[kind]
            ycols.append(yc)
            valid.append((kind, h) not in seen)
            seen.add((kind, h))
    wz = np.concatenate([w[:, 2048 + yc:2048 + yc + 64] for yc in ycols], 1)
    wo_full = inp["e_w_out"][0]
    wo = np.concatenate([wo_full[yc:yc + 64] if v else np.zeros((64, D), np.float32) for yc, v in zip(ycols, valid)], 0)
    m["wz0"] = np.ascontiguousarray(wz)
    m["wo0"] = np.ascontiguousarray(wo)
    kk = np.arange(128)[:, None]
    qq = np.arange(128)[None, :]
    mA = np.zeros((128, 3, 2, 128), np.float32)
    for i, h in enumerate(ah):
        dist_prev = qq - kk + 128
        dist_cur = qq - kk
        mA[:, i, 0, :] = np.where((dist_prev >= 0) & (dist_prev < 128), -sl[h] * dist_prev, NEGB)
        mA[:, i, 1, :] = np.where((dist_cur >= 0) & (dist_cur < 128), -sl[h] * dist_cur, NEGB)
    m["maskA"] = mA
    m["sinkb"] = np.ascontiguousarray(np.broadcast_to(inp["e_sinks"][0][ah][None, :], (128, 3)).astype(np.float32))
    kpos = (128 * np.arange(64)[None, :] + np.arange(128)[:, None]).astype(np.float32)
    m["rq0"] = np.ascontiguousarray(np.stack([-sl[6 + h] * kpos for h in bh], 1).astype(np.float32))
    m["kb0"] = np.ascontiguousarray(np.stack([sl[6 + h] * kpos for h in bh], 1).astype(np.float32))
    w = inp["o_w_in"][0]
    qk = inp["o_qk_norm"][0]
    ch = [3 * j + i for i in range(3)]
    u = lambda c0: w[:, c0:c0 + 64]
    cq = [u(64 * h) for h in ch]
    ck = [u(768 + 64 * h) for h in ch]
    mq = u(2316 + 64 * j)
    m["wt1"] = np.ascontiguousarray(np.concatenate([cq[0], cq[1], cq[2], mq, ck[0], ck[1], ck[2], ck[2]], 1))
    m["wv1"] = np.ascontiguousarray(np.concatenate([u(1536 + 64 * h) for h in ch] + [w[:, 2304 + h:2304 + h + 1] for h in ch], 1))
    wm = inp["o_w_mem_kv"][0]
    m["wmk1"] = np.ascontiguousarray(np.concatenate([wm[:, 64 * j:64 * j + 64]] * 2, 1))
    m["wmv1"] = np.ascontiguousarray(wm[:, 256 + 64 * j:256 + 64 * j + 64])
    m["gcol1"] = np.ascontiguousarray(np.concatenate([
        g2(qk[0] * SCALE, qk[0] * SCALE), g2(qk[0] * SCALE, qk[2] * SCALE), g2(qk[1], qk[1]), g2(qk[1], qk[1]),
        g2(qk[3], qk[3])], 1).astype(np.float32))
    m["gx1"] = _col(inp["o_norm"][0])
    m["gm1"] = _col(inp["o_mem_norm"][0])
    ycols = []
    for si in range(4):
        for r in range(4):
            ycols.append((64 * (3 * r + si)) if si < 3 else (768 + 64 * r))
    m["wz1"] = np.ascontiguousarray(np.concatenate([w[:, 2572 + yc:2572 + yc + 64] for yc in ycols], 1))
    wo_full = inp["o_w_out"][0]
    m["wo1"] = np.ascontiguousarray(np.concatenate([wo_full[yc:yc + 64] for yc in ycols], 0))
    m["bfb"] = np.ascontiguousarray(np.broadcast_to(inp["o_b_f"][0][ch][None, :], (128, 3)).astype(np.float32))
    return m


_NC_CACHE = {}


def kernel(**inputs):
    inp = {k: np.asarray(v, dtype=np.float32) for k, v in inputs.items()}
    if "nc" not in _NC_CACHE:
        _NC_CACHE["nc"] = build()
    nc = _NC_CACHE["nc"]
    cst = _consts()
    in_maps = []
    for c in range(8):
        m = _core_inputs(inp, c // 4, c % 4)
        m.update(cst)
        in_maps.append(m)
    res = run_bass_kernel_spmd(nc, in_maps, core_ids=list(range(8)))
    out = np.empty((2, S, D), np.float32)
    for c in range(8):
        out[c // 4, (c % 4) * 2048:(c % 4 + 1) * 2048, :] = np.asarray(res.results[c]["out"]).T
    return out
```

```python
from contextlib import ExitStack
import numpy as np
import ml_dtypes
import concourse.bass as bass
import concourse.mybir as mybir
from concourse.bass_utils import run_bass_kernel_spmd

F32 = mybir.dt.float32
BF16 = mybir.dt.bfloat16
AF = mybir.ActivationFunctionType
ALU = mybir.AluOpType

S = 8192
D = 1024
HD = 64
NG = 16
EPS = 1e-6
SCALE = HD ** -0.5
NEGB = -30000.0
N_ALIBI = 12


class Op:
    __slots__ = ("eng", "fn", "reads", "writes", "kind", "grp", "ticket", "deps", "need_inc", "idx")


class Prog:
    def __init__(self, nc):
        self.nc = nc
        self.ops = []
        self.last_w = {}
        self.readers = {}
        self.bar = None

    def add(self, eng, fn, reads=(), writes=(), kind="c", grp=None):
        op = Op()
        op.eng, op.fn, op.kind, op.grp = eng, fn, kind, grp
        op.reads, op.writes = tuple(reads), tuple(writes)
        op.ticket, op.need_inc = None, False
        op.idx = len(self.ops)
        deps = set()
        for r in op.reads:
            w = self.last_w.get(r)
            if w is not None:
                deps.add(w)
        for w_ in op.writes:
            w = self.last_w.get(w_)
            if w is not None:
                deps.add(w)
            deps.update(self.readers.get(w_, ()))
        if self.bar is not None:
            deps.add(self.bar)
        deps.discard(op.idx)
        op.deps = sorted(deps)
        for r in op.reads:
            self.readers.setdefault(r, []).append(op.idx)
        for w_ in op.writes:
            self.last_w[w_] = op.idx
            self.readers[w_] = []
        self.ops.append(op)
        return op

    def pe(self, fn, reads=(), writes=()):
        return self.add("pe", fn, reads, writes)

    def act(self, fn, reads=(), writes=()):
        return self.add("act", fn, reads, writes)

    def dve(self, fn, reads=(), writes=()):
        return self.add("dve", fn, reads, writes)

    def pool(self, fn, reads=(), writes=()):
        return self.add("pool", fn, reads, writes)

    def dma(self, fn, reads=(), writes=(), q="sp", grp="d0"):
        return self.add(q, fn, reads, writes, kind="d", grp=grp)

    def barrier(self):
        op = self.add("sp", lambda e: e.nop(), (), ())
        last = {}
        for o in self.ops[:-1]:
            if o.kind == "c":
                last[("e", o.eng)] = o.idx
            elif o.kind == "d":
                last[(o.kind, o.grp, o.idx)] = o.idx
        op.deps = sorted(set(last.values()))
        self.bar = op.idx

    def emit(self, stack):
        nc = self.nc
        ops = self.ops

        def skip(dop, op):
            return dop.kind == "c" and dop.eng == "pe" and op.eng == "pe" and op.kind == "c"

        for op in ops:
            if op.kind != "c":
                op.need_inc = True
            for d in op.deps:
                if not skip(ops[d], op):
                    ops[d].need_inc = True
        cnt = {}
        sems = {}
        NSLOT = {"sp": 24, "pool": 6, "act": 4, "cc": 8}
        rr = {}
        for op in ops:
            if not op.need_inc:
                continue
            if op.kind == "c":
                key, inc = ("e", op.eng), 1
            elif op.kind == "d":
                i = rr.get(op.eng, 0)
                rr[op.eng] = i + 1
                key, inc = ("d", op.eng, i % NSLOT[op.eng]), 16
            else:
                i = rr.get("cc", 0)
                rr["cc"] = i + 1
                key, inc = ("cc", i % NSLOT["cc"]), 1
            cnt[key] = cnt.get(key, 0) + inc
            op.ticket = (key, cnt[key])
            if key not in sems:
                sems[key] = stack.enter_context(nc.semaphore("s_" + "_".join(str(k) for k in key)))
        final = dict(cnt)
        engobj = {"pe": nc.tensor, "act": nc.scalar, "dve": nc.vector, "pool": nc.gpsimd, "sp": nc.sync}
        block = stack.enter_context(nc.Block())

        def stream(ename):
            def body(_e):
                e = engobj[ename]
                seen = {}
                for op in ops:
                    if op.eng != ename:
                        continue
                    need = {}
                    for d in op.deps:
                        dop = ops[d]
                        if dop.ticket is None or skip(dop, op):
                            continue
                        k, v = dop.ticket
                        if v > need.get(k, 0):
                            need[k] = v
                    for k, v in need.items():
                        if seen.get(k, 0) >= v:
                            continue
                        e.wait_ge(sems[k], v)
                        seen[k] = v
                    if op.kind != "c" and op.ticket is not None:
                        k, v = op.ticket
                        prev = v - (16 if op.kind == "d" else 1)
                        if prev > 0 and seen.get(k, 0) < prev:
                            e.wait_ge(sems[k], prev)
                            seen[k] = prev
                    ins = op.fn(e)
                    if op.ticket is not None:
                        k, v = op.ticket
                        ins.then_inc(sems[k], 16 if op.kind == "d" else 1)
                if ename == "sp":
                    for k, v in final.items():
                        if k[0] in ("d", "cc"):
                            e.wait_ge(sems[k], v)
            return body

        for en, deco in (("pe", block.tensor), ("act", block.scalar), ("dve", block.vector),
                         ("pool", block.gpsimd), ("sp", block.sync)):
            deco(stream(en))


def layer_cfg(L):
    if L == 0:
        return dict(NT=5, NV=192, NSLOT=6, NBIG=2, NZ=1536, KR=96, mq_tile=1, nvh=3, nf=0)
    return dict(NT=4, NV=195, NSLOT=4, NBIG=3, NZ=1024, KR=65, mq_tile=1, nvh=3, nf=3)


def alibi_slopes():
    h = np.arange(1, N_ALIBI + 1, dtype=np.float32)
    return (2.0 ** (-8.0 * h / N_ALIBI)).astype(np.float32)


def build(n_layers=2, debug=False, stop=None):
    nc = bass.Bass("TRN2", target_bir_lowering=False)
    P = Prog(nc)
    AX = mybir.AxisListType.X

    def din(name, shape, dt=F32):
        return nc.dram_tensor(name, list(shape), dt, kind="ExternalInput").ap()

    xT = din("xT", [D, S])
    memT = din("memT", [D, 256])
    out = nc.dram_tensor("out", [D, 2048], F32, kind="ExternalOutput").ap()
    cst_bf = din("cst_bf", [128, 4, 128], BF16)
    cst_f = din("cst_f", [128, 2, 128])
    kaug = din("kaug", [32, S], BF16)
    pastb = din("pastb", [128, 32, 32])
    ownz = din("ownz", [128, 32, 32])
    LW = []
    for L in range(2):
        c = layer_cfg(L)
        LW.append(dict(
            wt=din(f"wt{L}", [D, c["NT"] * 128]), wv=din(f"wv{L}", [D, c["NV"]]),
            wmk=din(f"wmk{L}", [D, 128]), wmv=din(f"wmv{L}", [D, 64]),
            wz=din(f"wz{L}", [D, c["NZ"]]), wo=din(f"wo{L}", [c["NZ"], D]),
            gx=din(f"gx{L}", [128, 8]), gm=din(f"gm{L}", [128, 8]),
            gcol=din(f"gcol{L}", [128, c["NT"] + 1]),
        ))
    maskA = din("maskA", [128, 3, 2, 128])
    sinkb = din("sinkb", [128, 3])
    rq0 = din("rq0", [128, 2, 64])
    kb0 = din("kb0", [128, 2, 64])
    bfb = din("bfb", [128, 3])
    QTs = nc.dram_tensor("QTs", [3, 97, S], BF16).ap()
    KTs = nc.dram_tensor("KTs", [3, 64, S], BF16).ap()
    VAs = nc.dram_tensor("VAs", [3, 128, 64, 128], BF16).ap()
    YT = [[nc.dram_tensor(f"YT{L}_{i}", [64, S], BF16).ap() for i in range(layer_cfg(L)["NSLOT"])] for L in range(2)]
    YG = [[nc.dram_tensor(f"YG{L}_{i}", [256, S], BF16).ap() for i in range(layer_cfg(L)["NSLOT"])] for L in range(2)]
    X1in = [[nc.dram_tensor(f"X1in{g}_{h}", [512, 512], F32).ap() for h in range(2)] for g in range(4)]
    X1out = [[nc.dram_tensor(f"X1out{g}_{h}", [2048, 512], F32).ap() for h in range(2)] for g in range(4)]
    if debug:
        dbg_y = [nc.dram_tensor("dbg_y0", [384, S], BF16, kind="ExternalOutput").ap(),
                 nc.dram_tensor("dbg_y1", [256, S], BF16, kind="ExternalOutput").ap()]
        dbg_x1 = nc.dram_tensor("dbg_x1", [D, 2048], F32, kind="ExternalOutput").ap()

    with ExitStack() as st:
        def sb(name, shape, dt=F32):
            return st.enter_context(nc.sbuf_tensor(name, list(shape), dt))

        dyn_cache = {}
        pb = [st.enter_context(nc.psum_tensor(f"pb{i}", [128, 512], F32)) for i in range(7)]
        pbT = st.enter_context(nc.psum_tensor("pbT", [128, 1024], BF16))
        rot = {}

        def bank(role, banks):
            i = rot.get(role, 0)
            rot[role] = i + 1
            b = banks[i % len(banks)]
            return pb[b], ("pb", b)

        def MM(out_, lhsT, rhs, start, stop, r, w):
            P.pe(lambda e: e.matmul(out_, lhsT=lhsT, rhs=rhs, start=start, stop=stop), r, w)

        def TR(out_, in_, idn, r, w):
            P.pe(lambda e: e.transpose(out=out_, in_=in_, identity=idn), r, w)

        def ACTV(out_, in_, func, r, w, bias=None, scale=None):
            kw = {}
            if bias is not None:
                kw["bias"] = bias
            if scale is not None:
                kw["scale"] = scale
            P.act(lambda e: e.activation(out=out_, in_=in_, func=func, **kw), r, w)

        def TT(eng, out_, in0, in1, op, r, w):
            P.add(eng, lambda e: e.tensor_tensor(out=out_, in0=in0, in1=in1, op=op), r, w)

        def STT(out_, in0, scalar, in1, op0, op1, r, w):
            P.dve(lambda e: e.scalar_tensor_tensor(out=out_, in0=in0, scalar=scalar, in1=in1, op0=op0, op1=op1), r, w)

        def TS(out_, in0, s1, op0, r, w):
            P.dve(lambda e: e.tensor_scalar(out=out_, in0=in0, scalar1=s1, scalar2=None, op0=op0), r, w)

        def CP(eng, out_, in_, r, w):
            P.add(eng, lambda e: e.tensor_copy(out=out_, in_=in_), r, w)

        def RECIP(out_, in_, r, w):
            P.dve(lambda e: e.reciprocal(out=out_, in_=in_), r, w)

        def MAX8(out_, in_, r, w):
            P.dve(lambda e: e.max(out=out_, in_=in_), r, w)

        def RED(out_, in_, r, w):
            P.dve(lambda e: e.tensor_reduce(out=out_, in_=in_, axis=AX, op=ALU.add), r, w)

        def MEMSET(ap, val, w):
            P.pool(lambda e: e.memset(ap, val), (), w)

        def AG(src, dst, r, w):
            P.add("pool", lambda e: e.collective_compute("AllGather", ALU.bypass, replica_groups=[[0, 1, 2, 3], [4, 5, 6, 7]],
                                                        ins=[src.opt()], outs=[dst.opt()]), r, w, kind="cc", grp="g")

        def DMA(out_, in_, r, w, q="sp", grp="ld", force=False):
            op = P.dma(lambda e: e.dma_start(out=out_, in_=in_), r, w, q=q, grp=grp)
            if force:
                op.need_inc = True
            return op

        cbf = sb("cbf", [128, 4, 128], BF16)
        cf = sb("cf", [128, 2, 128])
        ident, ones, onesbd, tri = cbf[:, 0, :], cbf[:, 1, :], cbf[:, 2, :], cbf[:, 3, :]
        Umat, E127 = cf[:, 0, :], cf[:, 1, :]
        ARENA = sb("arena", [128, 24576], BF16)
        Kg = ARENA[:, 0:8192]
        Vg = ARENA[:, 8192:16384].rearrange("p (g c) -> p g c", c=128)
        WTb = sb("WTb", [128, 8, 640], BF16)
        Wvb = sb("Wvb", [128, 8, 195], BF16)
        WMKb = sb("WMKb", [128, 8, 128], BF16)
        WMVb = sb("WMVb", [128, 8, 64], BF16)
        gx = sb("gx", [128, 8]); gmm = sb("gmm", [128, 8]); gcol = sb("gcol", [128, 6])
        xs = [sb(f"xs{i}", [128, 8, 512]) for i in range(2)]
        hT = [sb(f"hT{i}", [128, 8, 512], BF16) for i in range(2)]
        sq = sb("sq", [128, 8, 512], BF16)
        lnv = [sb(f"lnv{i}", [128, 512]) for i in range(2)]
        rs = [sb(f"rs{i}", [128, 512]) for i in range(2)]
        sq1 = [sb(f"sq1{i}", [128, 512], BF16) for i in range(2)]
        stg = [sb(f"stg{i}", [128, 512], BF16) for i in range(5)]
        Vstg = [sb(f"Vstg{i}", [128, 3, 4, 128], BF16) for i in range(2)]
        mkT = sb("mkT", [128, 256], BF16)
        mva = sb("mva", [128, 2, 128], BF16)
        Pm = sb("Pm", [128, 2, 512], BF16)
        Pt = [sb(f"Pt{i}", [128, 512], BF16) for i in range(4)]
        Qg = [sb(f"Qg{i}", [128, 512], BF16) for i in range(3)]
        rec = [sb(f"rec{i}", [128, 512]) for i in range(2)]
        ystg = [sb(f"ystg{i}", [128, 512], BF16) for i in range(4)]
        kbt = sb("kbt", [128, 3, 64])
        pastb_s = sb("pastb_s", [128, 32, 32])
        ownz_s = sb("ownz_s", [128, 32, 32])
        rq0_s = sb("rq0_s", [128, 2, 64])
        maskA_s = sb("maskA_s", [128, 3, 2, 128]); sink_s = sb("sink_s", [128, 3]); esink = sb("esink", [128, 3])
        KAr = sb("KAr", [128, 5, 128], BF16)
        VAr = sb("VAr", [128, 5, 128], BF16)
        kmT = sb("kmT", [128, 32], BF16)
        ksum = sb("ksum", [128, 32])
        gmt = sb("gmt", [128, 4, 32]); top8 = sb("top8", [128, 4, 8]); thr = sb("thr", [128, 4, 1])
        selx = sb("selx", [128, 4, 32])
        selm = sb("selm", [128, 4, 32], BF16)
        selT = sb("selT", [32, 512], BF16)
        swS = sb("swS", [128, 2, 128]); swP = sb("swP", [128, 2, 128], BF16)
        bfb_s = sb("bfb_s", [128, 3])
        lall = sb("lall", [128, 64, 3]); nloc = sb("nloc", [128, 64, 3]); scA = sb("scA", [128, 64, 3]); scB = sb("scB", [128, 64, 3])
        nbf = sb("nbf", [128, 64, 3], BF16)
        fb = sb("fb", [128, 3]); fe = sb("fe", [128, 3])
        rstg = sb("rstg", [3, 512], BF16)
        yg = sb("yg", [128, 12, 512], BF16)
        ez = [sb(f"ez{i}", [128, 512], BF16) for i in range(3)]
        WZ = ARENA[:, 0:12288]
        WO = ARENA[:, 12288:24576]

        DMA(cbf[:], cst_bf, [], ["cbf"])
        DMA(cf[:], cst_f, [], ["cf"])
        DMA(pastb_s[:], pastb, [], ["pastb_s"])
        DMA(ownz_s[:], ownz, [], ["ownz_s"])
        DMA(rq0_s[:], rq0, [], ["rq0_s"])
        DMA(maskA_s[:], maskA, [], ["maskA_s"])
        DMA(sink_s[:], sinkb, [], ["sink_s"])
        DMA(bfb_s[:], bfb, [], ["bfb_s"])
        ACTV(esink[:], sink_s[:], AF.Exp, ["sink_s"], ["esink"])

        def rmsnorm(xsrc, xkey, hdst, hkey, gains, n):
            ACTV(sq[:, :, 0:n], xsrc, AF.Square, [xkey], ["sq"])
            ps, pk = bank("ss", [2])
            for t in range(8):
                MM(ps[:, 0:n], ones, sq[:, t, 0:n], t == 0, t == 7, ["sq", "cbf"], [pk])
            ACTV(lnv[0][:, 0:n], ps[:, 0:n], AF.Ln, [pk], ["lnv0"], bias=EPS, scale=1.0 / D)
            ACTV(rs[0][:, 0:n], lnv[0][:, 0:n], AF.Exp, ["lnv0"], ["rs0"], scale=-0.5)
            for t in range(8):
                STT(hdst[:, t, :], xsrc[:, t, :], gains[:, t:t + 1], rs[0][:, 0:n], ALU.mult, ALU.mult,
                    [xkey, "rs0", "gains"], [hkey])

        def head_rms(ps, pk, n, gc, dst, dkey, i):
            ACTV(sq1[i][:, 0:n], ps[:, 0:n], AF.Square, [pk], [f"sq1{i}"])
            p2, p2k = bank("ss", [2])
            MM(p2[:, 0:n], onesbd, sq1[i][:, 0:n], True, True, [f"sq1{i}", "cbf"], [p2k])
            ACTV(lnv[i][:, 0:n], p2[:, 0:n], AF.Ln, [p2k], [f"lnv{i}"], bias=EPS, scale=1.0 / HD)
            ACTV(rs[i][:, 0:n], lnv[i][:, 0:n], AF.Exp, [f"lnv{i}"], [f"rs{i}"], scale=-0.5)
            STT(dst, ps[:, 0:n], gc, rs[i][:, 0:n], ALU.mult, ALU.mult, [pk, f"rs{i}", "gains"], [dkey])

        def finalize(ops_, opk, dst, dkey, ri, extra=None):
            r = rec[ri]
            rk = f"rec{ri}"
            if extra is None:
                RECIP(r[64:128, :], ops_[64:128, :], [opk], [rk])
            else:
                TS(r[64:128, :], ops_[64:128, :], extra, ALU.add, [opk, "esink"], [rk])
                RECIP(r[64:128, :], r[64:128, :], [rk], [rk])
            TT("dve", dst, ops_[0:64, :], r[64:128, :], ALU.mult, [opk, rk], [dkey])

        def layer(L):
            c = layer_cfg(L)
            W = LW[L]
            NT, NV, KR, NBIG, NSLOT = c["NT"], c["NV"], c["KR"], c["NBIG"], c["NSLOT"]
            xT_v = xT.rearrange("(t p) n -> p t n", p=128)
            out_v = out.rearrange("(t p) n -> p t n", p=128)
            last = (L == n_layers - 1)

            def load_x_full(xb, xk, tg):
                if L == 0:
                    DMA(xb[:], xT_v[:, :, tg * 512:(tg + 1) * 512], [], [xk])
                else:
                    r, g = tg // 4, tg % 4
                    for h in range(2):
                        DMA(xb[:, 4 * h:4 * h + 4, :], X1out[g][h].rearrange("(r t p) n -> r p t n", r=4, t=4)[r],
                            [("X1out", g, h)], [xk])

            def DMAdyn(out_, src_v, base, r, w):
                def f(e):
                    if "j2048" not in dyn_cache:
                        dyn_cache["j2048"] = e.snap((e.partition_id() % 4) * 2048)
                    return e.dma_start(out=out_, in_=src_v[:, :, bass.ds(dyn_cache["j2048"] + base, 512)])
                P.dma(f, r, w, q="sp", grp="ld")
            P.barrier()
            wv3 = lambda a: a.rearrange("(t p) c -> p t c", p=128)
            DMA(WTb[:, :, 0:NT * 128], wv3(W["wt"]), [], ["WTb"], q="pool", grp="w")
            DMA(Wvb[:, :, 0:NV], wv3(W["wv"]), [], ["Wvb"], q="pool", grp="w")
            DMA(WMKb[:], wv3(W["wmk"]), [], ["WMKb"], q="pool", grp="w")
            DMA(WMVb[:], wv3(W["wmv"]), [], ["WMVb"], q="pool", grp="w")
            DMA(gx[:], W["gx"], [], ["gains"])
            DMA(gmm[:], W["gm"], [], ["gains"])
            DMA(gcol[:, 0:NT + 1], W["gcol"], [], ["gains"])
            mx = xs[1][:, :, 0:256]
            DMA(mx, memT.rearrange("(t p) n -> p t n", p=128), [], ["xs1"])
            mh = hT[1][:, :, 0:256]
            rmsnorm(mx, "xs1", mh, "hT1", gmm, 256)
            ps, pk = bank("pa", [0, 1])
            for t in range(8):
                MM(ps[:, 0:256], WMKb[:, t, :], mh[:, t, :], t == 0, t == 7, ["WMKb", "hT1"], [pk])
            head_rms(ps, pk, 256, gcol[:, NT:NT + 1], mkT[:], "mkT", 0)
            MEMSET(mva[:, :, 64:128], 1.0, ["mva1"])
            for mt in range(2):
                ps, pk = bank("pv", [3])
                for t in range(8):
                    MM(ps[:, 0:64], mh[:, t, mt * 128:(mt + 1) * 128], WMVb[:, t, :], t == 0, t == 7, ["WMVb", "hT1"], [pk])
                ACTV(mva[:, mt, 0:64], ps[:, 0:64], AF.Copy, [pk], ["mva0"])
            if L == 0:
                DMA(kbt[:, 0:2, :], kb0, [], ["kbt"])
                MEMSET(ksum[:], 0.0, ["ksum"])
                MEMSET(kmT[:], 0.0, ["kmT"])
                MEMSET(VAr[:, :, 64:128], 1.0, ["VAr1"])
            for i in range(2):
                MEMSET(Vstg[i][:, :, :, 64:128], 1.0, [f"Vstg{i}"])

            tg_order = list(range(NG)) if L == 0 else [r * 4 + g for g in range(4) for r in range(4)]
            for tg in tg_order:
                c0 = tg * 512
                xb, hb = xs[tg % 2], hT[tg % 2]
                xk, hk = f"xs{tg % 2}", f"hT{tg % 2}"
                load_x_full(xb, xk, tg)
                rmsnorm(xb[:], xk, hb, hk, gx, 512)
                for ti in range(NT):
                    ps, pk = bank("pa", [0, 1])
                    for t in range(8):
                        MM(ps[:], WTb[:, t, ti * 128:(ti + 1) * 128], hb[:, t, :], t == 0, t == 7, ["WTb", hk], [pk])
                    head_rms(ps, pk, 512, gcol[:, ti:ti + 1], stg[ti][:], f"stg{ti}", ti % 2)
                vs = Vstg[tg % 2]
                vsk = f"Vstg{tg % 2}"
                for tt in range(4):
                    G = tg * 4 + tt
                    ps, pk = bank("pv", [3])
                    for t in range(8):
                        MM(ps[:, 0:NV], hb[:, t, tt * 128:(tt + 1) * 128], Wvb[:, t, 0:NV], t == 0, t == 7, ["Wvb", hk], [pk])
                    ACTV(vs[:, :, tt, 0:64], ps[:, 0:192].rearrange("p (h c) -> p h c", c=64), AF.Copy, [pk], [vsk])
                    if L == 0:
                        CP("pool", VAr[:, tt + 1, 0:64], vs[:, 0, tt, 0:64], [vsk], ["VAr0"])
                    else:
                        TT("dve", fb[:], ps[:, 192:195], bfb_s[:], ALU.add, [pk, "bfb_s"], ["fb"])
                        ACTV(fe[:], fb[:], AF.Exp, ["fb"], ["fe"], scale=-1.0)
                        ACTV(lall[:, G, :], fe[:], AF.Ln, ["fe"], ["lall"], bias=1.0)
                if L == 0:
                    DMA(VAs[0:2, :, tg * 4:tg * 4 + 4, :].rearrange("h p g c -> p h g c"), vs[:, 1:3, :, :],
                        [vsk], [("VAs", 0), ("VAs", 1)], grp="st")
                    for u in range(2):
                        DMA(QTs[u][0:64, c0:c0 + 512], stg[3][u * 64:(u + 1) * 64, :], ["stg3"], [("QTs", u)], grp="st")
                        DMA(KTs[u][:, c0:c0 + 512], stg[4][u * 64:(u + 1) * 64, :], ["stg4"], [("KTs", u)], grp="st")
                else:
                    DMA(VAs[0:3, :, tg * 4:tg * 4 + 4, :].rearrange("h p g c -> p h g c"), vs[:, 0:3, :, :],
                        [vsk], [("VAs", 0), ("VAs", 1), ("VAs", 2)], grp="st")
                    for u in range(3):
                        qsrc = stg[0][u * 64:(u + 1) * 64, :] if u < 2 else stg[1][0:64, :]
                        ksrc = stg[2][u * 64:(u + 1) * 64, :] if u < 2 else stg[3][0:64, :]
                        DMA(QTs[u][0:64, c0:c0 + 512], qsrc, ["stg0" if u < 2 else "stg1"], [("QTs", u)], grp="st")
                        DMA(KTs[u][:, c0:c0 + 512], ksrc, ["stg2" if u < 2 else "stg3"], [("KTs", u)], grp="st")
                mslot = NSLOT - 1
                for mt in range(2):
                    ps, pk = bank("ms", [4, 5])
                    MM(ps[:], mkT[64:128, mt * 128:(mt + 1) * 128], stg[1][64:128, :], True, True, ["mkT", "stg1"], [pk])
                    ACTV(Pm[:, mt, :], ps[:], AF.Exp, [pk], ["Pm"])
                po, pok = bank("o", [6])
                for mt in range(2):
                    MM(po[:], mva[:, mt, :], Pm[:, mt, :], mt == 0, mt == 1, ["Pm", "mva0", "mva1"], [pok])
                ys = ystg[tg % 2]
                ysk = f"ystg{tg % 2}"
                finalize(po, pok, ys[0:64, :], ysk, 0)
                DMA(YT[L][mslot][:, c0:c0 + 512], ys[0:64, :], [ysk], [("YT", L, mslot)], grp="st")

                if L == 0:
                    RED(ksum[:, 2 * tg:2 * tg + 2], stg[4][:].rearrange("p (b t) -> p b t", t=256), ["stg4"], ["ksum"])
                    CP("dve", kmT[:, 2 * tg:2 * tg + 2], ksum[:, 2 * tg:2 * tg + 2], ["ksum"], ["kmT"])
                    for u in range(2):
                        gp, gpk = bank("g", [3])
                        for qt in range(4):
                            MM(gp[:, qt * 32:(qt + 1) * 32], stg[3][u * 64:(u + 1) * 64, qt * 128:(qt + 1) * 128],
                               kmT[u * 64:(u + 1) * 64, :], True, True, ["stg3", "kmT"], [gpk])
                        TT("dve", gmt[:].rearrange("p (b s) n -> p b s n", s=2),
                           gp[:, 0:128].rearrange("p (b s n) -> p b s n", b=2, s=2),
                           pastb_s[:, 2 * tg:2 * tg + 2, :].unsqueeze(2).to_broadcast([128, 2, 2, 32]), ALU.add,
                           [gpk, "pastb_s"], ["gmt"])
                        for qt in range(4):
                            MAX8(top8[:, qt, :], gmt[:, qt, :], ["gmt"], ["top8"])
                        TS(thr[:], top8[:, :, 2:3], -1e29, ALU.max, ["top8"], ["thr"])
                        TT("dve", selx[:], gmt[:], thr[:].to_broadcast([128, 4, 32]), ALU.is_lt, ["gmt", "thr"], ["selx"])
                        TT("dve", selm[:, :, 1:32].rearrange("p (b s) n -> p b s n", s=2),
                           selx[:, :, 0:31].rearrange("p (b s) n -> p b s n", s=2),
                           ownz_s[:, 2 * tg:2 * tg + 2, 0:31].unsqueeze(2).to_broadcast([128, 2, 2, 31]), ALU.mult,
                           ["selx", "ownz_s"], ["selm"])
                        CP("dve", selm[:, :, 0:1], rq0_s[:, u, 4 * tg:4 * tg + 4].unsqueeze(2), ["rq0_s"], ["selm"])
                        for qt in range(4):
                            TR(pbT[0:32, qt * 128:(qt + 1) * 128], selm[:, qt, :], ident, ["selm", "cbf"], ["pbT"])
                        ACTV(selT[:], pbT[0:32, 0:512], AF.Copy, ["pbT"], ["selT"])
                        DMA(QTs[u][64:96, c0:c0 + 512], selT[:], ["selT"], [("QTs", u)], grp="st")
                    CP("pool", KAr[:, 1:5, :], stg[2][:].rearrange("p (t c) -> p t c", c=128), ["stg2"], ["KArc"])
                    for hh in range(3):
                        half = hh % 2 if hh < 2 else 0
                        qsrc = stg[0] if hh < 2 else stg[1]
                        qkey = "stg0" if hh < 2 else "stg1"
                        b0 = half * 64
                        yb = ystg[2 + (hh % 2)]
                        ybk = f"ystg{2 + (hh % 2)}"
                        po, pok = bank("o", [6])
                        for qt in range(4):
                            first = (tg == 0 and qt == 0)
                            kts = [1] if first else [0, 1]
                            k0 = kts[0]
                            sp_, spk = bank("ms", [4, 5])
                            for kk in kts:
                                MM(sp_[:, kk * 128:(kk + 1) * 128], KAr[b0:b0 + 64, qt + kk, :], qsrc[b0:b0 + 64, qt * 128:(qt + 1) * 128],
                                   True, True, ["KArc", "KArp", qkey], [spk])
                            TT("dve", swS[:, k0:2, :], sp_[:, k0 * 128:256].rearrange("p (k q) -> p k q", q=128),
                               maskA_s[:, hh, k0:2, :], ALU.add, [spk, "maskA_s"], ["swS"])
                            ACTV(swP[:, k0:2, :], swS[:, k0:2, :], AF.Exp, ["swS"], ["swP"])
                            for kk in kts:
                                MM(po[:, qt * 128:(qt + 1) * 128], VAr[:, qt + kk, :], swP[:, kk, :], kk == k0, kk == 1,
                                   ["swP", "VAr0", "VAr1", "VArp"], [pok])
                        finalize(po, pok, yb[0:64, :], ybk, 1, extra=esink[64:128, hh:hh + 1])
                        DMA(YT[0][hh][:, c0:c0 + 512], yb[0:64, :], [ybk], [("YT", 0, hh)], grp="st")
                    CP("pool", KAr[:, 0, :], KAr[:, 4, :], ["KArc"], ["KArp"])
                    CP("pool", VAr[:, 0, 0:64], VAr[:, 4, 0:64], ["VAr0"], ["VArp"])

            if stop == "A0" and L == 0:
                return
            if L == 1:
                fl = lambda a: a.rearrange("p g h -> p (g h)")
                ps, pk = bank("pa", [0, 1])
                MM(ps[:, 0:192], Umat, fl(lall[:]), True, True, ["cf", "lall"], [pk])
                ACTV(fl(nloc[:]), ps[:, 0:192], AF.Copy, [pk], ["nloc"])
                ps2, pk2 = bank("pa", [0, 1])
                MM(ps2[:, 0:192], E127, fl(nloc[:]), True, True, ["cf", "nloc"], [pk2])
                ACTV(fl(scA[:]), ps2[:, 0:192], AF.Copy, [pk2], ["scA"])
                a_, b_, ak_, bk_ = scA, scB, "scA", "scB"
                for d in (1, 2, 4, 8, 16, 32):
                    TT("dve", b_[:, d:64, :], a_[:, d:64, :], a_[:, 0:64 - d, :], ALU.add, [ak_], [bk_])
                    CP("dve", b_[:, 0:d, :], a_[:, 0:d, :], [ak_], [bk_])
                    a_, b_, ak_, bk_ = b_, a_, bk_, ak_
                TT("dve", kbt[:, :, 1:64].rearrange("p h g -> p g h"), nloc[:, 1:64, :], a_[:, 0:63, :], ALU.add, [ak_, "nloc"], ["kbt"])
                CP("dve", kbt[:, :, 0:1].rearrange("p h g -> p g h"), nloc[:, 0:1, :], ["nloc"], ["kbt"])
                CP("dve", nbf[:], kbt[:].rearrange("p h g -> p g h"), ["kbt"], ["nbf"])
                for ch in range(16):
                    ps, pk = bank("pa", [0, 1])
                    for tt in range(4):
                        G = ch * 4 + tt
                        MM(ps[0:3, tt * 128:(tt + 1) * 128], nbf[:, G, :], ident, True, True, ["nbf", "cbf"], [pk])
                    ACTV(rstg[:], ps[0:3, :], AF.Copy, [pk], ["rstg"], scale=-1.0)
                    DMA(QTs[0:3, 64, ch * 512:(ch + 1) * 512], rstg[:], ["rstg"], [("QTs", 0), ("QTs", 1), ("QTs", 2)], grp="st")

            if stop == "A":
                return

            def gather_slot(sl):
                if debug:
                    DMA(dbg_y[L][sl * 64:(sl + 1) * 64, :], YT[L][sl], [("YT", L, sl)], [("dbgy", L, sl)], grp="so", force=True)
                AG(YT[L][sl], YG[L][sl], [("YT", L, sl)], [("YG", L, sl)])

            for sl in ([0, 1, 2, 5] if L == 0 else [3]):
                gather_slot(sl)
            P.barrier()
            DMA(Kg[64:96, :], kaug, [], ["Kaug"])
            for s_ in range(NBIG):
                DMA(Kg[0:64, :], KTs[s_], [("KTs", s_)], ["Kg"])
                DMA(Vg, VAs[s_], [("VAs", s_)], ["Vg"])
                yslot = 3 + s_ if L == 0 else s_
                for qg in range(NG):
                    c0 = qg * 512
                    qb = Qg[qg % 3]
                    qk_ = f"Qg{qg % 3}"
                    DMA(qb[0:KR, :], QTs[s_][0:KR, c0:c0 + 512], [("QTs", s_)], [qk_], grp="lq")
                    po, pok = bank("O", [3, 4])
                    nkt = 4 * qg + 4
                    pend = []
                    for G in range(nkt + 2):
                        if G < nkt:
                            kt = G - 4 * qg
                            n0 = 128 * kt if kt > 0 else 0
                            sp_, spk = bank("S", [0, 1, 2])
                            MM(sp_[:, n0:512], Kg[0:KR, G * 128:(G + 1) * 128], qb[0:KR, n0:512], True, kt < 0,
                               ["Kg", "Kaug", qk_], [spk])
                            if kt >= 0:
                                MM(sp_[:, n0:n0 + 128], ident, tri, False, True, ["cbf"], [spk])
                            pend.append((G, n0, sp_, spk))
                        if G >= 2:
                            G2, n2, sp2, spk2 = pend.pop(0)
                            pt = Pt[G2 % 4]
                            ptk = f"Pt{G2 % 4}"
                            ACTV(pt[:, n2:512], sp2[:, n2:512], AF.Exp, [spk2, "kbt"], [ptk], bias=kbt[:, s_, G2:G2 + 1])
                            MM(po[:, n2:512], Vg[:, G2, :], pt[:, n2:512], G2 == 0, G2 == nkt - 1, [ptk, "Vg"], [pok])
                    ys = ystg[qg % 2]
                    ysk = f"ystg{qg % 2}"
                    finalize(po, pok, ys[0:64, :], ysk, qg % 2)
                    DMA(YT[L][yslot][:, c0:c0 + 512], ys[0:64, :], [ysk], [("YT", L, yslot)], grp="st")
                gather_slot(yslot)

            if stop in ("B", "G"):
                return
            P.barrier()
            NZ = c["NZ"]
            NCT = NZ // 128
            WZv = WZ[:, 0:8 * NZ].rearrange("p (t c) -> p t c", t=8)
            WOv = WO[:, 0:NCT * 1024].rearrange("p (t c) -> p t c", t=NCT)
            DMA(WZv, wv3(W["wz"]), [], ["WZ"], q="pool", grp="w")
            DMA(WOv, wv3(W["wo"]), [], ["WO"], q="pool", grp="w")
            for tgq in range(4):
                c0 = tgq * 512
                xb, hb = xs[tgq % 2], hT[tgq % 2]
                xk, hk = f"xs{tgq % 2}", f"hT{tgq % 2}"
                if L == 0:
                    DMAdyn(xb[:], xT_v, c0, [], [xk])
                else:
                    for h in range(2):
                        DMA(xb[:, 4 * h:4 * h + 4, :], X1in[tgq][h].rearrange("(t p) n -> p t n", p=128), [("X1in", tgq, h)], [xk])
                for sl in range(NSLOT):
                    DMAdyn(yg[:, 2 * sl:2 * sl + 2, :], YG[L][sl].rearrange("(t p) n -> p t n", p=128), c0, [("YG", L, sl)],
                           [("ygl", sl), ("ygc", 2 * sl), ("ygc", 2 * sl + 1)])
                rmsnorm(xb[:], xk, hb, hk, gx, 512)
                for ct in range(NCT):
                    ps, pk = bank("pa", [0, 1])
                    for t in range(8):
                        MM(ps[:], WZv[:, t, ct * 128:(ct + 1) * 128], hb[:, t, :], t == 0, t == 7, ["WZ", hk], [pk])
                    ezb = ez[ct % 3]
                    ezk = f"ez{ct % 3}"
                    ACTV(ezb[:], ps[:], AF.Silu, [pk], [ezk])
                    TT("pool" if ct % 2 else "dve", yg[:, ct, :], yg[:, ct, :], ezb[:], ALU.mult, [("ygl", ct // 2), ezk], [("ygc", ct)])
                for oc in range(8):
                    ps, pk = bank("po", [3, 4])
                    for ct in range(NCT):
                        MM(ps[:], WOv[:, ct, oc * 128:(oc + 1) * 128], yg[:, ct, :], ct == 0, ct == NCT - 1, ["WO", ("ygc", ct)], [pk])
                    TT("dve", xb[:, oc, :], ps[:], xb[:, oc, :], ALU.add, [pk, xk], [xk])
                if last:
                    DMA(out_v[:, :, c0:c0 + 512], xb[:], [xk], [("out", tgq)], grp="so")
                else:
                    for h in range(2):
                        DMA(X1in[tgq][h].rearrange("(t p) n -> p t n", p=128), xb[:, 4 * h:4 * h + 4, :], [xk], [("X1in", tgq, h)], grp="so")
                        AG(X1in[tgq][h], X1out[tgq][h], [("X1in", tgq, h)], [("X1out", tgq, h)])
                if debug and L == 0:
                    DMA(dbg_x1.rearrange("(t p) n -> p t n", p=128)[:, :, c0:c0 + 512], xb[:], [xk], [("dbgx", tgq)], grp="so")

        for L in range(n_layers):
            layer(L)
        P.emit(st)
    return nc


def _bf(a):
    return np.ascontiguousarray(a).astype(ml_dtypes.bfloat16)


def _consts():
    idn = np.eye(128, dtype=np.float32)
    ones = np.ones((128, 128), np.float32)
    bd = np.zeros((128, 128), np.float32)
    bd[:64, :64] = 1
    bd[64:, 64:] = 1
    k = np.arange(128)[:, None]
    q = np.arange(128)[None, :]
    tri = np.where(k > q, NEGB, 0.0).astype(np.float32)
    cst_bf = _bf(np.stack([idn, ones, bd, tri], 1))
    U = (k <= q).astype(np.float32)
    E = np.zeros((128, 128), np.float32)
    E[127, :] = 1
    cst_f = np.ascontiguousarray(np.stack([U, E], 1))
    kaug = np.zeros((32, S), np.float32)
    kaug[0] = 1
    for n in range(31):
        kaug[1 + n, n * 256:(n + 1) * 256] = NEGB
    pb = np.where(np.arange(32)[None, :] >= np.arange(32)[:, None], -1e30, 0.0).astype(np.float32)
    pastb = np.ascontiguousarray(np.broadcast_to(pb[None], (128, 32, 32)))
    oz = (np.arange(32)[None, :] != np.arange(32)[:, None]).astype(np.float32)
    ownz = np.ascontiguousarray(np.broadcast_to(oz[None], (128, 32, 32)))
    return dict(cst_bf=cst_bf, cst_f=cst_f, kaug=_bf(kaug), pastb=pastb, ownz=ownz)


def _col(v):
    return np.ascontiguousarray(v.reshape(8, 128).T)


def _core_inputs(inp, b, j):
    sl = alibi_slopes()
    m = {}
    m["xT"] = np.ascontiguousarray(inp["x"][b].T)
    m["memT"] = np.ascontiguousarray(inp["mem"][b].T)
    w = inp["e_w_in"][0]
    qk = inp["e_qk_norm"][0]
    ga = j % 2
    ah = [3 * ga + i for i in range(3)]
    bh = [(2 * j) % 6, (2 * j + 1) % 6]
    u = lambda c0: w[:, c0:c0 + 64]
    aq = [u(64 * h) for h in ah]
    ak = u(384 + 64 * ga)
    bq = [u(640 + 64 * h) for h in bh]
    bk = [u(1024 + 64 * h) for h in bh]
    mq = u(1792 + 64 * j)
    m["wt0"] = np.ascontiguousarray(np.concatenate([aq[0], aq[1], aq[2], mq, ak, ak, bq[0], bq[1], bk[0], bk[1]], 1))
    m["wv0"] = np.ascontiguousarray(np.concatenate([u(512 + 64 * ga), u(1408 + 64 * bh[0]), u(1408 + 64 * bh[1])], 1))
    wm = inp["e_w_mem_kv"][0]
    m["wmk0"] = np.ascontiguousarray(np.concatenate([wm[:, 64 * j:64 * j + 64]] * 2, 1))
    m["wmv0"] = np.ascontiguousarray(wm[:, 256 + 64 * j:256 + 64 * j + 64])
    g2 = lambda a, bb: np.concatenate([a, bb])[:, None]
    m["gcol0"] = np.ascontiguousarray(np.concatenate([
        g2(qk[0] * SCALE, qk[0] * SCALE), g2(qk[0] * SCALE, qk[4] * SCALE), g2(qk[1], qk[1]),
        g2(qk[2] * SCALE, qk[2] * SCALE), g2(qk[3], qk[3]), g2(qk[5], qk[5])], 1).astype(np.float32))
    m["gx0"] = _col(inp["e_norm"][0])
    m["gm0"] = _col(inp["e_mem_norm"][0])
    ycols, valid = [], []
    seen = set()
    for si in range(6):
        for r in range(4):
            gr = r % 2
            hs = [("A", 3 * gr + i) for i in range(3)] + [("B", (2 * r) % 6), ("B", (2 * r + 1) % 6), ("M", r)]
            kind, h = hs[si]
            yc = {"A": 64 * h, "B": 384 + 64 * h, "M": 768 + 64 * h}[kind]
            ycols.append(yc)
            valid.append((kind, h) not in seen)
            seen.add((kind, h))
    wz = np.concatenate([w[:, 2048 + yc:2048 + yc + 64] for yc in ycols], 1)
    wo_full = inp["e_w_out"][0]
    wo = np.concatenate([wo_full[yc:yc + 64] if v else np.zeros((64, D), np.float32) for yc, v in zip(ycols, valid)], 0)
    m["wz0"] = np.ascontiguousarray(wz)
    m["wo0"] = np.ascontiguousarray(wo)
    kk = np.arange(128)[:, None]
    qq = np.arange(128)[None, :]
    mA = np.zeros((128, 3, 2, 128), np.float32)
    for i, h in enumerate(ah):
        dist_prev = qq - kk + 128
        dist_cur = qq - kk
        mA[:, i, 0, :] = np.where((dist_prev >= 0) & (dist_prev < 128), -sl[h] * dist_prev, NEGB)
        mA[:, i, 1, :] = np.where((dist_cur >= 0) & (dist_cur < 128), -sl[h] * dist_cur, NEGB)
    m["maskA"] = mA
    m["sinkb"] = np.ascontiguousarray(np.broadcast_to(inp["e_sinks"][0][ah][None, :], (128, 3)).astype(np.float32))
    kpos = (128 * np.arange(64)[None, :] + np.arange(128)[:, None]).astype(np.float32)
    m["rq0"] = np.ascontiguousarray(np.stack([-sl[6 + h] * kpos for h in bh], 1).astype(np.float32))
    m["kb0"] = np.ascontiguousarray(np.stack([sl[6 + h] * kpos for h in bh], 1).astype(np.float32))
    w = inp["o_w_in"][0]
    qk = inp["o_qk_norm"][0]
    ch = [3 * j + i for i in range(3)]
    u = lambda c0: w[:, c0:c0 + 64]
    cq = [u(64 * h) for h in ch]
    ck = [u(768 + 64 * h) for h in ch]
    mq = u(2316 + 64 * j)
    m["wt1"] = np.ascontiguousarray(np.concatenate([cq[0], cq[1], cq[2], mq, ck[0], ck[1], ck[2], ck[2]], 1))
    m["wv1"] = np.ascontiguousarray(np.concatenate([u(1536 + 64 * h) for h in ch] + [w[:, 2304 + h:2304 + h + 1] for h in ch], 1))
    wm = inp["o_w_mem_kv"][0]
    m["wmk1"] = np.ascontiguousarray(np.concatenate([wm[:, 64 * j:64 * j + 64]] * 2, 1))
    m["wmv1"] = np.ascontiguousarray(wm[:, 256 + 64 * j:256 + 64 * j + 64])
    m["gcol1"] = np.ascontiguousarray(np.concatenate([
        g2(qk[0] * SCALE, qk[0] * SCALE), g2(qk[0] * SCALE, qk[2] * SCALE), g2(qk[1], qk[1]), g2(qk[1], qk[1]),
        g2(qk[3], qk[3])], 1).astype(np.float32))
    m["gx1"] = _col(inp["o_norm"][0])
    m["gm1"] = _col(inp["o_mem_norm"][0])
    ycols = []
    for si in range(4):
        for r in range(4):
            ycols.append((64 * (3 * r + si)) if si < 3 else (768 + 64 * r))
    m["wz1"] = np.ascontiguousarray(np.concatenate([w[:, 2572 + yc:2572 + yc + 64] for yc in ycols], 1))
    wo_full = inp["o_w_out"][0]
    m["wo1"] = np.ascontiguousarray(np.concatenate([wo_full[yc:yc + 64] for yc in ycols], 0))
    m["bfb"] = np.ascontiguousarray(np.broadcast_to(inp["o_b_f"][0][ch][None, :], (128, 3)).astype(np.float32))
    return m


_NC_CACHE = {}


def kernel(**inputs):
    inp = {k: np.asarray(v, dtype=np.float32) for k, v in inputs.items()}
    if "nc" not in _NC_CACHE:
        _NC_CACHE["nc"] = build()
    nc = _NC_CACHE["nc"]
    cst = _consts()
    in_maps = []
    for c in range(8):
        m = _core_inputs(inp, c // 4, c % 4)
        m.update(cst)
        in_maps.append(m)
    res = run_bass_kernel_spmd(nc, in_maps, core_ids=list(range(8)))
    out = np.empty((2, S, D), np.float32)
    for c in range(8):
        out[c // 4, (c % 4) * 2048:(c % 4 + 1) * 2048, :] = np.asarray(res.results[c]["out"]).T
    return out
```

```python
from contextlib import ExitStack
import numpy as np
import ml_dtypes
import concourse.bass as bass
import concourse.mybir as mybir
from concourse.bass_utils import run_bass_kernel_spmd

F32 = mybir.dt.float32
BF16 = mybir.dt.bfloat16
AF = mybir.ActivationFunctionType
ALU = mybir.AluOpType

S = 8192
D = 1024
HD = 64
NG = 16
EPS = 1e-6
SCALE = HD ** -0.5
NEGB = -30000.0
N_ALIBI = 12


class Op:
    __slots__ = ("eng", "fn", "reads", "writes", "kind", "grp", "ticket", "deps", "need_inc", "idx")


class Prog:
    def __init__(self, nc):
        self.nc = nc
        self.ops = []
        self.last_w = {}
        self.readers = {}
        self.bar = None

    def add(self, eng, fn, reads=(), writes=(), kind="c", grp=None):
        op = Op()
        op.eng, op.fn, op.kind, op.grp = eng, fn, kind, grp
        op.reads, op.writes = tuple(reads), tuple(writes)
        op.ticket, op.need_inc = None, False
        op.idx = len(self.ops)
        deps = set()
        for r in op.reads:
            w = self.last_w.get(r)
            if w is not None:
                deps.add(w)
        for w_ in op.writes:
            w = self.last_w.get(w_)
            if w is not None:
                deps.add(w)
            deps.update(self.readers.get(w_, ()))
        if self.bar is not None:
            deps.add(self.bar)
        deps.discard(op.idx)
        op.deps = sorted(deps)
        for r in op.reads:
            self.readers.setdefault(r, []).append(op.idx)
        for w_ in op.writes:
            self.last_w[w_] = op.idx
            self.readers[w_] = []
        self.ops.append(op)
        return op

    def pe(self, fn, reads=(), writes=()):
        return self.add("pe", fn, reads, writes)

    def act(self, fn, reads=(), writes=()):
        return self.add("act", fn, reads, writes)

    def dve(self, fn, reads=(), writes=()):
        return self.add("dve", fn, reads, writes)

    def pool(self, fn, reads=(), writes=()):
        return self.add("pool", fn, reads, writes)

    def dma(self, fn, reads=(), writes=(), q="sp", grp="d0"):
        return self.add(q, fn, reads, writes, kind="d", grp=grp)

    def barrier(self):
        op = self.add("sp", lambda e: e.nop(), (), ())
        last = {}
        for o in self.ops[:-1]:
            if o.kind == "c":
                last[("e", o.eng)] = o.idx
            elif o.kind == "d":
                last[(o.kind, o.grp, o.idx)] = o.idx
        op.deps = sorted(set(last.values()))
        self.bar = op.idx

    def emit(self, stack):
        nc = self.nc
        ops = self.ops

        def skip(dop, op):
            return dop.kind == "c" and dop.eng == "pe" and op.eng == "pe" and op.kind == "c"

        for op in ops:
            if op.kind != "c":
                op.need_inc = True
            for d in op.deps:
                if not skip(ops[d], op):
                    ops[d].need_inc = True
        cnt = {}
        sems = {}
        NSLOT = {"sp": 24, "pool": 6, "act": 4, "cc": 8}
        rr = {}
        for op in ops:
            if not op.need_inc:
                continue
            if op.kind == "c":
                key, inc = ("e", op.eng), 1
            elif op.kind == "d":
                i = rr.get(op.eng, 0)
                rr[op.eng] = i + 1
                key, inc = ("d", op.eng, i % NSLOT[op.eng]), 16
            else:
                i = rr.get("cc", 0)
                rr["cc"] = i + 1
                key, inc = ("cc", i % NSLOT["cc"]), 1
            cnt[key] = cnt.get(key, 0) + inc
            op.ticket = (key, cnt[key])
            if key not in sems:
                sems[key] = stack.enter_context(nc.semaphore("s_" + "_".join(str(k) for k in key)))
        final = dict(cnt)
        engobj = {"pe": nc.tensor, "act": nc.scalar, "dve": nc.vector, "pool": nc.gpsimd, "sp": nc.sync}
        block = stack.enter_context(nc.Block())

        def stream(ename):
            def body(_e):
                e = engobj[ename]
                seen = {}
                for op in ops:
                    if op.eng != ename:
                        continue
                    need = {}
                    for d in op.deps:
                        dop = ops[d]
                        if dop.ticket is None or skip(dop, op):
                            continue
                        k, v = dop.ticket
                        if v > need.get(k, 0):
                            need[k] = v
                    for k, v in need.items():
                        if seen.get(k, 0) >= v:
                            continue
                        e.wait_ge(sems[k], v)
                        seen[k] = v
                    if op.kind != "c" and op.ticket is not None:
                        k, v = op.ticket
                        prev = v - (16 if op.kind == "d" else 1)
                        if prev > 0 and seen.get(k, 0) < prev:
                            e.wait_ge(sems[k], prev)
                            seen[k] = prev
                    ins = op.fn(e)
                    if op.ticket is not None:
                        k, v = op.ticket
                        ins.then_inc(sems[k], 16 if op.kind == "d" else 1)
                if ename == "sp":
                    for k, v in final.items():
                        if k[0] in ("d", "cc"):
                            e.wait_ge(sems[k], v)
            return body

        for en, deco in (("pe", block.tensor), ("act", block.scalar), ("dve", block.vector),
                         ("pool", block.gpsimd), ("sp", block.sync)):
            deco(stream(en))


def layer_cfg(L):
    if L == 0:
        return dict(NT=5, NV=192, NSLOT=6, NBIG=2, NZ=1536, KR=96, mq_tile=1, nvh=3, nf=0)
    return dict(NT=4, NV=195, NSLOT=4, NBIG=3, NZ=1024, KR=65, mq_tile=1, nvh=3, nf=3)


def alibi_slopes():
    h = np.arange(1, N_ALIBI + 1, dtype=np.float32)
    return (2.0 ** (-8.0 * h / N_ALIBI)).astype(np.float32)


def build(n_layers=2, debug=False, stop=None):
    nc = bass.Bass("TRN2", target_bir_lowering=False)
    P = Prog(nc)
    AX = mybir.AxisListType.X

    def din(name, shape, dt=F32):
        return nc.dram_tensor(name, list(shape), dt, kind="ExternalInput").ap()

    xT = din("xT", [D, S])
    memT = din("memT", [D, 256])
    out = nc.dram_tensor("out", [D, 2048], F32, kind="ExternalOutput").ap()
    cst_bf = din("cst_bf", [128, 4, 128], BF16)
    cst_f = din("cst_f", [128, 2, 128])
    kaug = din("kaug", [32, S], BF16)
    pastb = din("pastb", [128, 32, 32], BF16)
    ownz = din("ownz", [128, 32, 32], BF16)
    LW = []
    for L in range(2):
        c = layer_cfg(L)
        LW.append(dict(
            wt=din(f"wt{L}", [D, c["NT"] * 128]), wv=din(f"wv{L}", [D, c["NV"]]),
            wmk=din(f"wmk{L}", [D, 128]), wmv=din(f"wmv{L}", [D, 64]),
            wz=din(f"wz{L}", [D, c["NZ"]]), wo=din(f"wo{L}", [c["NZ"], D]),
            gx=din(f"gx{L}", [128, 8]), gm=din(f"gm{L}", [128, 8]),
            gcol=din(f"gcol{L}", [128, c["NT"] + 1]),
        ))
    maskA = din("maskA", [128, 3, 2, 128])
    sinkb = din("sinkb", [128, 3])
    rq0 = din("rq0", [128, 2, 64])
    kb0 = din("kb0", [128, 2, 64])
    bfb = din("bfb", [128, 3])
    QTs = nc.dram_tensor("QTs", [3, 97, S], BF16).ap()
    KTs = nc.dram_tensor("KTs", [3, 64, S], BF16).ap()
    VAs = nc.dram_tensor("VAs", [3, 128, 64, 128], BF16).ap()
    YT = [[nc.dram_tensor(f"YT{L}_{i}", [64, S], BF16).ap() for i in range(layer_cfg(L)["NSLOT"])] for L in range(2)]
    YG = [[nc.dram_tensor(f"YG{L}_{i}", [256, S], BF16).ap() for i in range(layer_cfg(L)["NSLOT"])] for L in range(2)]
    X1in = [[nc.dram_tensor(f"X1in{g}_{h}", [512, 512], F32).ap() for h in range(2)] for g in range(4)]
    H1in = [nc.dram_tensor(f"H1in{g}", [1024, 512], BF16).ap() for g in range(4)]
    H1out = [nc.dram_tensor(f"H1out{g}", [4096, 512], BF16).ap() for g in range(4)]
    if debug:
        dbg_y = [nc.dram_tensor("dbg_y0", [384, S], BF16, kind="ExternalOutput").ap(),
                 nc.dram_tensor("dbg_y1", [256, S], BF16, kind="ExternalOutput").ap()]
        dbg_x1 = nc.dram_tensor("dbg_x1", [D, 2048], F32, kind="ExternalOutput").ap()

    with ExitStack() as st:
        def sb(name, shape, dt=F32):
            return st.enter_context(nc.sbuf_tensor(name, list(shape), dt))

        dyn_cache = {}
        pb = [st.enter_context(nc.psum_tensor(f"pb{i}", [128, 512], F32)) for i in range(7)]
        pbT = st.enter_context(nc.psum_tensor("pbT", [128, 1024], BF16))
        rot = {}

        def bank(role, banks):
            i = rot.get(role, 0)
            rot[role] = i + 1
            b = banks[i % len(banks)]
            return pb[b], ("pb", b)

        def MM(out_, lhsT, rhs, start, stop, r, w):
            P.pe(lambda e: e.matmul(out_, lhsT=lhsT, rhs=rhs, start=start, stop=stop), r, w)

        def TR(out_, in_, idn, r, w):
            P.pe(lambda e: e.transpose(out=out_, in_=in_, identity=idn), r, w)

        def ACTV(out_, in_, func, r, w, bias=None, scale=None):
            kw = {}
            if bias is not None:
                kw["bias"] = bias
            if scale is not None:
                kw["scale"] = scale
            P.act(lambda e: e.activation(out=out_, in_=in_, func=func, **kw), r, w)

        def TT(eng, out_, in0, in1, op, r, w):
            P.add(eng, lambda e: e.tensor_tensor(out=out_, in0=in0, in1=in1, op=op), r, w)

        def STT(out_, in0, scalar, in1, op0, op1, r, w):
            P.dve(lambda e: e.scalar_tensor_tensor(out=out_, in0=in0, scalar=scalar, in1=in1, op0=op0, op1=op1), r, w)

        def TS(out_, in0, s1, op0, r, w):
            P.dve(lambda e: e.tensor_scalar(out=out_, in0=in0, scalar1=s1, scalar2=None, op0=op0), r, w)

        def CP(eng, out_, in_, r, w):
            P.add(eng, lambda e: e.tensor_copy(out=out_, in_=in_), r, w)

        def RECIP(out_, in_, r, w):
            P.dve(lambda e: e.reciprocal(out=out_, in_=in_), r, w)

        def MAX8(out_, in_, r, w):
            P.dve(lambda e: e.max(out=out_, in_=in_), r, w)

        def RED(out_, in_, r, w):
            P.dve(lambda e: e.tensor_reduce(out=out_, in_=in_, axis=AX, op=ALU.add), r, w)

        def MEMSET(ap, val, w):
            P.pool(lambda e: e.memset(ap, val), (), w)

        def AG(src, dst, r, w):
            P.add("pool", lambda e: e.collective_compute("AllGather", ALU.bypass, replica_groups=[[0, 1, 2, 3], [4, 5, 6, 7]],
                                                        ins=[src.opt()], outs=[dst.opt()]), r, w, kind="cc", grp="g")

        def DMA(out_, in_, r, w, q="sp", grp="ld", force=False):
            op = P.dma(lambda e: e.dma_start(out=out_, in_=in_), r, w, q=q, grp=grp)
            if force:
                op.need_inc = True
            return op

        cbf = sb("cbf", [128, 4, 128], BF16)
        cf = sb("cf", [128, 2, 128])
        ident, ones, onesbd, tri = cbf[:, 0, :], cbf[:, 1, :], cbf[:, 2, :], cbf[:, 3, :]
        Umat, E127 = cf[:, 0, :], cf[:, 1, :]
        ARENA = sb("arena", [128, 24576], BF16)
        Kg = ARENA[:, 0:8192]
        Vg = ARENA[:, 8192:16384].rearrange("p (g c) -> p g c", c=128)
        WTb = sb("WTb", [128, 8, 640], BF16)
        Wvb = sb("Wvb", [128, 8, 195], BF16)
        WMKb = sb("WMKb", [128, 8, 128], BF16)
        WMVb = sb("WMVb", [128, 8, 64], BF16)
        gx = sb("gx", [128, 8]); gmm = sb("gmm", [128, 8]); gcol = sb("gcol", [128, 6]); gxn = sb("gxn", [128, 8])
        xs = [sb(f"xs{i}", [128, 8, 512]) for i in range(2)]
        hT = [sb(f"hT{i}", [128, 8, 512], BF16) for i in range(2)]
        sq = sb("sq", [128, 8, 512], BF16)
        lnv = [sb(f"lnv{i}", [128, 512]) for i in range(2)]
        rs = [sb(f"rs{i}", [128, 512]) for i in range(2)]
        sq1 = [sb(f"sq1{i}", [128, 512], BF16) for i in range(2)]
        stg = [[sb(f"stg{i}_{p}", [128, 512], BF16) for p in range(2)] for i in range(5)]
        Vstg = [sb(f"Vstg{i}", [128, 3, 4, 128], BF16) for i in range(2)]
        mkT = sb("mkT", [128, 256], BF16)
        mva = sb("mva", [128, 2, 128], BF16)
        Pm = sb("Pm", [128, 2, 512], BF16)
        Pt = [sb(f"Pt{i}", [128, 512], BF16) for i in range(4)]
        Qg = [sb(f"Qg{i}", [128, 512], BF16) for i in range(3)]
        rec = [sb(f"rec{i}", [128, 512]) for i in range(2)]
        ystg = [sb(f"ystg{i}", [128, 512], BF16) for i in range(4)]
        kbt = sb("kbt", [128, 3, 64])
        pastb_s = sb("pastb_s", [128, 32, 32], BF16)
        ownz_s = sb("ownz_s", [128, 32, 32], BF16)
        rq0_s = sb("rq0_s", [128, 2, 64])
        maskA_s = sb("maskA_s", [128, 3, 2, 128]); sink_s = sb("sink_s", [128, 3]); esink = sb("esink", [128, 3])
        KAr = sb("KAr", [128, 5, 128], BF16)
        VAr = sb("VAr", [128, 5, 128], BF16)
        kmT = sb("kmT", [128, 32], BF16)
        ksum = sb("ksum", [128, 32])
        gmt = sb("gmt", [128, 4, 32]); top8 = sb("top8", [128, 4, 8]); thr = sb("thr", [128, 4, 1])
        selx = sb("selx", [128, 4, 32])
        selm2 = [sb(f"selm{i}", [128, 4, 32], BF16) for i in range(2)]
        selT = sb("selT", [32, 512], BF16)
        swS = [sb(f"swS{i}", [128, 4, 128]) for i in range(2)]
        swP = [sb(f"swP{i}", [128, 4, 128], BF16) for i in range(2)]
        bfb_s = sb("bfb_s", [128, 3])
        lall = sb("lall", [128, 64, 3]); nloc = sb("nloc", [128, 64, 3]); scA = sb("scA", [128, 64, 3]); scB = sb("scB", [128, 64, 3])
        nbf = sb("nbf", [128, 64, 3], BF16)
        fb = sb("fb", [128, 3]); fe = sb("fe", [128, 3])
        rstg = sb("rstg", [3, 512], BF16)
        yg = sb("yg", [128, 12, 512], BF16)
        ez = [sb(f"ez{i}", [128, 512], BF16) for i in range(3)]
        WZ = ARENA[:, 0:12288]
        WO = ARENA[:, 12288:24576]

        DMA(cbf[:], cst_bf, [], ["cbf"])
        DMA(cf[:], cst_f, [], ["cf"])
        DMA(pastb_s[:], pastb, [], ["pastb_s"])
        DMA(ownz_s[:], ownz, [], ["ownz_s"])
        DMA(rq0_s[:], rq0, [], ["rq0_s"])
        DMA(maskA_s[:], maskA, [], ["maskA_s"])
        DMA(sink_s[:], sinkb, [], ["sink_s"])
        DMA(bfb_s[:], bfb, [], ["bfb_s"])
        ACTV(esink[:], sink_s[:], AF.Exp, ["sink_s"], ["esink"])

        def rmsnorm(xsrc, xkey, hdst, hkey, gains, n):
            ACTV(sq[:, :, 0:n], xsrc, AF.Square, [xkey], ["sq"])
            ps, pk = bank("ss", [2])
            for t in range(8):
                MM(ps[:, 0:n], ones, sq[:, t, 0:n], t == 0, t == 7, ["sq", "cbf"], [pk])
            ACTV(lnv[0][:, 0:n], ps[:, 0:n], AF.Ln, [pk], ["lnv0"], bias=EPS, scale=1.0 / D)
            ACTV(rs[0][:, 0:n], lnv[0][:, 0:n], AF.Exp, ["lnv0"], ["rs0"], scale=-0.5)
            for t in range(8):
                STT(hdst[:, t, :], xsrc[:, t, :], gains[:, t:t + 1], rs[0][:, 0:n], ALU.mult, ALU.mult,
                    [xkey, "rs0", "gains"], [hkey])

        def head_rms(ps, pk, n, gc, dst, dkey, i):
            ACTV(sq1[i][:, 0:n], ps[:, 0:n], AF.Square, [pk], [f"sq1{i}"])
            p2, p2k = bank("ss", [2])
            MM(p2[:, 0:n], onesbd, sq1[i][:, 0:n], True, True, [f"sq1{i}", "cbf"], [p2k])
            ACTV(lnv[i][:, 0:n], p2[:, 0:n], AF.Ln, [p2k], [f"lnv{i}"], bias=EPS, scale=1.0 / HD)
            ACTV(rs[i][:, 0:n], lnv[i][:, 0:n], AF.Exp, [f"lnv{i}"], [f"rs{i}"], scale=-0.5)
            STT(dst, ps[:, 0:n], gc, rs[i][:, 0:n], ALU.mult, ALU.mult, [pk, f"rs{i}", "gains"], [dkey])

        def finalize(ops_, opk, dst, dkey, ri, extra=None):
            r = rec[ri]
            rk = f"rec{ri}"
            if extra is None:
                RECIP(r[64:128, :], ops_[64:128, :], [opk], [rk])
            else:
                TS(r[64:128, :], ops_[64:128, :], extra, ALU.add, [opk, "esink"], [rk])
                RECIP(r[64:128, :], r[64:128, :], [rk], [rk])
            TT("dve", dst, ops_[0:64, :], r[64:128, :], ALU.mult, [opk, rk], [dkey])

        def layer(L):
            c = layer_cfg(L)
            W = LW[L]
            NT, NV, KR, NBIG, NSLOT = c["NT"], c["NV"], c["KR"], c["NBIG"], c["NSLOT"]
            xT_v = xT.rearrange("(t p) n -> p t n", p=128)
            out_v = out.rearrange("(t p) n -> p t n", p=128)
            last = (L == n_layers - 1)

            def DMAdyn(out_, src_v, base, r, w):
                def f(e):
                    if "j2048" not in dyn_cache:
                        dyn_cache["j2048"] = e.snap((e.partition_id() % 4) * 2048)
                    return e.dma_start(out=out_, in_=src_v[:, :, bass.ds(dyn_cache["j2048"] + base, 512)])
                P.dma(f, r, w, q="sp", grp="ld")
            P.barrier()
            wv3 = lambda a: a.rearrange("(t p) c -> p t c", p=128)
            DMA(WTb[:, :, 0:NT * 128], wv3(W["wt"]), [], ["WTb"], q="pool", grp="w")
            DMA(Wvb[:, :, 0:NV], wv3(W["wv"]), [], ["Wvb"], q="pool", grp="w")
            DMA(WMKb[:], wv3(W["wmk"]), [], ["WMKb"], q="pool", grp="w")
            DMA(WMVb[:], wv3(W["wmv"]), [], ["WMVb"], q="pool", grp="w")
            DMA(gx[:], W["gx"], [], ["gains"])
            if L == 0:
                DMA(gxn[:], LW[1]["gx"], [], ["gains"])
            DMA(gmm[:], W["gm"], [], ["gains"])
            DMA(gcol[:, 0:NT + 1], W["gcol"], [], ["gains"])
            mx = xs[1][:, :, 0:256]
            DMA(mx, memT.rearrange("(t p) n -> p t n", p=128), [], ["xs1"])
            mh = hT[1][:, :, 0:256]
            rmsnorm(mx, "xs1", mh, "hT1", gmm, 256)
            ps, pk = bank("pa", [0, 1])
            for t in range(8):
                MM(ps[:, 0:256], WMKb[:, t, :], mh[:, t, :], t == 0, t == 7, ["WMKb", "hT1"], [pk])
            head_rms(ps, pk, 256, gcol[:, NT:NT + 1], mkT[:], "mkT", 0)
            MEMSET(mva[:, :, 64:128], 1.0, ["mva1"])
            for mt in range(2):
                ps, pk = bank("pv", [3])
                for t in range(8):
                    MM(ps[:, 0:64], mh[:, t, mt * 128:(mt + 1) * 128], WMVb[:, t, :], t == 0, t == 7, ["WMVb", "hT1"], [pk])
                ACTV(mva[:, mt, 0:64], ps[:, 0:64], AF.Copy, [pk], ["mva0"])
            if L == 0:
                DMA(kbt[:, 0:2, :], kb0, [], ["kbt"])
                MEMSET(ksum[:], 0.0, ["ksum"])
                MEMSET(kmT[:], 0.0, ["kmT"])
                MEMSET(VAr[:, 1:5, 64:128], 1.0, ["VAr1"])
                MEMSET(VAr[:, 0, :], 0.0, ["VArp"])
                MEMSET(KAr[:, 0, :], 0.0, ["KArp"])
            for i in range(2):
                MEMSET(Vstg[i][:, :, :, 64:128], 1.0, [f"Vstg{i}"])

            tg_order = list(range(NG)) if L == 0 else [r * 4 + g for g in range(4) for r in range(4)]

            def stage1(tg, par):
                units = []
                c0 = tg * 512
                xb, hb = xs[par], hT[par]
                xk, hk = f"xs{par}", f"hT{par}"

                def u_load():
                    if L == 0:
                        DMA(xb[:], xT_v[:, :, c0:c0 + 512], [], [xk])
                        rmsnorm(xb[:], xk, hb, hk, gx, 512)
                    else:
                        r, g = tg // 4, tg % 4
                        DMA(hb[:], H1out[g].rearrange("(r t p) n -> r p t n", r=4, t=8)[r], [("H1out", g)], [hk])
                units.append(u_load)
                for ti in range(NT):
                    def u_tile(ti=ti):
                        ps, pk = bank("pa", [0, 1])
                        for t in range(8):
                            MM(ps[:], WTb[:, t, ti * 128:(ti + 1) * 128], hb[:, t, :], t == 0, t == 7, ["WTb", hk], [pk])
                        head_rms(ps, pk, 512, gcol[:, ti:ti + 1], stg[ti][par][:], f"stg{ti}_{par}", ti % 2)
                    units.append(u_tile)
                vs = Vstg[par]
                vsk = f"Vstg{par}"
                for tt in range(4):
                    def u_v(tt=tt):
                        G = tg * 4 + tt
                        ps, pk = bank("pv", [3])
                        for t in range(8):
                            MM(ps[:, 0:NV], hb[:, t, tt * 128:(tt + 1) * 128], Wvb[:, t, 0:NV], t == 0, t == 7, ["Wvb", hk], [pk])
                        ACTV(vs[:, :, tt, 0:64], ps[:, 0:192].rearrange("p (h c) -> p h c", c=64), AF.Copy, [pk], [vsk])
                        if L == 1:
                            TT("dve", fb[:], ps[:, 192:195], bfb_s[:], ALU.add, [pk, "bfb_s"], ["fb"])
                            ACTV(fe[:], fb[:], AF.Exp, ["fb"], ["fe"], scale=-1.0)
                            ACTV(lall[:, G, :], fe[:], AF.Ln, ["fe"], ["lall"], bias=1.0)
                    units.append(u_v)
                return units

            def stage2(tg, par):
                fr, bk = [], []
                c0 = tg * 512
                vs = Vstg[par]
                vsk = f"Vstg{par}"
                S_ = lambda ti: stg[ti][par]
                K_ = lambda ti: f"stg{ti}_{par}"

                def u_stores():
                    if L == 0:
                        DMA(VAs[0:2, :, tg * 4:tg * 4 + 4, :].rearrange("h p g c -> p h g c"), vs[:, 1:3, :, :],
                            [vsk], [("VAs", 0), ("VAs", 1)], grp="st")
                        for u in range(2):
                            DMA(QTs[u][0:64, c0:c0 + 512], S_(3)[u * 64:(u + 1) * 64, :], [K_(3)], [("QTs", u)], grp="st")
                            DMA(KTs[u][:, c0:c0 + 512], S_(4)[u * 64:(u + 1) * 64, :], [K_(4)], [("KTs", u)], grp="st")
                    else:
                        DMA(VAs[0:3, :, tg * 4:tg * 4 + 4, :].rearrange("h p g c -> p h g c"), vs[:, 0:3, :, :],
                            [vsk], [("VAs", 0), ("VAs", 1), ("VAs", 2)], grp="st")
                        for u in range(3):
                            qsrc = S_(0)[u * 64:(u + 1) * 64, :] if u < 2 else S_(1)[0:64, :]
                            ksrc = S_(2)[u * 64:(u + 1) * 64, :] if u < 2 else S_(3)[0:64, :]
                            DMA(QTs[u][0:64, c0:c0 + 512], qsrc, [K_(0) if u < 2 else K_(1)], [("QTs", u)], grp="st")
                            DMA(KTs[u][:, c0:c0 + 512], ksrc, [K_(2) if u < 2 else K_(3)], [("KTs", u)], grp="st")
                fr.append(u_stores)
                bk.append(None)

                mslot = NSLOT - 1
                st_ = {}

                def mem_f():
                    for mt in range(2):
                        ps, pk = bank("ms", [4, 5])
                        MM(ps[:], mkT[64:128, mt * 128:(mt + 1) * 128], S_(1)[64:128, :], True, True, ["mkT", K_(1)], [pk])
                        ACTV(Pm[:, mt, :], ps[:], AF.Exp, [pk], ["Pm"])

                def mem_b():
                    po, pok = bank("o", [6])
                    for mt in range(2):
                        MM(po[:], mva[:, mt, :], Pm[:, mt, :], mt == 0, mt == 1, ["Pm", "mva0", "mva1"], [pok])
                    ys = ystg[par]
                    ysk = f"ystg{par}"
                    finalize(po, pok, ys[0:64, :], ysk, 0)
                    DMA(YT[L][mslot][:, c0:c0 + 512], ys[0:64, :], [ysk], [("YT", L, mslot)], grp="st")
                fr.append(mem_f)
                bk.append(mem_b)

                if L == 0:
                    def ring():
                        RED(ksum[:, 2 * tg:2 * tg + 2], S_(4)[:].rearrange("p (b t) -> p b t", t=256), [K_(4)], ["ksum"])
                        CP("dve", kmT[:, 2 * tg:2 * tg + 2], ksum[:, 2 * tg:2 * tg + 2], ["ksum"], ["kmT"])
                        CP("pool", KAr[:, 1:5, :], S_(2)[:].rearrange("p (t c) -> p t c", c=128), [K_(2)], ["KArc"])
                        CP("pool", VAr[:, 1:5, 0:64], vs[:, 0, :, 0:64], [vsk], ["VAr0"])
                    fr.append(ring)
                    bk.append(None)
                    for u in range(2):
                        def gate_f(u=u):
                            selm = selm2[u]
                            gp, gpk = bank("o", [6])
                            for qt in range(4):
                                MM(gp[:, qt * 32:(qt + 1) * 32], S_(3)[u * 64:(u + 1) * 64, qt * 128:(qt + 1) * 128],
                                   kmT[u * 64:(u + 1) * 64, :], True, True, [K_(3), "kmT"], [gpk])
                            TT("dve", gmt[:].rearrange("p (b s) n -> p b s n", s=2),
                               gp[:, 0:128].rearrange("p (b s n) -> p b s n", b=2, s=2),
                               pastb_s[:, 2 * tg:2 * tg + 2, :].unsqueeze(2).to_broadcast([128, 2, 2, 32]), ALU.add,
                               [gpk, "pastb_s"], ["gmt"])
                            for qt in range(4):
                                MAX8(top8[:, qt, :], gmt[:, qt, :], ["gmt"], ["top8"])
                            TS(thr[:], top8[:, :, 2:3], -1e29, ALU.max, ["top8"], ["thr"])
                            TT("dve", selx[:], gmt[:], thr[:].to_broadcast([128, 4, 32]), ALU.is_lt, ["gmt", "thr"], ["selx"])
                            TT("dve", selm[:, :, 1:32].rearrange("p (b s) n -> p b s n", s=2),
                               selx[:, :, 0:31].rearrange("p (b s) n -> p b s n", s=2),
                               ownz_s[:, 2 * tg:2 * tg + 2, 0:31].unsqueeze(2).to_broadcast([128, 2, 2, 31]), ALU.mult,
                               ["selx", "ownz_s"], [f"selm{u}"])
                            CP("dve", selm[:, :, 0:1], rq0_s[:, u, 4 * tg:4 * tg + 4].unsqueeze(2), ["rq0_s"], [f"selm{u}"])

                        def gate_b(u=u):
                            selm = selm2[u]
                            for qt in range(4):
                                TR(pbT[0:32, qt * 128:(qt + 1) * 128], selm[:, qt, :], ident, [f"selm{u}", "cbf"], ["pbT"])
                            ACTV(selT[:], pbT[0:32, 0:512], AF.Copy, ["pbT"], ["selT"])
                            DMA(QTs[u][64:96, c0:c0 + 512], selT[:], ["selT"], [("QTs", u)], grp="st")
                        fr.append(gate_f)
                        bk.append(gate_b)
                    swa_o = {}
                    for hh in range(3):
                        for hq in range(2):
                            half = hh % 2 if hh < 2 else 0
                            qsrc, qkey = (S_(0), K_(0)) if hh < 2 else (S_(1), K_(1))
                            b0 = half * 64
                            it = hh * 2 + hq

                            def swa_f(hh=hh, hq=hq, qsrc=qsrc, qkey=qkey, b0=b0, it=it):
                                sp_, spk = bank("ms", [4, 5])
                                for q2 in range(2):
                                    qt = hq * 2 + q2
                                    for kk in range(2):
                                        MM(sp_[:, (q2 * 2 + kk) * 128:(q2 * 2 + kk + 1) * 128], KAr[b0:b0 + 64, qt + kk, :],
                                           qsrc[b0:b0 + 64, qt * 128:(qt + 1) * 128], True, True, ["KArc", "KArp", qkey], [spk])
                                sS, sP = swS[it % 2], swP[it % 2]
                                TT("dve", sS[:].rearrange("p (q k) c -> p q k c", k=2), sp_[:].rearrange("p (q k c) -> p q k c", q=2, k=2),
                                   maskA_s[:, hh, :, :].unsqueeze(1).to_broadcast([128, 2, 2, 128]), ALU.add,
                                   [spk, "maskA_s"], [f"swS{it % 2}"])
                                ACTV(sP[:], sS[:], AF.Exp, [f"swS{it % 2}"], [f"swP{it % 2}"])

                            def swa_b(hh=hh, hq=hq, it=it):
                                if hq == 0:
                                    swa_o[hh] = bank("o", [6])
                                po, pok = swa_o[hh]
                                sP = swP[it % 2]
                                for q2 in range(2):
                                    qt = hq * 2 + q2
                                    for kk in range(2):
                                        MM(po[:, qt * 128:(qt + 1) * 128], VAr[:, qt + kk, :], sP[:, q2 * 2 + kk, :], kk == 0, kk == 1,
                                           [f"swP{it % 2}", "VAr0", "VAr1", "VArp"], [pok])
                                if hq == 1:
                                    yb = ystg[2 + (hh % 2)]
                                    ybk = f"ystg{2 + (hh % 2)}"
                                    finalize(po, pok, yb[0:64, :], ybk, 1, extra=esink[64:128, hh:hh + 1])
                                    DMA(YT[0][hh][:, c0:c0 + 512], yb[0:64, :], [ybk], [("YT", 0, hh)], grp="st")
                            fr.append(swa_f)
                            bk.append(swa_b)

                    def ring2():
                        CP("pool", KAr[:, 0, :], KAr[:, 4, :], ["KArc"], ["KArp"])
                        CP("pool", VAr[:, 0, :], VAr[:, 4, :], ["VAr0", "VAr1"], ["VArp"])
                    fr.append(ring2)
                    bk.append(None)
                seq = []
                for i in range(len(fr) + 1):
                    if i < len(fr):
                        seq.append(fr[i])
                    if i >= 1 and bk[i - 1] is not None:
                        seq.append(bk[i - 1])
                return seq

            prev = None
            for step in range(NG + 1):
                s1 = stage1(tg_order[step], step % 2) if step < NG else []
                s2 = stage2(*prev) if prev is not None else []
                n = max(len(s1), len(s2))
                i1 = i2 = 0
                for i in range(n):
                    while i1 < len(s1) and i1 * n <= i * len(s1):
                        s1[i1]()
                        i1 += 1
                    while i2 < len(s2) and i2 * n <= i * len(s2):
                        s2[i2]()
                        i2 += 1
                while i1 < len(s1):
                    s1[i1](); i1 += 1
                while i2 < len(s2):
                    s2[i2](); i2 += 1
                prev = (tg_order[step], step % 2) if step < NG else None

            if stop == "A0" and L == 0:
                return
            if L == 1:
                fl = lambda a: a.rearrange("p g h -> p (g h)")
                ps, pk = bank("pa", [0, 1])
                MM(ps[:, 0:192], Umat, fl(lall[:]), True, True, ["cf", "lall"], [pk])
                ACTV(fl(nloc[:]), ps[:, 0:192], AF.Copy, [pk], ["nloc"])
                ps2, pk2 = bank("pa", [0, 1])
                MM(ps2[:, 0:192], E127, fl(nloc[:]), True, True, ["cf", "nloc"], [pk2])
                ACTV(fl(scA[:]), ps2[:, 0:192], AF.Copy, [pk2], ["scA"])
                a_, b_, ak_, bk_ = scA, scB, "scA", "scB"
                for d in (1, 2, 4, 8, 16, 32):
                    TT("dve", b_[:, d:64, :], a_[:, d:64, :], a_[:, 0:64 - d, :], ALU.add, [ak_], [bk_])
                    CP("dve", b_[:, 0:d, :], a_[:, 0:d, :], [ak_], [bk_])
                    a_, b_, ak_, bk_ = b_, a_, bk_, ak_
                TT("dve", kbt[:, :, 1:64].rearrange("p h g -> p g h"), nloc[:, 1:64, :], a_[:, 0:63, :], ALU.add, [ak_, "nloc"], ["kbt"])
                CP("dve", kbt[:, :, 0:1].rearrange("p h g -> p g h"), nloc[:, 0:1, :], ["nloc"], ["kbt"])
                CP("dve", nbf[:], kbt[:].rearrange("p h g -> p g h"), ["kbt"], ["nbf"])
                for ch in range(16):
                    ps, pk = bank("pa", [0, 1])
                    for tt in range(4):
                        G = ch * 4 + tt
                        MM(ps[0:3, tt * 128:(tt + 1) * 128], nbf[:, G, :], ident, True, True, ["nbf", "cbf"], [pk])
                    ACTV(rstg[:], ps[0:3, :], AF.Copy, [pk], ["rstg"], scale=-1.0)
                    DMA(QTs[0:3, 64, ch * 512:(ch + 1) * 512], rstg[:], ["rstg"], [("QTs", 0), ("QTs", 1), ("QTs", 2)], grp="st")

            if stop == "A":
                return

            def gather_slot(sl):
                if debug:
                    DMA(dbg_y[L][sl * 64:(sl + 1) * 64, :], YT[L][sl], [("YT", L, sl)], [("dbgy", L, sl)], grp="so", force=True)
                AG(YT[L][sl], YG[L][sl], [("YT", L, sl)], [("YG", L, sl)])

            for sl in ([0, 1, 2, 5] if L == 0 else [3]):
                gather_slot(sl)
            P.barrier()
            DMA(Kg[64:96, :], kaug, [], ["Kaug"])
            for s_ in range(NBIG):
                DMA(Kg[0:64, :], KTs[s_], [("KTs", s_)], ["Kg"])
                DMA(Vg, VAs[s_], [("VAs", s_)], ["Vg"])
                yslot = 3 + s_ if L == 0 else s_
                for qg in range(NG):
                    c0 = qg * 512
                    qb = Qg[qg % 3]
                    qk_ = f"Qg{qg % 3}"
                    DMA(qb[0:KR, :], QTs[s_][0:KR, c0:c0 + 512], [("QTs", s_)], [qk_], grp="lq")
                    po, pok = bank("O", [3, 4])
                    nkt = 4 * qg + 4
                    pend = []
                    for G in range(nkt + 2):
                        if G < nkt:
                            kt = G - 4 * qg
                            n0 = 128 * kt if kt > 0 else 0
                            sp_, spk = bank("S", [0, 1, 2])
                            MM(sp_[:, n0:512], Kg[0:KR, G * 128:(G + 1) * 128], qb[0:KR, n0:512], True, kt < 0,
                               ["Kg", "Kaug", qk_], [spk])
                            if kt >= 0:
                                MM(sp_[:, n0:n0 + 128], ident, tri, False, True, ["cbf"], [spk])
                            pend.append((G, n0, sp_, spk))
                        if G >= 2:
                            G2, n2, sp2, spk2 = pend.pop(0)
                            pt = Pt[G2 % 4]
                            ptk = f"Pt{G2 % 4}"
                            ACTV(pt[:, n2:512], sp2[:, n2:512], AF.Exp, [spk2, "kbt"], [ptk], bias=kbt[:, s_, G2:G2 + 1])
                            MM(po[:, n2:512], Vg[:, G2, :], pt[:, n2:512], G2 == 0, G2 == nkt - 1, [ptk, "Vg"], [pok])
                    ys = ystg[qg % 2]
                    ysk = f"ystg{qg % 2}"
                    finalize(po, pok, ys[0:64, :], ysk, qg % 2)
                    DMA(YT[L][yslot][:, c0:c0 + 512], ys[0:64, :], [ysk], [("YT", L, yslot)], grp="st")
                gather_slot(yslot)

            if stop in ("B", "G"):
                return
            P.barrier()
            NZ = c["NZ"]
            NCT = NZ // 128
            WZv = WZ[:, 0:8 * NZ].rearrange("p (t c) -> p t c", t=8)
            WOv = WO[:, 0:NCT * 1024].rearrange("p (t c) -> p t c", t=NCT)
            DMA(WZv, wv3(W["wz"]), [], ["WZ"], q="pool", grp="w")
            DMA(WOv, wv3(W["wo"]), [], ["WO"], q="pool", grp="w")
            for tgq in range(4):
                c0 = tgq * 512
                xb, hb = xs[tgq % 2], hT[tgq % 2]
                xk, hk = f"xs{tgq % 2}", f"hT{tgq % 2}"
                if L == 0:
                    DMAdyn(xb[:], xT_v, c0, [], [xk])
                else:
                    for h in range(2):
                        DMA(xb[:, 4 * h:4 * h + 4, :], X1in[tgq][h].rearrange("(t p) n -> p t n", p=128), [("X1in", tgq, h)], [xk])
                    DMA(hb[:], H1in[tgq].rearrange("(t p) n -> p t n", p=128), [("H1in", tgq)], [hk])
                for sl in range(NSLOT):
                    DMAdyn(yg[:, 2 * sl:2 * sl + 2, :], YG[L][sl].rearrange("(t p) n -> p t n", p=128), c0, [("YG", L, sl)],
                           [("ygl", sl), ("ygc", 2 * sl), ("ygc", 2 * sl + 1)])
                if L == 0:
                    rmsnorm(xb[:], xk, hb, hk, gx, 512)
                for ct in range(NCT):
                    ps, pk = bank("pa", [0, 1])
                    for t in range(8):
                        MM(ps[:], WZv[:, t, ct * 128:(ct + 1) * 128], hb[:, t, :], t == 0, t == 7, ["WZ", hk], [pk])
                    ezb = ez[ct % 3]
                    ezk = f"ez{ct % 3}"
                    ACTV(ezb[:], ps[:], AF.Silu, [pk], [ezk])
                    TT("pool" if ct % 2 else "dve", yg[:, ct, :], yg[:, ct, :], ezb[:], ALU.mult, [("ygl", ct // 2), ezk], [("ygc", ct)])
                for oc in range(8):
                    ps, pk = bank("po", [3, 4])
                    for ct in range(NCT):
                        MM(ps[:], WOv[:, ct, oc * 128:(oc + 1) * 128], yg[:, ct, :], ct == 0, ct == NCT - 1, ["WO", ("ygc", ct)], [pk])
                    TT("dve", xb[:, oc, :], ps[:], xb[:, oc, :], ALU.add, [pk, xk], [xk])
                if last:
                    DMA(out_v[:, :, c0:c0 + 512], xb[:], [xk], [("out", tgq)], grp="so")
                else:
                    for h in range(2):
                        DMA(X1in[tgq][h].rearrange("(t p) n -> p t n", p=128), xb[:, 4 * h:4 * h + 4, :], [xk], [("X1in", tgq, h)], grp="so")
                    rmsnorm(xb[:], xk, hb, hk, gxn, 512)
                    DMA(H1in[tgq].rearrange("(t p) n -> p t n", p=128), hb[:], [hk], [("H1in", tgq)], grp="so")
                    AG(H1in[tgq], H1out[tgq], [("H1in", tgq)], [("H1out", tgq)])
                if debug and L == 0:
                    DMA(dbg_x1.rearrange("(t p) n -> p t n", p=128)[:, :, c0:c0 + 512], xb[:], [xk], [("dbgx", tgq)], grp="so")

        for L in range(n_layers):
            layer(L)
        P.emit(st)
    return nc


def _bf(a):
    return np.ascontiguousarray(a).astype(ml_dtypes.bfloat16)


def _consts():
    idn = np.eye(128, dtype=np.float32)
    ones = np.ones((128, 128), np.float32)
    bd = np.zeros((128, 128), np.float32)
    bd[:64, :64] = 1
    bd[64:, 64:] = 1
    k = np.arange(128)[:, None]
    q = np.arange(128)[None, :]
    tri = np.where(k > q, NEGB, 0.0).astype(np.float32)
    cst_bf = _bf(np.stack([idn, ones, bd, tri], 1))
    U = (k <= q).astype(np.float32)
    E = np.zeros((128, 128), np.float32)
    E[127, :] = 1
    cst_f = np.ascontiguousarray(np.stack([U, E], 1))
    kaug = np.zeros((32, S), np.float32)
    kaug[0] = 1
    for n in range(31):
        kaug[1 + n, n * 256:(n + 1) * 256] = NEGB
    pb = np.where(np.arange(32)[None, :] >= np.arange(32)[:, None], -1e30, 0.0).astype(np.float32)
    pastb = np.ascontiguousarray(np.broadcast_to(pb[None], (128, 32, 32)))
    oz = (np.arange(32)[None, :] != np.arange(32)[:, None]).astype(np.float32)
    ownz = np.ascontiguousarray(np.broadcast_to(oz[None], (128, 32, 32)))
    return dict(cst_bf=cst_bf, cst_f=cst_f, kaug=_bf(kaug), pastb=_bf(pastb), ownz=_bf(ownz))


def _col(v):
    return np.ascontiguousarray(v.reshape(8, 128).T)


def _core_inputs(inp, b, j):
    sl = alibi_slopes()
    m = {}
    m["xT"] = np.ascontiguousarray(inp["x"][b].T)
    m["memT"] = np.ascontiguousarray(inp["mem"][b].T)
    w = inp["e_w_in"][0]
    qk = inp["e_qk_norm"][0]
    ga = j % 2
    ah = [3 * ga + i for i in range(3)]
    bh = [(2 * j) % 6, (2 * j + 1) % 6]
    u = lambda c0: w[:, c0:c0 + 64]
    aq = [u(64 * h) for h in ah]
    ak = u(384 + 64 * ga)
    bq = [u(640 + 64 * h) for h in bh]
    bk = [u(1024 + 64 * h) for h in bh]
    mq = u(1792 + 64 * j)
    m["wt0"] = np.ascontiguousarray(np.concatenate([aq[0], aq[1], aq[2], mq, ak, ak, bq[0], bq[1], bk[0], bk[1]], 1))
    m["wv0"] = np.ascontiguousarray(np.concatenate([u(512 + 64 * ga), u(1408 + 64 * bh[0]), u(1408 + 64 * bh[1])], 1))
    wm = inp["e_w_mem_kv"][0]
    m["wmk0"] = np.ascontiguousarray(np.concatenate([wm[:, 64 * j:64 * j + 64]] * 2, 1))
    m["wmv0"] = np.ascontiguousarray(wm[:, 256 + 64 * j:256 + 64 * j + 64])
    g2 = lambda a, bb: np.concatenate([a, bb])[:, None]
    m["gcol0"] = np.ascontiguousarray(np.concatenate([
        g2(qk[0] * SCALE, qk[0] * SCALE), g2(qk[0] * SCALE, qk[4] * SCALE), g2(qk[1], qk[1]),
        g2(qk[2] * SCALE, qk[2] * SCALE), g2(qk[3], qk[3]), g2(qk[5], qk[5])], 1).astype(np.float32))
    m["gx0"] = _col(inp["e_norm"][0])
    m["gm0"] = _col(inp["e_mem_norm"][0])
    ycols, valid = [], []
    seen = set()
    for si in range(6):
        for r in range(4):
            gr = r % 2
            hs = [("A", 3 * gr + i) for i in range(3)] + [("B", (2 * r) % 6), ("B", (2 * r + 1) % 6), ("M", r)]
            kind, h = hs[si]
            yc = {"A": 64 * h, "B": 384 + 64 * h, "M": 768 + 64 * h}[kind]
            ycols.append(yc)
            valid.append((kind, h) not in seen)
            seen.add((kind, h))
    wz = np.concatenate([w[:, 2048 + yc:2048 + yc + 64] for yc in ycols], 1)
    wo_full = inp["e_w_out"][0]
    wo = np.concatenate([wo_full[yc:yc + 64] if v else np.zeros((64, D), np.float32) for yc, v in zip(ycols, valid)], 0)
    m["wz0"] = np.ascontiguousarray(wz)
    m["wo0"] = np.ascontiguousarray(wo)
    kk = np.arange(128)[:, None]
    qq = np.arange(128)[None, :]
    mA = np.zeros((128, 3, 2, 128), np.float32)
    for i, h in enumerate(ah):
        dist_prev = qq - kk + 128
        dist_cur = qq - kk
        mA[:, i, 0, :] = np.where((dist_prev >= 0) & (dist_prev < 128), -sl[h] * dist_prev, NEGB)
        mA[:, i, 1, :] = np.where((dist_cur >= 0) & (dist_cur < 128), -sl[h] * dist_cur, NEGB)
    m["maskA"] = mA
    m["sinkb"] = np.ascontiguousarray(np.broadcast_to(inp["e_sinks"][0][ah][None, :], (128, 3)).astype(np.float32))
    kpos = (128 * np.arange(64)[None, :] + np.arange(128)[:, None]).astype(np.float32)
    m["rq0"] = np.ascontiguousarray(np.stack([-sl[6 + h] * kpos for h in bh], 1).astype(np.float32))
    m["kb0"] = np.ascontiguousarray(np.stack([sl[6 + h] * kpos for h in bh], 1).astype(np.float32))
    w = inp["o_w_in"][0]
    qk = inp["o_qk_norm"][0]
    ch = [3 * j + i for i in range(3)]
    u = lambda c0: w[:, c0:c0 + 64]
    cq = [u(64 * h) for h in ch]
    ck = [u(768 + 64 * h) for h in ch]
    mq = u(2316 + 64 * j)
    m["wt1"] = np.ascontiguousarray(np.concatenate([cq[0], cq[1], cq[2], mq, ck[0], ck[1], ck[2], ck[2]], 1))
    m["wv1"] = np.ascontiguousarray(np.concatenate([u(1536 + 64 * h) for h in ch] + [w[:, 2304 + h:2304 + h + 1] for h in ch], 1))
    wm = inp["o_w_mem_kv"][0]
    m["wmk1"] = np.ascontiguousarray(np.concatenate([wm[:, 64 * j:64 * j + 64]] * 2, 1))
    m["wmv1"] = np.ascontiguousarray(wm[:, 256 + 64 * j:256 + 64 * j + 64])
    m["gcol1"] = np.ascontiguousarray(np.concatenate([
        g2(qk[0] * SCALE, qk[0] * SCALE), g2(qk[0] * SCALE, qk[2] * SCALE), g2(qk[1], qk[1]), g2(qk[1], qk[1]),
        g2(qk[3], qk[3])], 1).astype(np.float32))
    m["gx1"] = _col(inp["o_norm"][0])
    m["gm1"] = _col(inp["o_mem_norm"][0])
    ycols = []
    for si in range(4):
        for r in range(4):
            ycols.append((64 * (3 * r + si)) if si < 3 else (768 + 64 * r))
    m["wz1"] = np.ascontiguousarray(np.concatenate([w[:, 2572 + yc:2572 + yc + 64] for yc in ycols], 1))
    wo_full = inp["o_w_out"][0]
    m["wo1"] = np.ascontiguousarray(np.concatenate([wo_full[yc:yc + 64] for yc in ycols], 0))
    m["bfb"] = np.ascontiguousarray(np.broadcast_to(inp["o_b_f"][0][ch][None, :], (128, 3)).astype(np.float32))
    return m


_NC_CACHE = {}


def kernel(**inputs):
    inp = {k: np.asarray(v, dtype=np.float32) for k, v in inputs.items()}
    if "nc" not in _NC_CACHE:
        _NC_CACHE["nc"] = build()
    nc = _NC_CACHE["nc"]
    cst = _consts()
    in_maps = []
    for c in range(8):
        m = _core_inputs(inp, c // 4, c % 4)
        m.update(cst)
        in_maps.append(m)
    res = run_bass_kernel_spmd(nc, in_maps, core_ids=list(range(8)))
    out = np.empty((2, S, D), np.float32)
    for c in range(8):
        out[c // 4, (c % 4) * 2048:(c % 4 + 1) * 2048, :] = np.asarray(res.results[c]["out"]).T
    return out
```

```python
from contextlib import ExitStack
import numpy as np
import ml_dtypes
import concourse.bass as bass
import concourse.mybir as mybir
from concourse.bass_utils import run_bass_kernel_spmd

F32 = mybir.dt.float32
BF16 = mybir.dt.bfloat16
AF = mybir.ActivationFunctionType
ALU = mybir.AluOpType

S = 8192
D = 1024
HD = 64
NG = 16
EPS = 1e-6
SCALE = HD ** -0.5
NEGB = -30000.0
N_ALIBI = 12


class Op:
    __slots__ = ("eng", "fn", "reads", "writes", "kind", "grp", "ticket", "deps", "need_inc", "idx")


class Prog:
    def __init__(self, nc):
        self.nc = nc
        self.ops = []
        self.last_w = {}
        self.readers = {}
        self.bar = None

    def add(self, eng, fn, reads=(), writes=(), kind="c", grp=None):
        op = Op()
        op.eng, op.fn, op.kind, op.grp = eng, fn, kind, grp
        op.reads, op.writes = tuple(reads), tuple(writes)
        op.ticket, op.need_inc = None, False
        op.idx = len(self.ops)
        deps = set()
        for r in op.reads:
            w = self.last_w.get(r)
            if w is not None:
                deps.add(w)
        for w_ in op.writes:
            w = self.last_w.get(w_)
            if w is not None:
                deps.add(w)
            deps.update(self.readers.get(w_, ()))
        if self.bar is not None:
            deps.add(self.bar)
        deps.discard(op.idx)
        op.deps = sorted(deps)
        for r in op.reads:
            self.readers.setdefault(r, []).append(op.idx)
        for w_ in op.writes:
            self.last_w[w_] = op.idx
            self.readers[w_] = []
        self.ops.append(op)
        return op

    def pe(self, fn, reads=(), writes=()):
        return self.add("pe", fn, reads, writes)

    def act(self, fn, reads=(), writes=()):
        return self.add("act", fn, reads, writes)

    def dve(self, fn, reads=(), writes=()):
        return self.add("dve", fn, reads, writes)

    def pool(self, fn, reads=(), writes=()):
        return self.add("pool", fn, reads, writes)

    def dma(self, fn, reads=(), writes=(), q="sp", grp="d0"):
        return self.add(q, fn, reads, writes, kind="d", grp=grp)

    def barrier(self):
        op = self.add("sp", lambda e: e.nop(), (), ())
        last = {}
        for o in self.ops[:-1]:
            if o.kind == "c":
                last[("e", o.eng)] = o.idx
            elif o.kind == "d":
                last[(o.kind, o.grp, o.idx)] = o.idx
        op.deps = sorted(set(last.values()))
        self.bar = op.idx

    def emit(self, stack):
        nc = self.nc
        ops = self.ops

        def skip(dop, op):
            return dop.kind == "c" and dop.eng == "pe" and op.eng == "pe" and op.kind == "c"

        for op in ops:
            if op.kind != "c":
                op.need_inc = True
            for d in op.deps:
                if not skip(ops[d], op):
                    ops[d].need_inc = True
        cnt = {}
        sems = {}
        NSLOT = {"sp": 24, "pool": 6, "act": 4, "cc": 8}
        rr = {}
        for op in ops:
            if not op.need_inc:
                continue
            if op.kind == "c":
                key, inc = ("e", op.eng), 1
            elif op.kind == "d":
                i = rr.get(op.eng, 0)
                rr[op.eng] = i + 1
                key, inc = ("d", op.eng, i % NSLOT[op.eng]), 16
            else:
                i = rr.get("cc", 0)
                rr["cc"] = i + 1
                key, inc = ("cc", i % NSLOT["cc"]), 1
            cnt[key] = cnt.get(key, 0) + inc
            op.ticket = (key, cnt[key])
            if key not in sems:
                sems[key] = stack.enter_context(nc.semaphore("s_" + "_".join(str(k) for k in key)))
        final = dict(cnt)
        engobj = {"pe": nc.tensor, "act": nc.scalar, "dve": nc.vector, "pool": nc.gpsimd, "sp": nc.sync}
        block = stack.enter_context(nc.Block())

        def stream(ename):
            def body(_e):
                e = engobj[ename]
                seen = {}
                for op in ops:
                    if op.eng != ename:
                        continue
                    need = {}
                    for d in op.deps:
                        dop = ops[d]
                        if dop.ticket is None or skip(dop, op):
                            continue
                        k, v = dop.ticket
                        if v > need.get(k, 0):
                            need[k] = v
                    for k, v in need.items():
                        if seen.get(k, 0) >= v:
                            continue
                        e.wait_ge(sems[k], v)
                        seen[k] = v
                    if op.kind != "c" and op.ticket is not None:
                        k, v = op.ticket
                        prev = v - (16 if op.kind == "d" else 1)
                        if prev > 0 and seen.get(k, 0) < prev:
                            e.wait_ge(sems[k], prev)
                            seen[k] = prev
                    ins = op.fn(e)
                    if op.ticket is not None:
                        k, v = op.ticket
                        ins.then_inc(sems[k], 16 if op.kind == "d" else 1)
                if ename == "sp":
                    for k, v in final.items():
                        if k[0] in ("d", "cc"):
                            e.wait_ge(sems[k], v)
            return body

        for en, deco in (("pe", block.tensor), ("act", block.scalar), ("dve", block.vector),
                         ("pool", block.gpsimd), ("sp", block.sync)):
            deco(stream(en))


def layer_cfg(L):
    if L == 0:
        return dict(NT=5, NV=192, NSLOT=6, NBIG=2, NZ=1536, KR=96, mq_tile=1, nvh=3, nf=0)
    return dict(NT=4, NV=195, NSLOT=4, NBIG=3, NZ=1024, KR=65, mq_tile=1, nvh=3, nf=3)


def alibi_slopes():
    h = np.arange(1, N_ALIBI + 1, dtype=np.float32)
    return (2.0 ** (-8.0 * h / N_ALIBI)).astype(np.float32)


def build(n_layers=2, debug=False, stop=None):
    nc = bass.Bass("TRN2", target_bir_lowering=False)
    P = Prog(nc)
    AX = mybir.AxisListType.X

    def din(name, shape, dt=F32):
        return nc.dram_tensor(name, list(shape), dt, kind="ExternalInput").ap()

    xT = din("xT", [D, S])
    memT = din("memT", [D, 256])
    out = nc.dram_tensor("out", [D, 2048], F32, kind="ExternalOutput").ap()
    cst_bf = din("cst_bf", [128, 4, 128], BF16)
    cst_f = din("cst_f", [128, 2, 128])
    kaug = din("kaug", [32, S], BF16)
    pastb = din("pastb", [128, 32, 32], BF16)
    ownz = din("ownz", [128, 32, 32], BF16)
    LW = []
    for L in range(2):
        c = layer_cfg(L)
        LW.append(dict(
            wt=din(f"wt{L}", [D, c["NT"] * 128]), wv=din(f"wv{L}", [D, c["NV"]]),
            wmk=din(f"wmk{L}", [D, 128]), wmv=din(f"wmv{L}", [D, 64]),
            wz=din(f"wz{L}", [D, c["NZ"]]), wo=din(f"wo{L}", [c["NZ"], D]),
            gx=din(f"gx{L}", [128, 8]), gm=din(f"gm{L}", [128, 8]),
            gcol=din(f"gcol{L}", [128, c["NT"] + 1]),
        ))
    maskA = din("maskA", [128, 3, 2, 128])
    sinkb = din("sinkb", [128, 3])
    rq0 = din("rq0", [128, 2, 64])
    kb0 = din("kb0", [128, 2, 64])
    bfb = din("bfb", [128, 3])
    QTs = nc.dram_tensor("QTs", [3, 97, S], BF16).ap()
    KTs = nc.dram_tensor("KTs", [3, 64, S], BF16).ap()
    VAs = nc.dram_tensor("VAs", [3, 128, 64, 128], BF16).ap()
    YT = [[nc.dram_tensor(f"YT{L}_{i}", [64, S], BF16).ap() for i in range(layer_cfg(L)["NSLOT"])] for L in range(2)]
    YG = [[nc.dram_tensor(f"YG{L}_{i}", [256, S], BF16).ap() for i in range(layer_cfg(L)["NSLOT"])] for L in range(2)]
    X1in = [[nc.dram_tensor(f"X1in{g}_{h}", [512, 512], F32).ap() for h in range(2)] for g in range(4)]
    H1in = [nc.dram_tensor(f"H1in{g}", [1024, 512], BF16).ap() for g in range(4)]
    H1out = [nc.dram_tensor(f"H1out{g}", [4096, 512], BF16).ap() for g in range(4)]
    if debug:
        dbg_y = [nc.dram_tensor("dbg_y0", [384, S], BF16, kind="ExternalOutput").ap(),
                 nc.dram_tensor("dbg_y1", [256, S], BF16, kind="ExternalOutput").ap()]
        dbg_x1 = nc.dram_tensor("dbg_x1", [D, 2048], F32, kind="ExternalOutput").ap()

    with ExitStack() as st:
        def sb(name, shape, dt=F32):
            return st.enter_context(nc.sbuf_tensor(name, list(shape), dt))

        dyn_cache = {}
        pb = [st.enter_context(nc.psum_tensor(f"pb{i}", [128, 512], F32)) for i in range(7)]
        pbT = st.enter_context(nc.psum_tensor("pbT", [128, 1024], BF16))
        rot = {}

        def bank(role, banks):
            i = rot.get(role, 0)
            rot[role] = i + 1
            b = banks[i % len(banks)]
            return pb[b], ("pb", b)

        def MM(out_, lhsT, rhs, start, stop, r, w):
            P.pe(lambda e: e.matmul(out_, lhsT=lhsT, rhs=rhs, start=start, stop=stop), r, w)

        def TR(out_, in_, idn, r, w):
            P.pe(lambda e: e.transpose(out=out_, in_=in_, identity=idn), r, w)

        def ACTV(out_, in_, func, r, w, bias=None, scale=None):
            kw = {}
            if bias is not None:
                kw["bias"] = bias
            if scale is not None:
                kw["scale"] = scale
            P.act(lambda e: e.activation(out=out_, in_=in_, func=func, **kw), r, w)

        def TT(eng, out_, in0, in1, op, r, w):
            P.add(eng, lambda e: e.tensor_tensor(out=out_, in0=in0, in1=in1, op=op), r, w)

        def STT(out_, in0, scalar, in1, op0, op1, r, w):
            P.dve(lambda e: e.scalar_tensor_tensor(out=out_, in0=in0, scalar=scalar, in1=in1, op0=op0, op1=op1), r, w)

        def TS(out_, in0, s1, op0, r, w):
            P.dve(lambda e: e.tensor_scalar(out=out_, in0=in0, scalar1=s1, scalar2=None, op0=op0), r, w)

        def CP(eng, out_, in_, r, w):
            P.add(eng, lambda e: e.tensor_copy(out=out_, in_=in_), r, w)

        def RECIP(out_, in_, r, w):
            P.dve(lambda e: e.reciprocal(out=out_, in_=in_), r, w)

        def MAX8(out_, in_, r, w):
            P.dve(lambda e: e.max(out=out_, in_=in_), r, w)

        def RED(out_, in_, r, w):
            P.dve(lambda e: e.tensor_reduce(out=out_, in_=in_, axis=AX, op=ALU.add), r, w)

        def MEMSET(ap, val, w):
            P.pool(lambda e: e.memset(ap, val), (), w)

        def AG(src, dst, r, w):
            P.add("pool", lambda e: e.collective_compute("AllGather", ALU.bypass, replica_groups=[[0, 1, 2, 3], [4, 5, 6, 7]],
                                                        ins=[src.opt()], outs=[dst.opt()]), r, w, kind="cc", grp="g")

        def DMA(out_, in_, r, w, q="sp", grp="ld", force=False):
            op = P.dma(lambda e: e.dma_start(out=out_, in_=in_), r, w, q=q, grp=grp)
            if force:
                op.need_inc = True
            return op

        cbf = sb("cbf", [128, 4, 128], BF16)
        cf = sb("cf", [128, 2, 128])
        ident, ones, onesbd, tri = cbf[:, 0, :], cbf[:, 1, :], cbf[:, 2, :], cbf[:, 3, :]
        Umat, E127 = cf[:, 0, :], cf[:, 1, :]
        ARENA = sb("arena", [128, 24576], BF16)
        Kg = ARENA[:, 0:8192]
        Vg = ARENA[:, 8192:16384].rearrange("p (g c) -> p g c", c=128)
        WTb = sb("WTb", [128, 8, 640], BF16)
        Wvb = sb("Wvb", [128, 8, 195], BF16)
        WMKb = sb("WMKb", [128, 8, 128], BF16)
        WMVb = sb("WMVb", [128, 8, 64], BF16)
        gx = sb("gx", [128, 8]); gmm = sb("gmm", [128, 8]); gcol = sb("gcol", [128, 6]); gxn = sb("gxn", [128, 8])
        xs = [sb(f"xs{i}", [128, 8, 512]) for i in range(2)]
        hT = [sb(f"hT{i}", [128, 8, 512], BF16) for i in range(2)]
        sq = sb("sq", [128, 8, 512], BF16)
        lnv = [sb(f"lnv{i}", [128, 512]) for i in range(2)]
        rs = [sb(f"rs{i}", [128, 512]) for i in range(2)]
        sq1 = [sb(f"sq1{i}", [128, 512], BF16) for i in range(2)]
        stg = [[sb(f"stg{i}_{p}", [128, 512], BF16) for p in range(2)] for i in range(5)]
        Vstg = [sb(f"Vstg{i}", [128, 3, 4, 128], BF16) for i in range(2)]
        mkT = sb("mkT", [128, 256], BF16)
        mva = sb("mva", [128, 2, 128], BF16)
        Pm = sb("Pm", [128, 2, 512], BF16)
        Pt = [sb(f"Pt{i}", [128, 512], BF16) for i in range(4)]
        Qg = [sb(f"Qg{i}", [128, 512], BF16) for i in range(3)]
        rec = [sb(f"rec{i}", [128, 512]) for i in range(2)]
        ystg = [sb(f"ystg{i}", [128, 512], BF16) for i in range(4)]
        kbt = sb("kbt", [128, 3, 64])
        pastb_s = sb("pastb_s", [128, 32, 32], BF16)
        ownz_s = sb("ownz_s", [128, 32, 32], BF16)
        rq0_s = sb("rq0_s", [128, 2, 64])
        maskA_s = sb("maskA_s", [128, 3, 2, 128]); sink_s = sb("sink_s", [128, 3]); esink = sb("esink", [128, 3])
        KAr = sb("KAr", [128, 5, 128], BF16)
        VAr = sb("VAr", [128, 5, 128], BF16)
        kmT = sb("kmT", [128, 32], BF16)
        ksum = sb("ksum", [128, 32])
        gmt = sb("gmt", [128, 4, 32]); top8 = sb("top8", [128, 4, 8]); thr = sb("thr", [128, 4, 1])
        selx = sb("selx", [128, 4, 32])
        selm2 = [sb(f"selm{i}", [128, 4, 32], BF16) for i in range(2)]
        selT = sb("selT", [32, 512], BF16)
        swS = [sb(f"swS{i}", [128, 4, 128]) for i in range(2)]
        swP = [sb(f"swP{i}", [128, 4, 128], BF16) for i in range(2)]
        bfb_s = sb("bfb_s", [128, 3])
        lall = sb("lall", [128, 64, 3]); nloc = sb("nloc", [128, 64, 3]); scA = sb("scA", [128, 64, 3]); scB = sb("scB", [128, 64, 3])
        nbf = sb("nbf", [128, 64, 3], BF16)
        fb = sb("fb", [128, 3]); fe = sb("fe", [128, 3])
        rstg = sb("rstg", [3, 512], BF16)
        yg = sb("yg", [128, 12, 512], BF16)
        ez = [sb(f"ez{i}", [128, 512], BF16) for i in range(3)]
        WZ = ARENA[:, 0:12288]
        WO = ARENA[:, 12288:24576]

        DMA(cbf[:], cst_bf, [], ["cbf"])
        DMA(cf[:], cst_f, [], ["cf"])
        DMA(pastb_s[:], pastb, [], ["pastb_s"])
        DMA(ownz_s[:], ownz, [], ["ownz_s"])
        DMA(rq0_s[:], rq0, [], ["rq0_s"])
        DMA(maskA_s[:], maskA, [], ["maskA_s"])
        DMA(sink_s[:], sinkb, [], ["sink_s"])
        DMA(bfb_s[:], bfb, [], ["bfb_s"])
        ACTV(esink[:], sink_s[:], AF.Exp, ["sink_s"], ["esink"])

        def rmsnorm(xsrc, xkey, hdst, hkey, gains, n):
            ACTV(sq[:, :, 0:n], xsrc, AF.Square, [xkey], ["sq"])
            ps, pk = bank("ss", [2])
            for t in range(8):
                MM(ps[:, 0:n], ones, sq[:, t, 0:n], t == 0, t == 7, ["sq", "cbf"], [pk])
            ACTV(lnv[0][:, 0:n], ps[:, 0:n], AF.Ln, [pk], ["lnv0"], bias=EPS, scale=1.0 / D)
            ACTV(rs[0][:, 0:n], lnv[0][:, 0:n], AF.Exp, ["lnv0"], ["rs0"], scale=-0.5)
            for t in range(8):
                eng = "dve"
                P.add(eng, lambda e, t=t: e.scalar_tensor_tensor(out=hdst[:, t, :], in0=xsrc[:, t, :], scalar=gains[:, t:t + 1],
                                                                 in1=rs[0][:, 0:n], op0=ALU.mult, op1=ALU.mult),
                      [xkey, "rs0", "gains"], [(hkey, t)])

        def head_rms(ps, pk, n, gc, dst, dkey, i):
            ACTV(sq1[i][:, 0:n], ps[:, 0:n], AF.Square, [pk], [f"sq1{i}"])
            p2, p2k = bank("ss", [2])
            MM(p2[:, 0:n], onesbd, sq1[i][:, 0:n], True, True, [f"sq1{i}", "cbf"], [p2k])
            ACTV(lnv[i][:, 0:n], p2[:, 0:n], AF.Ln, [p2k], [f"lnv{i}"], bias=EPS, scale=1.0 / HD)
            ACTV(rs[i][:, 0:n], lnv[i][:, 0:n], AF.Exp, [f"lnv{i}"], [f"rs{i}"], scale=-0.5)
            STT(dst, ps[:, 0:n], gc, rs[i][:, 0:n], ALU.mult, ALU.mult, [pk, f"rs{i}", "gains"], [dkey])

        def finalize(ops_, opk, dst, dkey, ri, extra=None):
            r = rec[ri]
            rk = f"rec{ri}"
            if extra is None:
                RECIP(r[64:128, :], ops_[64:128, :], [opk], [rk])
            else:
                TS(r[64:128, :], ops_[64:128, :], extra, ALU.add, [opk, "esink"], [rk])
                RECIP(r[64:128, :], r[64:128, :], [rk], [rk])
            TT("dve", dst, ops_[0:64, :], r[64:128, :], ALU.mult, [opk, rk], [dkey])

        def layer(L):
            c = layer_cfg(L)
            W = LW[L]
            NT, NV, KR, NBIG, NSLOT = c["NT"], c["NV"], c["KR"], c["NBIG"], c["NSLOT"]
            xT_v = xT.rearrange("(t p) n -> p t n", p=128)
            out_v = out.rearrange("(t p) n -> p t n", p=128)
            last = (L == n_layers - 1)

            def DMAdyn(out_, src_v, base, r, w):
                def f(e):
                    if "j2048" not in dyn_cache:
                        dyn_cache["j2048"] = e.snap((e.partition_id() % 4) * 2048)
                    return e.dma_start(out=out_, in_=src_v[:, :, bass.ds(dyn_cache["j2048"] + base, 512)])
                P.dma(f, r, w, q="sp", grp="ld")
            P.barrier()
            wv3 = lambda a: a.rearrange("(t p) c -> p t c", p=128)
            DMA(WTb[:, :, 0:NT * 128], wv3(W["wt"]), [], ["WTb"], q="pool", grp="w")
            DMA(Wvb[:, :, 0:NV], wv3(W["wv"]), [], ["Wvb"], q="pool", grp="w")
            DMA(WMKb[:], wv3(W["wmk"]), [], ["WMKb"], q="pool", grp="w")
            DMA(WMVb[:], wv3(W["wmv"]), [], ["WMVb"], q="pool", grp="w")
            DMA(gx[:], W["gx"], [], ["gains"])
            if L == 0:
                DMA(gxn[:], LW[1]["gx"], [], ["gains"])
            DMA(gmm[:], W["gm"], [], ["gains"])
            DMA(gcol[:, 0:NT + 1], W["gcol"], [], ["gains"])
            mx = xs[1][:, :, 0:256]
            DMA(mx, memT.rearrange("(t p) n -> p t n", p=128), [], ["xs1"])
            mh = hT[1][:, :, 0:256]
            rmsnorm(mx, "xs1", mh, "hT1", gmm, 256)
            ps, pk = bank("pa", [0, 1])
            for t in range(8):
                MM(ps[:, 0:256], WMKb[:, t, :], mh[:, t, :], t == 0, t == 7, ["WMKb", ("hT1", t)], [pk])
            head_rms(ps, pk, 256, gcol[:, NT:NT + 1], mkT[:], "mkT", 0)
            MEMSET(mva[:, :, 64:128], 1.0, ["mva1"])
            for mt in range(2):
                ps, pk = bank("pv", [3])
                for t in range(8):
                    MM(ps[:, 0:64], mh[:, t, mt * 128:(mt + 1) * 128], WMVb[:, t, :], t == 0, t == 7, ["WMVb", ("hT1", t)], [pk])
                ACTV(mva[:, mt, 0:64], ps[:, 0:64], AF.Copy, [pk], ["mva0"])
            if L == 0:
                DMA(kbt[:, 0:2, :], kb0, [], ["kbt"])
                MEMSET(ksum[:], 0.0, ["ksum"])
                MEMSET(kmT[:], 0.0, ["kmT"])
                MEMSET(VAr[:, 1:5, 64:128], 1.0, ["VAr1"])
                MEMSET(VAr[:, 0, :], 0.0, ["VArp"])
                MEMSET(KAr[:, 0, :], 0.0, ["KArp"])
            for i in range(2):
                MEMSET(Vstg[i][:, :, :, 64:128], 1.0, [f"Vstg{i}"])

            tg_order = list(range(NG)) if L == 0 else [r * 4 + g for g in range(4) for r in range(4)]

            def stage1(tg, par):
                units = []
                c0 = tg * 512
                xb, hb = xs[par], hT[par]
                xk, hk = f"xs{par}", f"hT{par}"

                def u_load():
                    if L == 0:
                        DMA(xb[:], xT_v[:, :, c0:c0 + 512], [], [xk])
                        rmsnorm(xb[:], xk, hb, hk, gx, 512)
                    else:
                        r, g = tg // 4, tg % 4
                        DMA(hb[:], H1out[g].rearrange("(r t p) n -> r p t n", r=4, t=8)[r], [("H1out", g)], [(hk, t_) for t_ in range(8)])
                units.append(u_load)
                tile_ps = {}

                def u_mm(ti):
                    ps, pk = bank("pa", [0, 1])
                    tile_ps[ti] = (ps, pk)
                    for t in range(8):
                        MM(ps[:], WTb[:, t, ti * 128:(ti + 1) * 128], hb[:, t, :], t == 0, t == 7, ["WTb", (hk, t)], [pk])

                def u_epi(ti):
                    ps, pk = tile_ps[ti]
                    head_rms(ps, pk, 512, gcol[:, ti:ti + 1], stg[ti][par][:], f"stg{ti}_{par}", ti % 2)

                def u_tiles(i):
                    if i < NT:
                        u_mm(i)
                    if i >= 1:
                        u_epi(i - 1)
                for i in range(NT + 1):
                    units.append(lambda i=i: u_tiles(i))
                vs = Vstg[par]
                vsk = f"Vstg{par}"
                for tt in range(4):
                    def u_v(tt=tt):
                        G = tg * 4 + tt
                        ps, pk = bank("pv", [3])
                        for t in range(8):
                            MM(ps[:, 0:NV], hb[:, t, tt * 128:(tt + 1) * 128], Wvb[:, t, 0:NV], t == 0, t == 7, ["Wvb", (hk, t)], [pk])
                        ACTV(vs[:, :, tt, 0:64], ps[:, 0:192].rearrange("p (h c) -> p h c", c=64), AF.Copy, [pk], [vsk])
                        if L == 1:
                            TT("dve", fb[:], ps[:, 192:195], bfb_s[:], ALU.add, [pk, "bfb_s"], ["fb"])
                            ACTV(fe[:], fb[:], AF.Exp, ["fb"], ["fe"], scale=-1.0)
                            ACTV(lall[:, G, :], fe[:], AF.Ln, ["fe"], ["lall"], bias=1.0)
                    units.append(u_v)
                return units

            def stage2(tg, par):
                fr, bk = [], []
                c0 = tg * 512
                vs = Vstg[par]
                vsk = f"Vstg{par}"
                S_ = lambda ti: stg[ti][par]
                K_ = lambda ti: f"stg{ti}_{par}"

                def u_stores():
                    if L == 0:
                        DMA(VAs[0:2, :, tg * 4:tg * 4 + 4, :].rearrange("h p g c -> p h g c"), vs[:, 1:3, :, :],
                            [vsk], [("VAs", 0), ("VAs", 1)], grp="st")
                        for u in range(2):
                            DMA(QTs[u][0:64, c0:c0 + 512], S_(3)[u * 64:(u + 1) * 64, :], [K_(3)], [("QTs", u)], grp="st")
                            DMA(KTs[u][:, c0:c0 + 512], S_(4)[u * 64:(u + 1) * 64, :], [K_(4)], [("KTs", u)], grp="st")
                    else:
                        DMA(VAs[0:3, :, tg * 4:tg * 4 + 4, :].rearrange("h p g c -> p h g c"), vs[:, 0:3, :, :],
                            [vsk], [("VAs", 0), ("VAs", 1), ("VAs", 2)], grp="st")
                        for u in range(3):
                            qsrc = S_(0)[u * 64:(u + 1) * 64, :] if u < 2 else S_(1)[0:64, :]
                            ksrc = S_(2)[u * 64:(u + 1) * 64, :] if u < 2 else S_(3)[0:64, :]
                            DMA(QTs[u][0:64, c0:c0 + 512], qsrc, [K_(0) if u < 2 else K_(1)], [("QTs", u)], grp="st")
                            DMA(KTs[u][:, c0:c0 + 512], ksrc, [K_(2) if u < 2 else K_(3)], [("KTs", u)], grp="st")
                fr.append(u_stores)
                bk.append(None)

                mslot = NSLOT - 1
                st_ = {}

                def mem_f():
                    for mt in range(2):
                        ps, pk = bank("ms", [4, 5])
                        MM(ps[:], mkT[64:128, mt * 128:(mt + 1) * 128], S_(1)[64:128, :], True, True, ["mkT", K_(1)], [pk])
                        ACTV(Pm[:, mt, :], ps[:], AF.Exp, [pk], ["Pm"])

                def mem_b():
                    po, pok = bank("o", [6])
                    for mt in range(2):
                        MM(po[:], mva[:, mt, :], Pm[:, mt, :], mt == 0, mt == 1, ["Pm", "mva0", "mva1"], [pok])
                    ys = ystg[par]
                    ysk = f"ystg{par}"
                    finalize(po, pok, ys[0:64, :], ysk, 0)
                    DMA(YT[L][mslot][:, c0:c0 + 512], ys[0:64, :], [ysk], [("YT", L, mslot)], grp="st")
                fr.append(mem_f)
                bk.append(mem_b)

                if L == 0:
                    def ring():
                        RED(ksum[:, 2 * tg:2 * tg + 2], S_(4)[:].rearrange("p (b t) -> p b t", t=256), [K_(4)], ["ksum"])
                        CP("dve", kmT[:, 2 * tg:2 * tg + 2], ksum[:, 2 * tg:2 * tg + 2], ["ksum"], ["kmT"])
                        CP("pool", KAr[:, 1:5, :], S_(2)[:].rearrange("p (t c) -> p t c", c=128), [K_(2)], ["KArc"])
                        CP("pool", VAr[:, 1:5, 0:64], vs[:, 0, :, 0:64], [vsk], ["VAr0"])
                    fr.append(ring)
                    bk.append(None)
                    for u in range(2):
                        def gate_f(u=u):
                            selm = selm2[u]
                            gp, gpk = bank("o", [6])
                            for qt in range(4):
                                MM(gp[:, qt * 32:(qt + 1) * 32], S_(3)[u * 64:(u + 1) * 64, qt * 128:(qt + 1) * 128],
                                   kmT[u * 64:(u + 1) * 64, :], True, True, [K_(3), "kmT"], [gpk])
                            TT("dve", gmt[:].rearrange("p (b s) n -> p b s n", s=2),
                               gp[:, 0:128].rearrange("p (b s n) -> p b s n", b=2, s=2),
                               pastb_s[:, 2 * tg:2 * tg + 2, :].unsqueeze(2).to_broadcast([128, 2, 2, 32]), ALU.add,
                               [gpk, "pastb_s"], ["gmt"])
                            for qt in range(4):
                                MAX8(top8[:, qt, :], gmt[:, qt, :], ["gmt"], ["top8"])
                            TS(thr[:], top8[:, :, 2:3], -1e29, ALU.max, ["top8"], ["thr"])
                            TT("dve", selx[:], gmt[:], thr[:].to_broadcast([128, 4, 32]), ALU.is_lt, ["gmt", "thr"], ["selx"])
                            TT("dve", selm[:, :, 1:32].rearrange("p (b s) n -> p b s n", s=2),
                               selx[:, :, 0:31].rearrange("p (b s) n -> p b s n", s=2),
                               ownz_s[:, 2 * tg:2 * tg + 2, 0:31].unsqueeze(2).to_broadcast([128, 2, 2, 31]), ALU.mult,
                               ["selx", "ownz_s"], [f"selm{u}"])
                            CP("dve", selm[:, :, 0:1], rq0_s[:, u, 4 * tg:4 * tg + 4].unsqueeze(2), ["rq0_s"], [f"selm{u}"])

                        def gate_b(u=u):
                            selm = selm2[u]
                            for qt in range(4):
                                TR(pbT[0:32, qt * 128:(qt + 1) * 128], selm[:, qt, :], ident, [f"selm{u}", "cbf"], ["pbT"])
                            ACTV(selT[:], pbT[0:32, 0:512], AF.Copy, ["pbT"], ["selT"])
                            DMA(QTs[u][64:96, c0:c0 + 512], selT[:], ["selT"], [("QTs", u)], grp="st")
                        fr.append(gate_f)
                        bk.append(gate_b)
                    swa_o = {}
                    for hh in range(3):
                        for hq in range(2):
                            half = hh % 2 if hh < 2 else 0
                            qsrc, qkey = (S_(0), K_(0)) if hh < 2 else (S_(1), K_(1))
                            b0 = half * 64
                            it = hh * 2 + hq

                            def swa_f(hh=hh, hq=hq, qsrc=qsrc, qkey=qkey, b0=b0, it=it):
                                sp_, spk = bank("ms", [4, 5])
                                for q2 in range(2):
                                    qt = hq * 2 + q2
                                    for kk in range(2):
                                        MM(sp_[:, (q2 * 2 + kk) * 128:(q2 * 2 + kk + 1) * 128], KAr[b0:b0 + 64, qt + kk, :],
                                           qsrc[b0:b0 + 64, qt * 128:(qt + 1) * 128], True, True, ["KArc", "KArp", qkey], [spk])
                                sS, sP = swS[it % 2], swP[it % 2]
                                TT("dve", sS[:].rearrange("p (q k) c -> p q k c", k=2), sp_[:].rearrange("p (q k c) -> p q k c", q=2, k=2),
                                   maskA_s[:, hh, :, :].unsqueeze(1).to_broadcast([128, 2, 2, 128]), ALU.add,
                                   [spk, "maskA_s"], [f"swS{it % 2}"])
                                ACTV(sP[:], sS[:], AF.Exp, [f"swS{it % 2}"], [f"swP{it % 2}"])

                            def swa_b(hh=hh, hq=hq, it=it):
                                if hq == 0:
                                    swa_o[hh] = bank("o", [6])
                                po, pok = swa_o[hh]
                                sP = swP[it % 2]
                                for q2 in range(2):
                                    qt = hq * 2 + q2
                                    for kk in range(2):
                                        MM(po[:, qt * 128:(qt + 1) * 128], VAr[:, qt + kk, :], sP[:, q2 * 2 + kk, :], kk == 0, kk == 1,
                                           [f"swP{it % 2}", "VAr0", "VAr1", "VArp"], [pok])
                                if hq == 1:
                                    yb = ystg[2 + (hh % 2)]
                                    ybk = f"ystg{2 + (hh % 2)}"
                                    finalize(po, pok, yb[0:64, :], ybk, 1, extra=esink[64:128, hh:hh + 1])
                                    DMA(YT[0][hh][:, c0:c0 + 512], yb[0:64, :], [ybk], [("YT", 0, hh)], grp="st")
                            fr.append(swa_f)
                            bk.append(swa_b)

                    def ring2():
                        CP("pool", KAr[:, 0, :], KAr[:, 4, :], ["KArc"], ["KArp"])
                        CP("pool", VAr[:, 0, :], VAr[:, 4, :], ["VAr0", "VAr1"], ["VArp"])
                    fr.append(ring2)
                    bk.append(None)
                seq = []
                for i in range(len(fr) + 1):
                    if i < len(fr):
                        seq.append(fr[i])
                    if i >= 1 and bk[i - 1] is not None:
                        seq.append(bk[i - 1])
                return seq

            prev = None
            for step in range(NG + 1):
                s1 = stage1(tg_order[step], step % 2) if step < NG else []
                s2 = stage2(*prev) if prev is not None else []
                n = max(len(s1), len(s2))
                i1 = i2 = 0
                for i in range(n):
                    while i1 < len(s1) and i1 * n <= i * len(s1):
                        s1[i1]()
                        i1 += 1
                    while i2 < len(s2) and i2 * n <= i * len(s2):
                        s2[i2]()
                        i2 += 1
                while i1 < len(s1):
                    s1[i1](); i1 += 1
                while i2 < len(s2):
                    s2[i2](); i2 += 1
                prev = (tg_order[step], step % 2) if step < NG else None

            if stop == "A0" and L == 0:
                return
            if L == 1:
                fl = lambda a: a.rearrange("p g h -> p (g h)")
                ps, pk = bank("pa", [0, 1])
                MM(ps[:, 0:192], Umat, fl(lall[:]), True, True, ["cf", "lall"], [pk])
                ACTV(fl(nloc[:]), ps[:, 0:192], AF.Copy, [pk], ["nloc"])
                ps2, pk2 = bank("pa", [0, 1])
                MM(ps2[:, 0:192], E127, fl(nloc[:]), True, True, ["cf", "nloc"], [pk2])
                ACTV(fl(scA[:]), ps2[:, 0:192], AF.Copy, [pk2], ["scA"])
                a_, b_, ak_, bk_ = scA, scB, "scA", "scB"
                for d in (1, 2, 4, 8, 16, 32):
                    TT("dve", b_[:, d:64, :], a_[:, d:64, :], a_[:, 0:64 - d, :], ALU.add, [ak_], [bk_])
                    CP("dve", b_[:, 0:d, :], a_[:, 0:d, :], [ak_], [bk_])
                    a_, b_, ak_, bk_ = b_, a_, bk_, ak_
                TT("dve", kbt[:, :, 1:64].rearrange("p h g -> p g h"), nloc[:, 1:64, :], a_[:, 0:63, :], ALU.add, [ak_, "nloc"], ["kbt"])
                CP("dve", kbt[:, :, 0:1].rearrange("p h g -> p g h"), nloc[:, 0:1, :], ["nloc"], ["kbt"])
                CP("dve", nbf[:], kbt[:].rearrange("p h g -> p g h"), ["kbt"], ["nbf"])
                for ch in range(16):
                    ps, pk = bank("pa", [0, 1])
                    for tt in range(4):
                        G = ch * 4 + tt
                        MM(ps[0:3, tt * 128:(tt + 1) * 128], nbf[:, G, :], ident, True, True, ["nbf", "cbf"], [pk])
                    ACTV(rstg[:], ps[0:3, :], AF.Copy, [pk], ["rstg"], scale=-1.0)
                    DMA(QTs[0:3, 64, ch * 512:(ch + 1) * 512], rstg[:], ["rstg"], [("QTs", 0), ("QTs", 1), ("QTs", 2)], grp="st")

            if stop == "A":
                return

            def gather_slot(sl):
                if debug:
                    DMA(dbg_y[L][sl * 64:(sl + 1) * 64, :], YT[L][sl], [("YT", L, sl)], [("dbgy", L, sl)], grp="so", force=True)
                AG(YT[L][sl], YG[L][sl], [("YT", L, sl)], [("YG", L, sl)])

            for sl in ([0, 1, 2, 5] if L == 0 else [3]):
                gather_slot(sl)
            P.barrier()
            DMA(Kg[64:96, :], kaug, [], ["Kaug"])
            for s_ in range(NBIG):
                DMA(Kg[0:64, :], KTs[s_], [("KTs", s_)], ["Kg"])
                DMA(Vg, VAs[s_], [("VAs", s_)], ["Vg"])
                yslot = 3 + s_ if L == 0 else s_
                tiles = []
                qinfo = {}
                for qg in range(NG):
                    nkt = 4 * qg + 4
                    for G in range(nkt):
                        tiles.append((qg, G))
                pend = []

                def start_q(qg):
                    qb = Qg[qg % 3]
                    qk_ = f"Qg{qg % 3}"
                    DMA(qb[0:KR, :], QTs[s_][0:KR, qg * 512:(qg + 1) * 512], [("QTs", s_)], [qk_], grp="lq")
                    qinfo[qg] = (qb, qk_, bank("O", [3, 4]))

                def issue_s(qg, G):
                    if G == 0:
                        start_q(qg)
                    qb, qk_, _ = qinfo[qg]
                    kt = G - 4 * qg
                    n0 = 128 * kt if kt > 0 else 0
                    sp_, spk = bank("S", [0, 1, 2])
                    MM(sp_[:, n0:512], Kg[0:KR, G * 128:(G + 1) * 128], qb[0:KR, n0:512], True, kt < 0, ["Kg", "Kaug", qk_], [spk])
                    if kt >= 0:
                        MM(sp_[:, n0:n0 + 128], ident, tri, False, True, ["cbf"], [spk])
                    pend.append((qg, G, n0, sp_, spk))

                def issue_pv():
                    qg, G, n0, sp_, spk = pend.pop(0)
                    _, _, (po, pok) = qinfo[qg]
                    nkt = 4 * qg + 4
                    pt = Pt[G % 4]
                    ptk = f"Pt{G % 4}"
                    ACTV(pt[:, n0:512], sp_[:, n0:512], AF.Exp, [spk, "kbt"], [ptk], bias=kbt[:, s_, G:G + 1])
                    MM(po[:, n0:512], Vg[:, G, :], pt[:, n0:512], G == 0, G == nkt - 1, [ptk, "Vg"], [pok])
                    if G == nkt - 1:
                        ys = ystg[qg % 2]
                        ysk = f"ystg{qg % 2}"
                        finalize(po, pok, ys[0:64, :], ysk, qg % 2)
                        DMA(YT[L][yslot][:, qg * 512:(qg + 1) * 512], ys[0:64, :], [ysk], [("YT", L, yslot)], grp="st")

                for (qg, G) in tiles:
                    issue_s(qg, G)
                    if len(pend) > 2:
                        issue_pv()
                while pend:
                    issue_pv()
                gather_slot(yslot)

            if stop in ("B", "G"):
                return
            P.barrier()
            NZ = c["NZ"]
            NCT = NZ // 128
            WZv = WZ[:, 0:8 * NZ].rearrange("p (t c) -> p t c", t=8)
            WOv = WO[:, 0:NCT * 1024].rearrange("p (t c) -> p t c", t=NCT)
            DMA(WZv, wv3(W["wz"]), [], ["WZ"], q="pool", grp="w")
            DMA(WOv, wv3(W["wo"]), [], ["WO"], q="pool", grp="w")
            for tgq in range(4):
                c0 = tgq * 512
                xb, hb = xs[tgq % 2], hT[tgq % 2]
                xk, hk = f"xs{tgq % 2}", f"hT{tgq % 2}"
                if L == 0:
                    DMAdyn(xb[:], xT_v, c0, [], [xk])
                else:
                    for h in range(2):
                        DMA(xb[:, 4 * h:4 * h + 4, :], X1in[tgq][h].rearrange("(t p) n -> p t n", p=128), [("X1in", tgq, h)], [xk])
                    DMA(hb[:], H1in[tgq].rearrange("(t p) n -> p t n", p=128), [("H1in", tgq)], [(hk, t_) for t_ in range(8)])
                for sl in range(NSLOT):
                    DMAdyn(yg[:, 2 * sl:2 * sl + 2, :], YG[L][sl].rearrange("(t p) n -> p t n", p=128), c0, [("YG", L, sl)],
                           [("ygl", sl), ("ygc", 2 * sl), ("ygc", 2 * sl + 1)])
                if L == 0:
                    rmsnorm(xb[:], xk, hb, hk, gx, 512)
                for ct in range(NCT):
                    ps, pk = bank("pa", [0, 1])
                    for t in range(8):
                        MM(ps[:], WZv[:, t, ct * 128:(ct + 1) * 128], hb[:, t, :], t == 0, t == 7, ["WZ", (hk, t)], [pk])
                    ezb = ez[ct % 3]
                    ezk = f"ez{ct % 3}"
                    ACTV(ezb[:], ps[:], AF.Silu, [pk], [ezk])
                    TT("pool" if ct % 2 else "dve", yg[:, ct, :], yg[:, ct, :], ezb[:], ALU.mult, [("ygl", ct // 2), ezk], [("ygc", ct)])
                for oc in range(8):
                    ps, pk = bank("po", [3, 4])
                    for ct in range(NCT):
                        MM(ps[:], WOv[:, ct, oc * 128:(oc + 1) * 128], yg[:, ct, :], ct == 0, ct == NCT - 1, ["WO", ("ygc", ct)], [pk])
                    TT("dve", xb[:, oc, :], ps[:], xb[:, oc, :], ALU.add, [pk, xk], [xk])
                if last:
                    DMA(out_v[:, :, c0:c0 + 512], xb[:], [xk], [("out", tgq)], grp="so")
                else:
                    for h in range(2):
                        DMA(X1in[tgq][h].rearrange("(t p) n -> p t n", p=128), xb[:, 4 * h:4 * h + 4, :], [xk], [("X1in", tgq, h)], grp="so")
                    rmsnorm(xb[:], xk, hb, hk, gxn, 512)
                    DMA(H1in[tgq].rearrange("(t p) n -> p t n", p=128), hb[:], [(hk, t_) for t_ in range(8)], [("H1in", tgq)], grp="so")
                    AG(H1in[tgq], H1out[tgq], [("H1in", tgq)], [("H1out", tgq)])
                if debug and L == 0:
                    DMA(dbg_x1.rearrange("(t p) n -> p t n", p=128)[:, :, c0:c0 + 512], xb[:], [xk], [("dbgx", tgq)], grp="so")

        for L in range(n_layers):
            layer(L)
        P.emit(st)
    return nc


def _bf(a):
    return np.ascontiguousarray(a).astype(ml_dtypes.bfloat16)


def _consts():
    idn = np.eye(128, dtype=np.float32)
    ones = np.ones((128, 128), np.float32)
    bd = np.zeros((128, 128), np.float32)
    bd[:64, :64] = 1
    bd[64:, 64:] = 1
    k = np.arange(128)[:, None]
    q = np.arange(128)[None, :]
    tri = np.where(k > q, NEGB, 0.0).astype(np.float32)
    cst_bf = _bf(np.stack([idn, ones, bd, tri], 1))
    U = (k <= q).astype(np.float32)
    E = np.zeros((128, 128), np.float32)
    E[127, :] = 1
    cst_f = np.ascontiguousarray(np.stack([U, E], 1))
    kaug = np.zeros((32, S), np.float32)
    kaug[0] = 1
    for n in range(31):
        kaug[1 + n, n * 256:(n + 1) * 256] = NEGB
    pb = np.where(np.arange(32)[None, :] >= np.arange(32)[:, None], -1e30, 0.0).astype(np.float32)
    pastb = np.ascontiguousarray(np.broadcast_to(pb[None], (128, 32, 32)))
    oz = (np.arange(32)[None, :] != np.arange(32)[:, None]).astype(np.float32)
    ownz = np.ascontiguousarray(np.broadcast_to(oz[None], (128, 32, 32)))
    return dict(cst_bf=cst_bf, cst_f=cst_f, kaug=_bf(kaug), pastb=_bf(pastb), ownz=_bf(ownz))


def _col(v):
    return np.ascontiguousarray(v.reshape(8, 128).T)


def _core_inputs(inp, b, j):
    sl = alibi_slopes()
    m = {}
    m["xT"] = np.ascontiguousarray(inp["x"][b].T)
    m["memT"] = np.ascontiguousarray(inp["mem"][b].T)
    w = inp["e_w_in"][0]
    qk = inp["e_qk_norm"][0]
    ga = j % 2
    ah = [3 * ga + i for i in range(3)]
    bh = [(2 * j) % 6, (2 * j + 1) % 6]
    u = lambda c0: w[:, c0:c0 + 64]
    aq = [u(64 * h) for h in ah]
    ak = u(384 + 64 * ga)
    bq = [u(640 + 64 * h) for h in bh]
    bk = [u(1024 + 64 * h) for h in bh]
    mq = u(1792 + 64 * j)
    m["wt0"] = np.ascontiguousarray(np.concatenate([aq[0], aq[1], aq[2], mq, ak, ak, bq[0], bq[1], bk[0], bk[1]], 1))
    m["wv0"] = np.ascontiguousarray(np.concatenate([u(512 + 64 * ga), u(1408 + 64 * bh[0]), u(1408 + 64 * bh[1])], 1))
    wm = inp["e_w_mem_kv"][0]
    m["wmk0"] = np.ascontiguousarray(np.concatenate([wm[:, 64 * j:64 * j + 64]] * 2, 1))
    m["wmv0"] = np.ascontiguousarray(wm[:, 256 + 64 * j:256 + 64 * j + 64])
    g2 = lambda a, bb: np.concatenate([a, bb])[:, None]
    m["gcol0"] = np.ascontiguousarray(np.concatenate([
        g2(qk[0] * SCALE, qk[0] * SCALE), g2(qk[0] * SCALE, qk[4] * SCALE), g2(qk[1], qk[1]),
        g2(qk[2] * SCALE, qk[2] * SCALE), g2(qk[3], qk[3]), g2(qk[5], qk[5])], 1).astype(np.float32))
    m["gx0"] = _col(inp["e_norm"][0])
    m["gm0"] = _col(inp["e_mem_norm"][0])
    ycols, valid = [], []
    seen = set()
    for si in range(6):
        for r in range(4):
            gr = r % 2
            hs = [("A", 3 * gr + i) for i in range(3)] + [("B", (2 * r) % 6), ("B", (2 * r + 1) % 6), ("M", r)]
            kind, h = hs[si]
            yc = {"A": 64 * h, "B": 384 + 64 * h, "M": 768 + 64 * h}[kind]
            ycols.append(yc)
            valid.append((kind, h) not in seen)
            seen.add((kind, h))
    wz = np.concatenate([w[:, 2048 + yc:2048 + yc + 64] for yc in ycols], 1)
    wo_full = inp["e_w_out"][0]
    wo = np.concatenate([wo_full[yc:yc + 64] if v else np.zeros((64, D), np.float32) for yc, v in zip(ycols, valid)], 0)
    m["wz0"] = np.ascontiguousarray(wz)
    m["wo0"] = np.ascontiguousarray(wo)
    kk = np.arange(128)[:, None]
    qq = np.arange(128)[None, :]
    mA = np.zeros((128, 3, 2, 128), np.float32)
    for i, h in enumerate(ah):
        dist_prev = qq - kk + 128
        dist_cur = qq - kk
        mA[:, i, 0, :] = np.where((dist_prev >= 0) & (dist_prev < 128), -sl[h] * dist_prev, NEGB)
        mA[:, i, 1, :] = np.where((dist_cur >= 0) & (dist_cur < 128), -sl[h] * dist_cur, NEGB)
    m["maskA"] = mA
    m["sinkb"] = np.ascontiguousarray(np.broadcast_to(inp["e_sinks"][0][ah][None, :], (128, 3)).astype(np.float32))
    kpos = (128 * np.arange(64)[None, :] + np.arange(128)[:, None]).astype(np.float32)
    m["rq0"] = np.ascontiguousarray(np.stack([-sl[6 + h] * kpos for h in bh], 1).astype(np.float32))
    m["kb0"] = np.ascontiguousarray(np.stack([sl[6 + h] * kpos for h in bh], 1).astype(np.float32))
    w = inp["o_w_in"][0]
    qk = inp["o_qk_norm"][0]
    ch = [3 * j + i for i in range(3)]
    u = lambda c0: w[:, c0:c0 + 64]
    cq = [u(64 * h) for h in ch]
    ck = [u(768 + 64 * h) for h in ch]
    mq = u(2316 + 64 * j)
    m["wt1"] = np.ascontiguousarray(np.concatenate([cq[0], cq[1], cq[2], mq, ck[0], ck[1], ck[2], ck[2]], 1))
    m["wv1"] = np.ascontiguousarray(np.concatenate([u(1536 + 64 * h) for h in ch] + [w[:, 2304 + h:2304 + h + 1] for h in ch], 1))
    wm = inp["o_w_mem_kv"][0]
    m["wmk1"] = np.ascontiguousarray(np.concatenate([wm[:, 64 * j:64 * j + 64]] * 2, 1))
    m["wmv1"] = np.ascontiguousarray(wm[:, 256 + 64 * j:256 + 64 * j + 64])
    m["gcol1"] = np.ascontiguousarray(np.concatenate([
        g2(qk[0] * SCALE, qk[0] * SCALE), g2(qk[0] * SCALE, qk[2] * SCALE), g2(qk[1], qk[1]), g2(qk[1], qk[1]),
        g2(qk[3], qk[3])], 1).astype(np.float32))
    m["gx1"] = _col(inp["o_norm"][0])
    m["gm1"] = _col(inp["o_mem_norm"][0])
    ycols = []
    for si in range(4):
        for r in range(4):
            ycols.append((64 * (3 * r + si)) if si < 3 else (768 + 64 * r))
    m["wz1"] = np.ascontiguousarray(np.concatenate([w[:, 2572 + yc:2572 + yc + 64] for yc in ycols], 1))
    wo_full = inp["o_w_out"][0]
    m["wo1"] = np.ascontiguousarray(np.concatenate([wo_full[yc:yc + 64] for yc in ycols], 0))
    m["bfb"] = np.ascontiguousarray(np.broadcast_to(inp["o_b_f"][0][ch][None, :], (128, 3)).astype(np.float32))
    return m


_NC_CACHE = {}


def kernel(**inputs):
    inp = {k: np.asarray(v, dtype=np.float32) for k, v in inputs.items()}
    if "nc" not in _NC_CACHE:
        _NC_CACHE["nc"] = build()
    nc = _NC_CACHE["nc"]
    cst = _consts()
    in_maps = []
    for c in range(8):
        m = _core_inputs(inp, c // 4, c % 4)
        m.update(cst)
        in_maps.append(m)
    res = run_bass_kernel_spmd(nc, in_maps, core_ids=list(range(8)))
    out = np.empty((2, S, D), np.float32)
    for c in range(8):
        out[c // 4, (c % 4) * 2048:(c % 4 + 1) * 2048, :] = np.asarray(res.results[c]["out"]).T
    return out
```

```python
from contextlib import ExitStack
import numpy as np
import ml_dtypes
import concourse.bass as bass
import concourse.mybir as mybir
from concourse.bass_utils import run_bass_kernel_spmd

F32 = mybir.dt.float32
BF16 = mybir.dt.bfloat16
AF = mybir.ActivationFunctionType
ALU = mybir.AluOpType

S = 8192
D = 1024
HD = 64
NG = 16
EPS = 1e-6
SCALE = HD ** -0.5
NEGB = -30000.0
N_ALIBI = 12


class Op:
    __slots__ = ("eng", "fn", "reads", "writes", "kind", "grp", "ticket", "deps", "need_inc", "idx")


class Prog:
    def __init__(self, nc):
        self.nc = nc
        self.ops = []
        self.last_w = {}
        self.readers = {}
        self.bar = None

    def add(self, eng, fn, reads=(), writes=(), kind="c", grp=None):
        op = Op()
        op.eng, op.fn, op.kind, op.grp = eng, fn, kind, grp
        op.reads, op.writes = tuple(reads), tuple(writes)
        op.ticket, op.need_inc = None, False
        op.idx = len(self.ops)
        deps = set()
        for r in op.reads:
            w = self.last_w.get(r)
            if w is not None:
                deps.add(w)
        for w_ in op.writes:
            w = self.last_w.get(w_)
            if w is not None:
                deps.add(w)
            deps.update(self.readers.get(w_, ()))
        if self.bar is not None:
            deps.add(self.bar)
        deps.discard(op.idx)
        op.deps = sorted(deps)
        for r in op.reads:
            self.readers.setdefault(r, []).append(op.idx)
        for w_ in op.writes:
            self.last_w[w_] = op.idx
            self.readers[w_] = []
        self.ops.append(op)
        return op

    def pe(self, fn, reads=(), writes=()):
        return self.add("pe", fn, reads, writes)

    def act(self, fn, reads=(), writes=()):
        return self.add("act", fn, reads, writes)

    def dve(self, fn, reads=(), writes=()):
        return self.add("dve", fn, reads, writes)

    def pool(self, fn, reads=(), writes=()):
        return self.add("pool", fn, reads, writes)

    def dma(self, fn, reads=(), writes=(), q="sp", grp="d0"):
        return self.add(q, fn, reads, writes, kind="d", grp=grp)

    def barrier(self):
        op = self.add("sp", lambda e: e.nop(), (), ())
        last = {}
        for o in self.ops[:-1]:
            if o.kind == "c":
                last[("e", o.eng)] = o.idx
            elif o.kind == "d":
                last[(o.kind, o.grp, o.idx)] = o.idx
        op.deps = sorted(set(last.values()))
        self.bar = op.idx

    def emit(self, stack):
        nc = self.nc
        ops = self.ops

        def skip(dop, op):
            return dop.kind == "c" and dop.eng == "pe" and op.eng == "pe" and op.kind == "c"

        for op in ops:
            if op.kind != "c":
                op.need_inc = True
            for d in op.deps:
                if not skip(ops[d], op):
                    ops[d].need_inc = True
        cnt = {}
        sems = {}
        NSLOT = {"sp": 24, "pool": 6, "act": 4, "cc": 8}
        rr = {}
        for op in ops:
            if not op.need_inc:
                continue
            if op.kind == "c":
                key, inc = ("e", op.eng), 1
            elif op.kind == "d":
                i = rr.get(op.eng, 0)
                rr[op.eng] = i + 1
                key, inc = ("d", op.eng, i % NSLOT[op.eng]), 16
            else:
                i = rr.get("cc", 0)
                rr["cc"] = i + 1
                key, inc = ("cc", i % NSLOT["cc"]), 1
            cnt[key] = cnt.get(key, 0) + inc
            op.ticket = (key, cnt[key])
            if key not in sems:
                sems[key] = stack.enter_context(nc.semaphore("s_" + "_".join(str(k) for k in key)))
        final = dict(cnt)
        engobj = {"pe": nc.tensor, "act": nc.scalar, "dve": nc.vector, "pool": nc.gpsimd, "sp": nc.sync}
        block = stack.enter_context(nc.Block())

        def stream(ename):
            def body(_e):
                e = engobj[ename]
                seen = {}
                for op in ops:
                    if op.eng != ename:
                        continue
                    need = {}
                    for d in op.deps:
                        dop = ops[d]
                        if dop.ticket is None or skip(dop, op):
                            continue
                        k, v = dop.ticket
                        if v > need.get(k, 0):
                            need[k] = v
                    for k, v in need.items():
                        if seen.get(k, 0) >= v:
                            continue
                        e.wait_ge(sems[k], v)
                        seen[k] = v
                    if op.kind != "c" and op.ticket is not None:
                        k, v = op.ticket
                        prev = v - (16 if op.kind == "d" else 1)
                        if prev > 0 and seen.get(k, 0) < prev:
                            e.wait_ge(sems[k], prev)
                            seen[k] = prev
                    ins = op.fn(e)
                    if op.ticket is not None:
                        k, v = op.ticket
                        ins.then_inc(sems[k], 16 if op.kind == "d" else 1)
                if ename == "sp":
                    for k, v in final.items():
                        if k[0] in ("d", "cc"):
                            e.wait_ge(sems[k], v)
            return body

        for en, deco in (("pe", block.tensor), ("act", block.scalar), ("dve", block.vector),
                         ("pool", block.gpsimd), ("sp", block.sync)):
            deco(stream(en))


def layer_cfg(L):
    if L == 0:
        return dict(NT=5, NV=192, NSLOT=6, NBIG=2, NZ=1536, KR=96, mq_tile=1, nvh=3, nf=0)
    return dict(NT=4, NV=195, NSLOT=4, NBIG=3, NZ=1024, KR=65, mq_tile=1, nvh=3, nf=3)


def alibi_slopes():
    h = np.arange(1, N_ALIBI + 1, dtype=np.float32)
    return (2.0 ** (-8.0 * h / N_ALIBI)).astype(np.float32)


def build(n_layers=2, debug=False, stop=None):
    nc = bass.Bass("TRN2", target_bir_lowering=False)
    P = Prog(nc)
    AX = mybir.AxisListType.X

    def din(name, shape, dt=F32):
        return nc.dram_tensor(name, list(shape), dt, kind="ExternalInput").ap()

    xT = din("xT", [D, S])
    memT = din("memT", [D, 256])
    out = nc.dram_tensor("out", [D, 2048], F32, kind="ExternalOutput").ap()
    cst_bf = din("cst_bf", [128, 4, 128], BF16)
    cst_f = din("cst_f", [128, 2, 128])
    kaug = din("kaug", [32, S], BF16)
    pastb = din("pastb", [128, 32, 32], BF16)
    ownz = din("ownz", [128, 32, 32], BF16)
    LW = []
    for L in range(2):
        c = layer_cfg(L)
        LW.append(dict(
            wt=din(f"wt{L}", [D, c["NT"] * 128]), wv=din(f"wv{L}", [D, c["NV"]]),
            wmk=din(f"wmk{L}", [D, 128]), wmv=din(f"wmv{L}", [D, 64]),
            wz=din(f"wz{L}", [D, c["NZ"]]), wo=din(f"wo{L}", [c["NZ"], D]),
            gx=din(f"gx{L}", [128, 8]), gm=din(f"gm{L}", [128, 8]),
            gcol=din(f"gcol{L}", [128, c["NT"] + 1]),
        ))
    maskA = din("maskA", [128, 3, 2, 128])
    sinkb = din("sinkb", [128, 3])
    rq0 = din("rq0", [128, 2, 64])
    kb0 = din("kb0", [128, 2, 64])
    bfb = din("bfb", [128, 3])
    QTs = nc.dram_tensor("QTs", [3, 97, S], BF16).ap()
    KTs = nc.dram_tensor("KTs", [3, 64, S], BF16).ap()
    VAs = nc.dram_tensor("VAs", [3, 128, 64, 128], BF16).ap()
    YT = [[nc.dram_tensor(f"YT{L}_{i}", [64, S], BF16).ap() for i in range(layer_cfg(L)["NSLOT"])] for L in range(2)]
    YG = [[nc.dram_tensor(f"YG{L}_{i}", [256, S], BF16).ap() for i in range(layer_cfg(L)["NSLOT"])] for L in range(2)]
    X1in = [[nc.dram_tensor(f"X1in{g}_{h}", [512, 512], F32).ap() for h in range(2)] for g in range(4)]
    H1in = [nc.dram_tensor(f"H1in{g}", [1024, 512], BF16).ap() for g in range(4)]
    H1out = [nc.dram_tensor(f"H1out{g}", [4096, 512], BF16).ap() for g in range(4)]
    if debug:
        dbg_y = [nc.dram_tensor("dbg_y0", [384, S], BF16, kind="ExternalOutput").ap(),
                 nc.dram_tensor("dbg_y1", [256, S], BF16, kind="ExternalOutput").ap()]
        dbg_x1 = nc.dram_tensor("dbg_x1", [D, 2048], F32, kind="ExternalOutput").ap()

    with ExitStack() as st:
        def sb(name, shape, dt=F32):
            return st.enter_context(nc.sbuf_tensor(name, list(shape), dt))

        dyn_cache = {}
        pb = [st.enter_context(nc.psum_tensor(f"pb{i}", [128, 512], F32)) for i in range(7)]
        pbT = st.enter_context(nc.psum_tensor("pbT", [128, 1024], BF16))
        rot = {}

        def bank(role, banks):
            i = rot.get(role, 0)
            rot[role] = i + 1
            b = banks[i % len(banks)]
            return pb[b], ("pb", b)

        def MM(out_, lhsT, rhs, start, stop, r, w):
            P.pe(lambda e: e.matmul(out_, lhsT=lhsT, rhs=rhs, start=start, stop=stop), r, w)

        def TR(out_, in_, idn, r, w):
            P.pe(lambda e: e.transpose(out=out_, in_=in_, identity=idn), r, w)

        def ACTV(out_, in_, func, r, w, bias=None, scale=None):
            kw = {}
            if bias is not None:
                kw["bias"] = bias
            if scale is not None:
                kw["scale"] = scale
            P.act(lambda e: e.activation(out=out_, in_=in_, func=func, **kw), r, w)

        def TT(eng, out_, in0, in1, op, r, w):
            P.add(eng, lambda e: e.tensor_tensor(out=out_, in0=in0, in1=in1, op=op), r, w)

        def STT(out_, in0, scalar, in1, op0, op1, r, w):
            P.dve(lambda e: e.scalar_tensor_tensor(out=out_, in0=in0, scalar=scalar, in1=in1, op0=op0, op1=op1), r, w)

        def TS(out_, in0, s1, op0, r, w):
            P.dve(lambda e: e.tensor_scalar(out=out_, in0=in0, scalar1=s1, scalar2=None, op0=op0), r, w)

        def CP(eng, out_, in_, r, w):
            P.add(eng, lambda e: e.tensor_copy(out=out_, in_=in_), r, w)

        def RECIP(out_, in_, r, w):
            P.dve(lambda e: e.reciprocal(out=out_, in_=in_), r, w)

        def MAX8(out_, in_, r, w):
            P.dve(lambda e: e.max(out=out_, in_=in_), r, w)

        def RED(out_, in_, r, w):
            P.dve(lambda e: e.tensor_reduce(out=out_, in_=in_, axis=AX, op=ALU.add), r, w)

        def MEMSET(ap, val, w):
            P.pool(lambda e: e.memset(ap, val), (), w)

        def AG(src, dst, r, w):
            P.add("pool", lambda e: e.collective_compute("AllGather", ALU.bypass, replica_groups=[[0, 1, 2, 3], [4, 5, 6, 7]],
                                                        ins=[src.opt()], outs=[dst.opt()]), r, w, kind="cc", grp="g")

        def DMA(out_, in_, r, w, q="sp", grp="ld", force=False):
            op = P.dma(lambda e: e.dma_start(out=out_, in_=in_), r, w, q=q, grp=grp)
            if force:
                op.need_inc = True
            return op

        cbf = sb("cbf", [128, 4, 128], BF16)
        cf = sb("cf", [128, 2, 128])
        ident, ones, onesbd, tri = cbf[:, 0, :], cbf[:, 1, :], cbf[:, 2, :], cbf[:, 3, :]
        Umat, E127 = cf[:, 0, :], cf[:, 1, :]
        ARENA = sb("arena", [128, 24576], BF16)
        Kg = ARENA[:, 0:8192]
        Vg = ARENA[:, 8192:16384].rearrange("p (g c) -> p g c", c=128)
        WTb = sb("WTb", [128, 8, 640], BF16)
        Wvb = sb("Wvb", [128, 8, 195], BF16)
        WMKb = sb("WMKb", [128, 8, 128], BF16)
        WMVb = sb("WMVb", [128, 8, 64], BF16)
        gx = sb("gx", [128, 8]); gmm = sb("gmm", [128, 8]); gcol = sb("gcol", [128, 6]); gxn = sb("gxn", [128, 8])
        xs = [sb(f"xs{i}", [128, 8, 512]) for i in range(2)]
        hT = [sb(f"hT{i}", [128, 8, 512], BF16) for i in range(2)]
        sq = sb("sq", [128, 8, 512], BF16)
        lnv = [sb(f"lnv{i}", [128, 512]) for i in range(2)]
        rs = [sb(f"rs{i}", [128, 512]) for i in range(2)]
        sq1 = [sb(f"sq1{i}", [128, 512], BF16) for i in range(2)]
        stg = [[sb(f"stg{i}_{p}", [128, 512], BF16) for p in range(2)] for i in range(5)]
        Vstg = [sb(f"Vstg{i}", [128, 3, 4, 128], BF16) for i in range(2)]
        mkT = sb("mkT", [128, 256], BF16)
        mva = sb("mva", [128, 2, 128], BF16)
        Pm = sb("Pm", [128, 2, 512], BF16)
        Pt = [sb(f"Pt{i}", [128, 512], BF16) for i in range(4)]
        Qg = [sb(f"Qg{i}", [128, 512], BF16) for i in range(3)]
        rec = [sb(f"rec{i}", [128, 512]) for i in range(2)]
        ystg = [sb(f"ystg{i}", [128, 512], BF16) for i in range(4)]
        kbt = sb("kbt", [128, 3, 64])
        pastb_s = sb("pastb_s", [128, 32, 32], BF16)
        ownz_s = sb("ownz_s", [128, 32, 32], BF16)
        rq0_s = sb("rq0_s", [128, 2, 64])
        maskA_s = sb("maskA_s", [128, 3, 2, 128]); sink_s = sb("sink_s", [128, 3]); esink = sb("esink", [128, 3])
        KAr = sb("KAr", [128, 5, 128], BF16)
        VAr = sb("VAr", [128, 5, 128], BF16)
        kmT = sb("kmT", [128, 32], BF16)
        ksum = sb("ksum", [128, 32])
        gmt = sb("gmt", [128, 4, 32]); top8 = sb("top8", [128, 4, 8]); thr = sb("thr", [128, 4, 1])
        selx = sb("selx", [128, 4, 32])
        selm2 = [sb(f"selm{i}", [128, 4, 32], BF16) for i in range(2)]
        selT = sb("selT", [32, 512], BF16)
        swS = [sb(f"swS{i}", [128, 4, 128]) for i in range(2)]
        swP = [sb(f"swP{i}", [128, 4, 128], BF16) for i in range(2)]
        bfb_s = sb("bfb_s", [128, 3])
        lall = sb("lall", [128, 64, 3]); nloc = sb("nloc", [128, 64, 3]); scA = sb("scA", [128, 64, 3]); scB = sb("scB", [128, 64, 3])
        nbf = sb("nbf", [128, 64, 3], BF16)
        fb = sb("fb", [128, 3]); fe = sb("fe", [128, 3])
        rstg = sb("rstg", [3, 512], BF16)
        yg = sb("yg", [128, 12, 512], BF16)
        ez = [sb(f"ez{i}", [128, 512], BF16) for i in range(3)]
        WZ = ARENA[:, 0:12288]
        WO = ARENA[:, 12288:24576]

        DMA(cbf[:], cst_bf, [], ["cbf"])
        DMA(cf[:], cst_f, [], ["cf"])
        DMA(pastb_s[:], pastb, [], ["pastb_s"])
        DMA(ownz_s[:], ownz, [], ["ownz_s"])
        DMA(rq0_s[:], rq0, [], ["rq0_s"])
        DMA(maskA_s[:], maskA, [], ["maskA_s"])
        DMA(sink_s[:], sinkb, [], ["sink_s"])
        DMA(bfb_s[:], bfb, [], ["bfb_s"])
        ACTV(esink[:], sink_s[:], AF.Exp, ["sink_s"], ["esink"])

        def rmsnorm(xsrc, xkey, hdst, hkey, gains, n):
            ACTV(sq[:, :, 0:n], xsrc, AF.Square, [xkey], ["sq"])
            ps, pk = bank("ss", [2])
            for t in range(8):
                MM(ps[:, 0:n], ones, sq[:, t, 0:n], t == 0, t == 7, ["sq", "cbf"], [pk])
            ACTV(lnv[0][:, 0:n], ps[:, 0:n], AF.Ln, [pk], ["lnv0"], bias=EPS, scale=1.0 / D)
            ACTV(rs[0][:, 0:n], lnv[0][:, 0:n], AF.Exp, ["lnv0"], ["rs0"], scale=-0.5)
            for t in range(8):
                eng = "dve"
                P.add(eng, lambda e, t=t: e.scalar_tensor_tensor(out=hdst[:, t, :], in0=xsrc[:, t, :], scalar=gains[:, t:t + 1],
                                                                 in1=rs[0][:, 0:n], op0=ALU.mult, op1=ALU.mult),
                      [xkey, "rs0", "gains"], [(hkey, t)])

        def head_rms(ps, pk, n, gc, dst, dkey, i):
            ACTV(sq1[i][:, 0:n], ps[:, 0:n], AF.Square, [pk], [f"sq1{i}"])
            p2, p2k = bank("ss", [2])
            MM(p2[:, 0:n], onesbd, sq1[i][:, 0:n], True, True, [f"sq1{i}", "cbf"], [p2k])
            ACTV(lnv[i][:, 0:n], p2[:, 0:n], AF.Ln, [p2k], [f"lnv{i}"], bias=EPS, scale=1.0 / HD)
            ACTV(rs[i][:, 0:n], lnv[i][:, 0:n], AF.Exp, [f"lnv{i}"], [f"rs{i}"], scale=-0.5)
            STT(dst, ps[:, 0:n], gc, rs[i][:, 0:n], ALU.mult, ALU.mult, [pk, f"rs{i}", "gains"], [dkey])

        def finalize(ops_, opk, dst, dkey, ri, extra=None):
            r = rec[ri]
            rk = f"rec{ri}"
            if extra is None:
                RECIP(r[64:128, :], ops_[64:128, :], [opk], [rk])
            else:
                TS(r[64:128, :], ops_[64:128, :], extra, ALU.add, [opk, "esink"], [rk])
                RECIP(r[64:128, :], r[64:128, :], [rk], [rk])
            TT("dve", dst, ops_[0:64, :], r[64:128, :], ALU.mult, [opk, rk], [dkey])

        def layer(L):
            c = layer_cfg(L)
            W = LW[L]
            NT, NV, KR, NBIG, NSLOT = c["NT"], c["NV"], c["KR"], c["NBIG"], c["NSLOT"]
            xT_v = xT.rearrange("(t p) n -> p t n", p=128)
            out_v = out.rearrange("(t p) n -> p t n", p=128)
            last = (L == n_layers - 1)

            def DMAdyn(out_, src_v, base, r, w):
                def f(e):
                    if "j2048" not in dyn_cache:
                        dyn_cache["j2048"] = e.snap((e.partition_id() % 4) * 2048)
                    return e.dma_start(out=out_, in_=src_v[:, :, bass.ds(dyn_cache["j2048"] + base, 512)])
                P.dma(f, r, w, q="sp", grp="ld")
            P.barrier()
            wv3 = lambda a: a.rearrange("(t p) c -> p t c", p=128)
            DMA(WTb[:, :, 0:NT * 128], wv3(W["wt"]), [], ["WTb"], q="pool", grp="w")
            DMA(Wvb[:, :, 0:NV], wv3(W["wv"]), [], ["Wvb"], q="pool", grp="w")
            DMA(WMKb[:], wv3(W["wmk"]), [], ["WMKb"], q="pool", grp="w")
            DMA(WMVb[:], wv3(W["wmv"]), [], ["WMVb"], q="pool", grp="w")
            DMA(gx[:], W["gx"], [], ["gains"])
            if L == 0:
                DMA(gxn[:], LW[1]["gx"], [], ["gains"])
            DMA(gmm[:], W["gm"], [], ["gains"])
            DMA(gcol[:, 0:NT + 1], W["gcol"], [], ["gains"])
            mx = xs[1][:, :, 0:256]
            DMA(mx, memT.rearrange("(t p) n -> p t n", p=128), [], ["xs1"])
            mh = hT[1][:, :, 0:256]
            rmsnorm(mx, "xs1", mh, "hT1", gmm, 256)
            ps, pk = bank("pa", [0, 1])
            for t in range(8):
                MM(ps[:, 0:256], WMKb[:, t, :], mh[:, t, :], t == 0, t == 7, ["WMKb", ("hT1", t)], [pk])
            head_rms(ps, pk, 256, gcol[:, NT:NT + 1], mkT[:], "mkT", 0)
            MEMSET(mva[:, :, 64:128], 1.0, ["mva1"])
            for mt in range(2):
                ps, pk = bank("pv", [3])
                for t in range(8):
                    MM(ps[:, 0:64], mh[:, t, mt * 128:(mt + 1) * 128], WMVb[:, t, :], t == 0, t == 7, ["WMVb", ("hT1", t)], [pk])
                ACTV(mva[:, mt, 0:64], ps[:, 0:64], AF.Copy, [pk], ["mva0"])
            if L == 0:
                DMA(kbt[:, 0:2, :], kb0, [], ["kbt"])
                MEMSET(ksum[:], 0.0, ["ksum"])
                MEMSET(kmT[:], 0.0, ["kmT"])
                MEMSET(VAr[:, 1:5, 64:128], 1.0, ["VAr1"])
                MEMSET(VAr[:, 0, :], 0.0, ["VArp"])
                MEMSET(KAr[:, 0, :], 0.0, ["KArp"])
            for i in range(2):
                MEMSET(Vstg[i][:, :, :, 64:128], 1.0, [f"Vstg{i}"])

            tg_order = list(range(NG)) if L == 0 else [r * 4 + g for g in range(4) for r in range(4)]

            def stage1(tg, par, step_):
                units = []
                c0 = tg * 512
                xb, hb = xs[par], hT[par]
                xk, hk = f"xs{par}", f"hT{par}"

                def issue_load(tg_, par_):
                    if L == 0:
                        DMA(xs[par_][:], xT_v[:, :, tg_ * 512:(tg_ + 1) * 512], [], [f"xs{par_}"])
                    else:
                        r, g = tg_ // 4, tg_ % 4
                        DMA(hT[par_][:], H1out[g].rearrange("(r t p) n -> r p t n", r=4, t=8)[r], [("H1out", g)],
                            [(f"hT{par_}", t_) for t_ in range(8)])

                def u_load():
                    if step_ == 0:
                        issue_load(tg, par)
                    if step_ + 1 < NG:
                        issue_load(tg_order[step_ + 1], 1 - par)
                    if L == 0:
                        rmsnorm(xb[:], xk, hb, hk, gx, 512)
                units.append(u_load)
                tile_ps = {}

                def u_mm(ti):
                    ps, pk = bank("pa", [0, 1])
                    tile_ps[ti] = (ps, pk)
                    for t in range(8):
                        MM(ps[:], WTb[:, t, ti * 128:(ti + 1) * 128], hb[:, t, :], t == 0, t == 7, ["WTb", (hk, t)], [pk])

                def u_epi(ti):
                    ps, pk = tile_ps[ti]
                    head_rms(ps, pk, 512, gcol[:, ti:ti + 1], stg[ti][par][:], f"stg{ti}_{par}", ti % 2)

                def u_tiles(i):
                    if i < NT:
                        u_mm(i)
                    if i >= 1:
                        u_epi(i - 1)
                for i in range(NT + 1):
                    units.append(lambda i=i: u_tiles(i))
                vs = Vstg[par]
                vsk = f"Vstg{par}"
                for tt in range(4):
                    def u_v(tt=tt):
                        G = tg * 4 + tt
                        ps, pk = bank("pv", [3])
                        for t in range(8):
                            MM(ps[:, 0:NV], hb[:, t, tt * 128:(tt + 1) * 128], Wvb[:, t, 0:NV], t == 0, t == 7, ["Wvb", (hk, t)], [pk])
                        ACTV(vs[:, :, tt, 0:64], ps[:, 0:192].rearrange("p (h c) -> p h c", c=64), AF.Copy, [pk], [vsk])
                        if L == 1:
                            TT("dve", fb[:], ps[:, 192:195], bfb_s[:], ALU.add, [pk, "bfb_s"], ["fb"])
                            ACTV(fe[:], fb[:], AF.Exp, ["fb"], ["fe"], scale=-1.0)
                            ACTV(lall[:, G, :], fe[:], AF.Ln, ["fe"], ["lall"], bias=1.0)
                    units.append(u_v)
                return units

            def stage2(tg, par):
                fr, bk = [], []
                c0 = tg * 512
                vs = Vstg[par]
                vsk = f"Vstg{par}"
                S_ = lambda ti: stg[ti][par]
                K_ = lambda ti: f"stg{ti}_{par}"

                def u_stores():
                    if L == 0:
                        DMA(VAs[0:2, :, tg * 4:tg * 4 + 4, :].rearrange("h p g c -> p h g c"), vs[:, 1:3, :, :],
                            [vsk], [("VAs", 0), ("VAs", 1)], grp="st")
                        for u in range(2):
                            DMA(QTs[u][0:64, c0:c0 + 512], S_(3)[u * 64:(u + 1) * 64, :], [K_(3)], [("QTs", u)], grp="st")
                            DMA(KTs[u][:, c0:c0 + 512], S_(4)[u * 64:(u + 1) * 64, :], [K_(4)], [("KTs", u)], grp="st")
                    else:
                        DMA(VAs[0:3, :, tg * 4:tg * 4 + 4, :].rearrange("h p g c -> p h g c"), vs[:, 0:3, :, :],
                            [vsk], [("VAs", 0), ("VAs", 1), ("VAs", 2)], grp="st")
                        for u in range(3):
                            qsrc = S_(0)[u * 64:(u + 1) * 64, :] if u < 2 else S_(1)[0:64, :]
                            ksrc = S_(2)[u * 64:(u + 1) * 64, :] if u < 2 else S_(3)[0:64, :]
                            DMA(QTs[u][0:64, c0:c0 + 512], qsrc, [K_(0) if u < 2 else K_(1)], [("QTs", u)], grp="st")
                            DMA(KTs[u][:, c0:c0 + 512], ksrc, [K_(2) if u < 2 else K_(3)], [("KTs", u)], grp="st")
                fr.append(u_stores)
                bk.append(None)

                mslot = NSLOT - 1
                st_ = {}

                def mem_f():
                    for mt in range(2):
                        ps, pk = bank("ms", [4, 5])
                        MM(ps[:], mkT[64:128, mt * 128:(mt + 1) * 128], S_(1)[64:128, :], True, True, ["mkT", K_(1)], [pk])
                        ACTV(Pm[:, mt, :], ps[:], AF.Exp, [pk], ["Pm"])

                def mem_b():
                    po, pok = bank("o", [6])
                    for mt in range(2):
                        MM(po[:], mva[:, mt, :], Pm[:, mt, :], mt == 0, mt == 1, ["Pm", "mva0", "mva1"], [pok])
                    ys = ystg[par]
                    ysk = f"ystg{par}"
                    finalize(po, pok, ys[0:64, :], ysk, 0)
                    DMA(YT[L][mslot][:, c0:c0 + 512], ys[0:64, :], [ysk], [("YT", L, mslot)], grp="st")
                fr.append(mem_f)
                bk.append(mem_b)

                if L == 0:
                    def ring():
                        RED(ksum[:, 2 * tg:2 * tg + 2], S_(4)[:].rearrange("p (b t) -> p b t", t=256), [K_(4)], ["ksum"])
                        CP("dve", kmT[:, 2 * tg:2 * tg + 2], ksum[:, 2 * tg:2 * tg + 2], ["ksum"], ["kmT"])
                        CP("pool", KAr[:, 1:5, :], S_(2)[:].rearrange("p (t c) -> p t c", c=128), [K_(2)], ["KArc"])
                        CP("pool", VAr[:, 1:5, 0:64], vs[:, 0, :, 0:64], [vsk], ["VAr0"])
                    fr.append(ring)
                    bk.append(None)
                    for u in range(2):
                        def gate_f(u=u):
                            selm = selm2[u]
                            gp, gpk = bank("o", [6])
                            for qt in range(4):
                                MM(gp[:, qt * 32:(qt + 1) * 32], S_(3)[u * 64:(u + 1) * 64, qt * 128:(qt + 1) * 128],
                                   kmT[u * 64:(u + 1) * 64, :], True, True, [K_(3), "kmT"], [gpk])
                            TT("dve", gmt[:].rearrange("p (b s) n -> p b s n", s=2),
                               gp[:, 0:128].rearrange("p (b s n) -> p b s n", b=2, s=2),
                               pastb_s[:, 2 * tg:2 * tg + 2, :].unsqueeze(2).to_broadcast([128, 2, 2, 32]), ALU.add,
                               [gpk, "pastb_s"], ["gmt"])
                            for qt in range(4):
                                MAX8(top8[:, qt, :], gmt[:, qt, :], ["gmt"], ["top8"])
                            TS(thr[:], top8[:, :, 2:3], -1e29, ALU.max, ["top8"], ["thr"])
                            TT("dve", selx[:], gmt[:], thr[:].to_broadcast([128, 4, 32]), ALU.is_lt, ["gmt", "thr"], ["selx"])
                            TT("dve", selm[:, :, 1:32].rearrange("p (b s) n -> p b s n", s=2),
                               selx[:, :, 0:31].rearrange("p (b s) n -> p b s n", s=2),
                               ownz_s[:, 2 * tg:2 * tg + 2, 0:31].unsqueeze(2).to_broadcast([128, 2, 2, 31]), ALU.mult,
                               ["selx", "ownz_s"], [f"selm{u}"])
                            CP("dve", selm[:, :, 0:1], rq0_s[:, u, 4 * tg:4 * tg + 4].unsqueeze(2), ["rq0_s"], [f"selm{u}"])

                        def gate_b(u=u):
                            selm = selm2[u]
                            for qt in range(4):
                                TR(pbT[0:32, qt * 128:(qt + 1) * 128], selm[:, qt, :], ident, [f"selm{u}", "cbf"], ["pbT"])
                            ACTV(selT[:], pbT[0:32, 0:512], AF.Copy, ["pbT"], ["selT"])
                            DMA(QTs[u][64:96, c0:c0 + 512], selT[:], ["selT"], [("QTs", u)], grp="st")
                        fr.append(gate_f)
                        bk.append(gate_b)
                    swa_o = {}
                    for hh in range(3):
                        for hq in range(2):
                            half = hh % 2 if hh < 2 else 0
                            qsrc, qkey = (S_(0), K_(0)) if hh < 2 else (S_(1), K_(1))
                            b0 = half * 64
                            it = hh * 2 + hq

                            def swa_f(hh=hh, hq=hq, qsrc=qsrc, qkey=qkey, b0=b0, it=it):
                                sp_, spk = bank("ms", [4, 5])
                                for q2 in range(2):
                                    qt = hq * 2 + q2
                                    for kk in range(2):
                                        MM(sp_[:, (q2 * 2 + kk) * 128:(q2 * 2 + kk + 1) * 128], KAr[b0:b0 + 64, qt + kk, :],
                                           qsrc[b0:b0 + 64, qt * 128:(qt + 1) * 128], True, True, ["KArc", "KArp", qkey], [spk])
                                sS, sP = swS[it % 2], swP[it % 2]
                                TT("dve", sS[:].rearrange("p (q k) c -> p q k c", k=2), sp_[:].rearrange("p (q k c) -> p q k c", q=2, k=2),
                                   maskA_s[:, hh, :, :].unsqueeze(1).to_broadcast([128, 2, 2, 128]), ALU.add,
                                   [spk, "maskA_s"], [f"swS{it % 2}"])
                                ACTV(sP[:], sS[:], AF.Exp, [f"swS{it % 2}"], [f"swP{it % 2}"])

                            def swa_b(hh=hh, hq=hq, it=it):
                                if hq == 0:
                                    swa_o[hh] = bank("o", [6])
                                po, pok = swa_o[hh]
                                sP = swP[it % 2]
                                for q2 in range(2):
                                    qt = hq * 2 + q2
                                    for kk in range(2):
                                        MM(po[:, qt * 128:(qt + 1) * 128], VAr[:, qt + kk, :], sP[:, q2 * 2 + kk, :], kk == 0, kk == 1,
                                           [f"swP{it % 2}", "VAr0", "VAr1", "VArp"], [pok])
                                if hq == 1:
                                    yb = ystg[2 + (hh % 2)]
                                    ybk = f"ystg{2 + (hh % 2)}"
                                    finalize(po, pok, yb[0:64, :], ybk, 1, extra=esink[64:128, hh:hh + 1])
                                    DMA(YT[0][hh][:, c0:c0 + 512], yb[0:64, :], [ybk], [("YT", 0, hh)], grp="st")
                            fr.append(swa_f)
                            bk.append(swa_b)

                    def ring2():
                        CP("pool", KAr[:, 0, :], KAr[:, 4, :], ["KArc"], ["KArp"])
                        CP("pool", VAr[:, 0, :], VAr[:, 4, :], ["VAr0", "VAr1"], ["VArp"])
                    fr.append(ring2)
                    bk.append(None)
                seq = []
                for i in range(len(fr) + 1):
                    if i < len(fr):
                        seq.append(fr[i])
                    if i >= 1 and bk[i - 1] is not None:
                        seq.append(bk[i - 1])
                return seq

            prev = None
            for step in range(NG + 1):
                s1 = stage1(tg_order[step], step % 2, step) if step < NG else []
                s2 = stage2(*prev) if prev is not None else []
                n = max(len(s1), len(s2))
                i1 = i2 = 0
                for i in range(n):
                    while i1 < len(s1) and i1 * n <= i * len(s1):
                        s1[i1]()
                        i1 += 1
                    while i2 < len(s2) and i2 * n <= i * len(s2):
                        s2[i2]()
                        i2 += 1
                while i1 < len(s1):
                    s1[i1](); i1 += 1
                while i2 < len(s2):
                    s2[i2](); i2 += 1
                prev = (tg_order[step], step % 2) if step < NG else None

            if stop == "A0" and L == 0:
                return
            if L == 1:
                fl = lambda a: a.rearrange("p g h -> p (g h)")
                ps, pk = bank("pa", [0, 1])
                MM(ps[:, 0:192], Umat, fl(lall[:]), True, True, ["cf", "lall"], [pk])
                ACTV(fl(nloc[:]), ps[:, 0:192], AF.Copy, [pk], ["nloc"])
                ps2, pk2 = bank("pa", [0, 1])
                MM(ps2[:, 0:192], E127, fl(nloc[:]), True, True, ["cf", "nloc"], [pk2])
                ACTV(fl(scA[:]), ps2[:, 0:192], AF.Copy, [pk2], ["scA"])
                a_, b_, ak_, bk_ = scA, scB, "scA", "scB"
                for d in (1, 2, 4, 8, 16, 32):
                    TT("dve", b_[:, d:64, :], a_[:, d:64, :], a_[:, 0:64 - d, :], ALU.add, [ak_], [bk_])
                    CP("dve", b_[:, 0:d, :], a_[:, 0:d, :], [ak_], [bk_])
                    a_, b_, ak_, bk_ = b_, a_, bk_, ak_
                TT("dve", kbt[:, :, 1:64].rearrange("p h g -> p g h"), nloc[:, 1:64, :], a_[:, 0:63, :], ALU.add, [ak_, "nloc"], ["kbt"])
                CP("dve", kbt[:, :, 0:1].rearrange("p h g -> p g h"), nloc[:, 0:1, :], ["nloc"], ["kbt"])
                CP("dve", nbf[:], kbt[:].rearrange("p h g -> p g h"), ["kbt"], ["nbf"])
                for ch in range(16):
                    ps, pk = bank("pa", [0, 1])
                    for tt in range(4):
                        G = ch * 4 + tt
                        MM(ps[0:3, tt * 128:(tt + 1) * 128], nbf[:, G, :], ident, True, True, ["nbf", "cbf"], [pk])
                    ACTV(rstg[:], ps[0:3, :], AF.Copy, [pk], ["rstg"], scale=-1.0)
                    DMA(QTs[0:3, 64, ch * 512:(ch + 1) * 512], rstg[:], ["rstg"], [("QTs", 0), ("QTs", 1), ("QTs", 2)], grp="st")

            if stop == "A":
                return

            def gather_slot(sl):
                if debug:
                    DMA(dbg_y[L][sl * 64:(sl + 1) * 64, :], YT[L][sl], [("YT", L, sl)], [("dbgy", L, sl)], grp="so", force=True)
                AG(YT[L][sl], YG[L][sl], [("YT", L, sl)], [("YG", L, sl)])

            for sl in ([0, 1, 2, 5] if L == 0 else [3]):
                gather_slot(sl)
            P.barrier()
            DMA(Kg[64:96, :], kaug, [], ["Kaug"])
            for s_ in range(NBIG):
                DMA(Kg[0:64, :], KTs[s_], [("KTs", s_)], ["Kg"])
                DMA(Vg, VAs[s_], [("VAs", s_)], ["Vg"])
                yslot = 3 + s_ if L == 0 else s_
                tiles = []
                qinfo = {}
                for qg in range(NG):
                    nkt = 4 * qg + 4
                    for G in range(nkt):
                        tiles.append((qg, G))
                pend = []

                def start_q(qg):
                    qb = Qg[qg % 3]
                    qk_ = f"Qg{qg % 3}"
                    DMA(qb[0:KR, :], QTs[s_][0:KR, qg * 512:(qg + 1) * 512], [("QTs", s_)], [qk_], grp="lq")
                    qinfo[qg] = (qb, qk_, (pb[3 + qg % 2], ("pb", 3 + qg % 2)))

                def issue_s(qg, G):
                    if G == 0:
                        if qg not in qinfo:
                            start_q(qg)
                        if qg + 1 < NG:
                            start_q(qg + 1)
                    qb, qk_, _ = qinfo[qg]
                    kt = G - 4 * qg
                    n0 = 128 * kt if kt > 0 else 0
                    sp_, spk = bank("S", [0, 1, 2])
                    MM(sp_[:, n0:512], Kg[0:KR, G * 128:(G + 1) * 128], qb[0:KR, n0:512], True, kt < 0, ["Kg", "Kaug", qk_], [spk])
                    if kt >= 0:
                        MM(sp_[:, n0:n0 + 128], ident, tri, False, True, ["cbf"], [spk])
                    pend.append((qg, G, n0, sp_, spk))

                def issue_pv():
                    qg, G, n0, sp_, spk = pend.pop(0)
                    _, _, (po, pok) = qinfo[qg]
                    nkt = 4 * qg + 4
                    pt = Pt[G % 4]
                    ptk = f"Pt{G % 4}"
                    ACTV(pt[:, n0:512], sp_[:, n0:512], AF.Exp, [spk, "kbt"], [ptk], bias=kbt[:, s_, G:G + 1])
                    MM(po[:, n0:512], Vg[:, G, :], pt[:, n0:512], G == 0, G == nkt - 1, [ptk, "Vg"], [pok])
                    if G == nkt - 1:
                        ys = ystg[qg % 2]
                        ysk = f"ystg{qg % 2}"
                        finalize(po, pok, ys[0:64, :], ysk, qg % 2)
                        DMA(YT[L][yslot][:, qg * 512:(qg + 1) * 512], ys[0:64, :], [ysk], [("YT", L, yslot)], grp="st")

                for (qg, G) in tiles:
                    issue_s(qg, G)
                    if len(pend) > 2:
                        issue_pv()
                while pend:
                    issue_pv()
                gather_slot(yslot)

            if stop in ("B", "G"):
                return
            P.barrier()
            NZ = c["NZ"]
            NCT = NZ // 128
            WZv = WZ[:, 0:8 * NZ].rearrange("p (t c) -> p t c", t=8)
            WOv = WO[:, 0:NCT * 1024].rearrange("p (t c) -> p t c", t=NCT)
            DMA(WZv, wv3(W["wz"]), [], ["WZ"], q="pool", grp="w")
            DMA(WOv, wv3(W["wo"]), [], ["WO"], q="pool", grp="w")
            for tgq in range(4):
                c0 = tgq * 512
                xb, hb = xs[tgq % 2], hT[tgq % 2]
                xk, hk = f"xs{tgq % 2}", f"hT{tgq % 2}"
                if L == 0:
                    DMAdyn(xb[:], xT_v, c0, [], [xk])
                else:
                    for h in range(2):
                        DMA(xb[:, 4 * h:4 * h + 4, :], X1in[tgq][h].rearrange("(t p) n -> p t n", p=128), [("X1in", tgq, h)], [xk])
                    DMA(hb[:], H1in[tgq].rearrange("(t p) n -> p t n", p=128), [("H1in", tgq)], [(hk, t_) for t_ in range(8)])
                for sl in range(NSLOT):
                    DMAdyn(yg[:, 2 * sl:2 * sl + 2, :], YG[L][sl].rearrange("(t p) n -> p t n", p=128), c0, [("YG", L, sl)],
                           [("ygl", sl), ("ygc", 2 * sl), ("ygc", 2 * sl + 1)])
                if L == 0:
                    rmsnorm(xb[:], xk, hb, hk, gx, 512)
                for ct in range(NCT):
                    ps, pk = bank("pa", [0, 1])
                    for t in range(8):
                        MM(ps[:], WZv[:, t, ct * 128:(ct + 1) * 128], hb[:, t, :], t == 0, t == 7, ["WZ", (hk, t)], [pk])
                    ezb = ez[ct % 3]
                    ezk = f"ez{ct % 3}"
                    ACTV(ezb[:], ps[:], AF.Silu, [pk], [ezk])
                    TT("pool" if ct % 2 else "dve", yg[:, ct, :], yg[:, ct, :], ezb[:], ALU.mult, [("ygl", ct // 2), ezk], [("ygc", ct)])
                for oc in range(8):
                    ps, pk = bank("po", [3, 4])
                    for ct in range(NCT):
                        MM(ps[:], WOv[:, ct, oc * 128:(oc + 1) * 128], yg[:, ct, :], ct == 0, ct == NCT - 1, ["WO", ("ygc", ct)], [pk])
                    TT("dve", xb[:, oc, :], ps[:], xb[:, oc, :], ALU.add, [pk, xk], [xk])
                if last:
                    DMA(out_v[:, :, c0:c0 + 512], xb[:], [xk], [("out", tgq)], grp="so")
                else:
                    for h in range(2):
                        DMA(X1in[tgq][h].rearrange("(t p) n -> p t n", p=128), xb[:, 4 * h:4 * h + 4, :], [xk], [("X1in", tgq, h)], grp="so")
                    rmsnorm(xb[:], xk, hb, hk, gxn, 512)
                    DMA(H1in[tgq].rearrange("(t p) n -> p t n", p=128), hb[:], [(hk, t_) for t_ in range(8)], [("H1in", tgq)], grp="so")
                    AG(H1in[tgq], H1out[tgq], [("H1in", tgq)], [("H1out", tgq)])
                if debug and L == 0:
                    DMA(dbg_x1.rearrange("(t p) n -> p t n", p=128)[:, :, c0:c0 + 512], xb[:], [xk], [("dbgx", tgq)], grp="so")

        for L in range(n_layers):
            layer(L)
        P.emit(st)
    return nc


def _bf(a):
    return np.ascontiguousarray(a).astype(ml_dtypes.bfloat16)


def _consts():
    idn = np.eye(128, dtype=np.float32)
    ones = np.ones((128, 128), np.float32)
    bd = np.zeros((128, 128), np.float32)
    bd[:64, :64] = 1
    bd[64:, 64:] = 1
    k = np.arange(128)[:, None]
    q = np.arange(128)[None, :]
    tri = np.where(k > q, NEGB, 0.0).astype(np.float32)
    cst_bf = _bf(np.stack([idn, ones, bd, tri], 1))
    U = (k <= q).astype(np.float32)
    E = np.zeros((128, 128), np.float32)
    E[127, :] = 1
    cst_f = np.ascontiguousarray(np.stack([U, E], 1))
    kaug = np.zeros((32, S), np.float32)
    kaug[0] = 1
    for n in range(31):
        kaug[1 + n, n * 256:(n + 1) * 256] = NEGB
    pb = np.where(np.arange(32)[None, :] >= np.arange(32)[:, None], -1e30, 0.0).astype(np.float32)
    pastb = np.ascontiguousarray(np.broadcast_to(pb[None], (128, 32, 32)))
    oz = (np.arange(32)[None, :] != np.arange(32)[:, None]).astype(np.float32)
    ownz = np.ascontiguousarray(np.broadcast_to(oz[None], (128, 32, 32)))
    return dict(cst_bf=cst_bf, cst_f=cst_f, kaug=_bf(kaug), pastb=_bf(pastb), ownz=_bf(ownz))


def _col(v):
    return np.ascontiguousarray(v.reshape(8, 128).T)


def _core_inputs(inp, b, j):
    sl = alibi_slopes()
    m = {}
    m["xT"] = np.ascontiguousarray(inp["x"][b].T)
    m["memT"] = np.ascontiguousarray(inp["mem"][b].T)
    w = inp["e_w_in"][0]
    qk = inp["e_qk_norm"][0]
    ga = j % 2
    ah = [3 * ga + i for i in range(3)]
    bh = [(2 * j) % 6, (2 * j + 1) % 6]
    u = lambda c0: w[:, c0:c0 + 64]
    aq = [u(64 * h) for h in ah]
    ak = u(384 + 64 * ga)
    bq = [u(640 + 64 * h) for h in bh]
    bk = [u(1024 + 64 * h) for h in bh]
    mq = u(1792 + 64 * j)
    m["wt0"] = np.ascontiguousarray(np.concatenate([aq[0], aq[1], aq[2], mq, ak, ak, bq[0], bq[1], bk[0], bk[1]], 1))
    m["wv0"] = np.ascontiguousarray(np.concatenate([u(512 + 64 * ga), u(1408 + 64 * bh[0]), u(1408 + 64 * bh[1])], 1))
    wm = inp["e_w_mem_kv"][0]
    m["wmk0"] = np.ascontiguousarray(np.concatenate([wm[:, 64 * j:64 * j + 64]] * 2, 1))
    m["wmv0"] = np.ascontiguousarray(wm[:, 256 + 64 * j:256 + 64 * j + 64])
    g2 = lambda a, bb: np.concatenate([a, bb])[:, None]
    m["gcol0"] = np.ascontiguousarray(np.concatenate([
        g2(qk[0] * SCALE, qk[0] * SCALE), g2(qk[0] * SCALE, qk[4] * SCALE), g2(qk[1], qk[1]),
        g2(qk[2] * SCALE, qk[2] * SCALE), g2(qk[3], qk[3]), g2(qk[5], qk[5])], 1).astype(np.float32))
    m["gx0"] = _col(inp["e_norm"][0])
    m["gm0"] = _col(inp["e_mem_norm"][0])
    ycols, valid = [], []
    seen = set()
    for si in range(6):
        for r in range(4):
            gr = r % 2
            hs = [("A", 3 * gr + i) for i in range(3)] + [("B", (2 * r) % 6), ("B", (2 * r + 1) % 6), ("M", r)]
            kind, h = hs[si]
            yc = {"A": 64 * h, "B": 384 + 64 * h, "M": 768 + 64 * h}[kind]
            ycols.append(yc)
            valid.append((kind, h) not in seen)
            seen.add((kind, h))
    wz = np.concatenate([w[:, 2048 + yc:2048 + yc + 64] for yc in ycols], 1)
    wo_full = inp["e_w_out"][0]
    wo = np.concatenate([wo_full[yc:yc + 64] if v else np.zeros((64, D), np.float32) for yc, v in zip(ycols, valid)], 0)
    m["wz0"] = np.ascontiguousarray(wz)
    m["wo0"] = np.ascontiguousarray(wo)
    kk = np.arange(128)[:, None]
    qq = np.arange(128)[None, :]
    mA = np.zeros((128, 3, 2, 128), np.float32)
    for i, h in enumerate(ah):
        dist_prev = qq - kk + 128
        dist_cur = qq - kk
        mA[:, i, 0, :] = np.where((dist_prev >= 0) & (dist_prev < 128), -sl[h] * dist_prev, NEGB)
        mA[:, i, 1, :] = np.where((dist_cur >= 0) & (dist_cur < 128), -sl[h] * dist_cur, NEGB)
    m["maskA"] = mA
    m["sinkb"] = np.ascontiguousarray(np.broadcast_to(inp["e_sinks"][0][ah][None, :], (128, 3)).astype(np.float32))
    kpos = (128 * np.arange(64)[None, :] + np.arange(128)[:, None]).astype(np.float32)
    m["rq0"] = np.ascontiguousarray(np.stack([-sl[6 + h] * kpos for h in bh], 1).astype(np.float32))
    m["kb0"] = np.ascontiguousarray(np.stack([sl[6 + h] * kpos for h in bh], 1).astype(np.float32))
    w = inp["o_w_in"][0]
    qk = inp["o_qk_norm"][0]
    ch = [3 * j + i for i in range(3)]
    u = lambda c0: w[:, c0:c0 + 64]
    cq = [u(64 * h) for h in ch]
    ck = [u(768 + 64 * h) for h in ch]
    mq = u(2316 + 64 * j)
    m["wt1"] = np.ascontiguousarray(np.concatenate([cq[0], cq[1], cq[2], mq, ck[0], ck[1], ck[2], ck[2]], 1))
    m["wv1"] = np.ascontiguousarray(np.concatenate([u(1536 + 64 * h) for h in ch] + [w[:, 2304 + h:2304 + h + 1] for h in ch], 1))
    wm = inp["o_w_mem_kv"][0]
    m["wmk1"] = np.ascontiguousarray(np.concatenate([wm[:, 64 * j:64 * j + 64]] * 2, 1))
    m["wmv1"] = np.ascontiguousarray(wm[:, 256 + 64 * j:256 + 64 * j + 64])
    m["gcol1"] = np.ascontiguousarray(np.concatenate([
        g2(qk[0] * SCALE, qk[0] * SCALE), g2(qk[0] * SCALE, qk[2] * SCALE), g2(qk[1], qk[1]), g2(qk[1], qk[1]),
        g2(qk[3], qk[3])], 1).astype(np.float32))
    m["gx1"] = _col(inp["o_norm"][0])
    m["gm1"] = _col(inp["o_mem_norm"][0])
    ycols = []
    for si in range(4):
        for r in range(4):
            ycols.append((64 * (3 * r + si)) if si < 3 else (768 + 64 * r))
    m["wz1"] = np.ascontiguousarray(np.concatenate([w[:, 2572 + yc:2572 + yc + 64] for yc in ycols], 1))
    wo_full = inp["o_w_out"][0]
    m["wo1"] = np.ascontiguousarray(np.concatenate([wo_full[yc:yc + 64] for yc in ycols], 0))
    m["bfb"] = np.ascontiguousarray(np.broadcast_to(inp["o_b_f"][0][ch][None, :], (128, 3)).astype(np.float32))
    return m


_NC_CACHE = {}


def kernel(**inputs):
    inp = {k: np.asarray(v, dtype=np.float32) for k, v in inputs.items()}
    if "nc" not in _NC_CACHE:
        _NC_CACHE["nc"] = build()
    nc = _NC_CACHE["nc"]
    cst = _consts()
    in_maps = []
    for c in range(8):
        m = _core_inputs(inp, c // 4, c % 4)
        m.update(cst)
        in_maps.append(m)
    res = run_bass_kernel_spmd(nc, in_maps, core_ids=list(range(8)))
    out = np.empty((2, S, D), np.float32)
    for c in range(8):
        out[c // 4, (c % 4) * 2048:(c % 4 + 1) * 2048, :] = np.asarray(res.results[c]["out"]).T
    return out
```
